# Optimizing a Trainium2 kernel written in Bass

```python
import jax, jax.numpy as jnp
from jax import lax
import numpy as np

D_MODEL = 2048
BATCH = 2
SEQ = 16384
DEPTH = 4

HEAD_DIM = 64
A_HEADS = 16
A_WIDTH = A_HEADS * HEAD_DIM
A_LORA_W = 64
A_LORA_A = 64
A_LORA_G = 128
A_COLS = 3 * A_WIDTH + A_LORA_W + A_LORA_A + A_LORA_G
A_GN_EPS = 64e-5
B_HEADS = 8
B_DK = 128
B_DV = 128
B_WIDTH = B_HEADS * B_DV
B_COLS = 2 * B_HEADS * B_DK + 2 * B_WIDTH
B_CHUNK = 32
EVEN_COLS = A_COLS + B_COLS
C_HEADS = 16
C_WIDTH = C_HEADS * HEAD_DIM
C_PAIRS = ((128, 1), (512, 4), (2048, 16))
D_HEADS = 4
D_DQK = 128
D_DV = 256
D_QK_WIDTH = D_HEADS * D_DQK
D_WIDTH = D_HEADS * D_DV
D_CONV = 4
D_CHUNK = 64
ODD_COLS = 3 * C_WIDTH + 2 * D_QK_WIDTH + 2 * D_WIDTH + 2 * D_HEADS
MIX_WIDTH = A_WIDTH + B_WIDTH
N_GROUPS = 4
EXPERTS_PER_GROUP = 8
N_EXPERTS = N_GROUPS * EXPERTS_PER_GROUP
TOP_K = 2
D_EXPERT = 512
MOE_BLOCK = 128
N_EVEN = (DEPTH + 1) // 2
N_ODD = DEPTH // 2
DN_ALPHA = (2 * DEPTH) ** 0.25
DN_BETA = (8 * DEPTH) ** -0.25
LN_EPS = 1e-5
RMS_EPS = 1e-6
F32 = jnp.float32

kernel_name = 'hybrid_rwkv7_hgrn2_dilattn_mlstm_hmoe_deepnorm'


def layer_norm(x, w, b):
    xf = x.astype(F32)
    mu = jnp.mean(xf, -1, keepdims=True)
    var = jnp.mean(jnp.square(xf - mu), -1, keepdims=True)
    return (xf - mu) * lax.rsqrt(var + LN_EPS) * w.astype(F32) + b.astype(F32)


def token_shift(t):
    return jnp.pad(t, ((0, 0), (1, 0), (0, 0)))[:, :-1]


def causal_depthwise_conv(t, w, b):
    ch = t.shape[-1]
    out = lax.conv_general_dilated(t, w[:, None, :].astype(t.dtype), window_strides=(1,),
                                   padding=[(w.shape[0] - 1, 0)],
                                   dimension_numbers=('NWC', 'WIO', 'NWC'),
                                   feature_group_count=ch)
    return out + b


def to_chunks(t, c):
    b_, s, h, f = t.shape
    return t.reshape(b_, s // c, c, h, f).transpose(1, 0, 3, 2, 4)


def from_chunks(t):
    n, b_, h, c, f = t.shape
    return t.transpose(1, 0, 3, 2, 4).reshape(b_, n * c, h, f)


def rwkv7_scan(r, w, k, v, a_vec, b_vec):
    b_, s, h, n = r.shape
    def step(state, inp):
        r_t, w_t, k_t, v_t, a_t, bb_t = inp
        sa = jnp.einsum('bhvk,bhk->bhv', state, a_t)
        state = (state * w_t[:, :, None, :] + sa[..., None] * bb_t[:, :, None, :]
                 + v_t[..., None] * k_t[:, :, None, :])
        return state, jnp.einsum('bhvk,bhk->bhv', state, r_t)
    xs = tuple(jnp.moveaxis(t.astype(F32), 1, 0) for t in (r, w, k, v, a_vec, b_vec))
    _, ys = lax.scan(step, jnp.zeros((b_, h, n, n), F32), xs)
    return jnp.moveaxis(ys, 0, 1)


def rwkv7_mix(pa, mu, w0, w2, a0, a2, g2, k_k, k_a, r_k, lnx_w, lnx_b):
    b_, s, _ = pa.shape
    pa = (pa + (token_shift(pa) - pa) * mu).astype(F32)
    r, k, v, xw, xa, xg = jnp.split(
        pa, [A_WIDTH, 2 * A_WIDTH, 3 * A_WIDTH, 3 * A_WIDTH + A_LORA_W,
             3 * A_WIDTH + A_LORA_W + A_LORA_A], axis=-1)
    w_log = -jax.nn.softplus(-(w0 + jnp.tanh(xw) @ w2)) - 0.5
    decay = jnp.exp(-jnp.exp(w_log))
    a = jax.nn.sigmoid(a0 + xa @ a2)
    g = jax.nn.sigmoid(xg) @ g2
    heads = lambda t: t.reshape(b_, s, A_HEADS, HEAD_DIM)
    kk = heads(k * k_k)
    kk = kk / jnp.maximum(jnp.linalg.norm(kk, axis=-1, keepdims=True), 1e-12)
    k = k * (1.0 + (a - 1.0) * k_a)
    r, k, v, a, decay = heads(r), heads(k), heads(v), heads(a), heads(decay)
    y = rwkv7_scan(r, decay, k, v, -kk, kk * a)
    mu_y = jnp.mean(y, -1, keepdims=True)
    var_y = jnp.mean(jnp.square(y - mu_y), -1, keepdims=True)
    y = ((y - mu_y) * lax.rsqrt(var_y + A_GN_EPS)).reshape(b_, s, A_WIDTH) * lnx_w + lnx_b
    y = y + (jnp.sum(r * k * r_k, -1, keepdims=True) * v).reshape(b_, s, A_WIDTH)
    return y * g


def gla_chunk_scan(q, k, v, log_f, chunk):
    b_, s, h, dk = q.shape
    dv = v.shape[-1]
    causal = jnp.tril(jnp.ones((chunk, chunk), bool))[:, :, None]
    def step(state, inp):
        qc, kc, vc, gc = inp
        bcum = jnp.cumsum(gc, axis=2)
        o_inter = jnp.einsum('bhtk,bhkv->bhtv', qc * jnp.exp(bcum), state)
        rel = jnp.exp(jnp.where(causal, bcum[:, :, :, None, :] - bcum[:, :, None, :, :], -jnp.inf))
        attn = jnp.einsum('bhtsk,bhsk->bhts', qc[:, :, :, None, :] * rel, kc)
        out = o_inter + jnp.einsum('bhts,bhsv->bhtv', attn, vc)
        b_last = bcum[:, :, -1:, :]
        state = (jnp.exp(b_last[:, :, 0, :])[..., None] * state
                 + jnp.einsum('bhsk,bhsv->bhkv', kc * jnp.exp(b_last - bcum), vc))
        return state, out
    xs = (to_chunks(q, chunk), to_chunks(k, chunk), to_chunks(v, chunk), to_chunks(log_f, chunk))
    _, out = lax.scan(step, jnp.zeros((b_, h, dk, dv), F32), xs)
    return from_chunks(out)


def hgrn2_mix(pb, lb, norm_w):
    b_, s, _ = pb.shape
    pb = pb.astype(F32)
    q, f, i, g = jnp.split(pb, [B_HEADS * B_DK, 2 * B_HEADS * B_DK, 2 * B_HEADS * B_DK + B_WIDTH], -1)
    q = jax.nn.silu(q)
    log_f = jnp.logaddexp(jnp.log(lb), jnp.log1p(-lb) + jax.nn.log_sigmoid(f))
    k = (1.0 - lb) * jax.nn.sigmoid(-f)
    hk = lambda t: t.reshape(b_, s, B_HEADS, B_DK)
    o = gla_chunk_scan(hk(q), hk(k), i.reshape(b_, s, B_HEADS, B_DV), hk(log_f), B_CHUNK)
    o = o * lax.rsqrt(jnp.mean(o * o, -1, keepdims=True) + RMS_EPS) * norm_w
    return o.reshape(b_, s, B_WIDTH) * jax.nn.silu(g)


def dilated_attention(q, k, v):
    b_, s, h, e = q.shape
    scale = e ** -0.5
    o_list, m_list, l_list = [], [], []
    for window, dil in C_PAIRS:
        nk = window // dil
        length = s // dil
        nb = -(-length // nk)
        lp = nb * nk
        def split_stride(t):
            t = t.astype(F32).reshape(b_, length, dil, h, e).transpose(0, 2, 3, 1, 4)
            return jnp.pad(t, ((0, 0), (0, 0), (0, 0), (0, lp - length), (0, 0)))
        qs, ks, vs = split_stride(q), split_stride(k), split_stride(v)
        blocks = lambda t: t.reshape(b_, dil, h, nb, nk, e)
        def banded(t):
            prev = jnp.pad(t, ((0, 0), (0, 0), (0, 0), (nk, 0), (0, 0)))[:, :, :, :lp]
            return jnp.concatenate([blocks(prev), blocks(t)], axis=4)
        sc = jnp.einsum('bdhnqe,bdhnke->bdhnqk', blocks(qs), banded(ks)) * scale
        qi = jnp.arange(nk)[:, None]
        kj = jnp.arange(2 * nk)[None, :]
        dist = qi + nk - kj
        kpos = jnp.arange(nb)[:, None, None] * nk - nk + kj
        valid = (dist >= 0) & (dist <= nk) & (kpos >= 0)
        sc = jnp.where(valid, sc, -jnp.inf)
        m = jnp.max(sc, -1)
        p = jnp.exp(sc - m[..., None])
        o = jnp.einsum('bdhnqk,bdhnke->bdhnqe', p, banded(vs))
        def merge_stride(t):
            fdim = t.shape[-1]
            t = t.reshape(b_, dil, h, lp, fdim)[:, :, :, :length]
            return t.transpose(0, 3, 1, 2, 4).reshape(b_, s, h, fdim)
        o_list.append(merge_stride(o))
        m_list.append(merge_stride(m[..., None])[..., 0])
        l_list.append(merge_stride(jnp.sum(p, -1)[..., None])[..., 0])
    m_all = jnp.stack(m_list)
    l_all = jnp.stack(l_list)
    o_all = jnp.stack(o_list)
    wgt = jnp.exp(m_all - jnp.max(m_all, 0))
    return jnp.sum(wgt[..., None] * o_all, 0) / jnp.sum(wgt * l_all, 0)[..., None]


def mlstm_chunk_scan(q, k, v, log_i, log_f, chunk):
    b_, s, h, dk = q.shape
    dv = v.shape[-1]
    n = s // chunk
    gate_chunks = lambda t: t.reshape(b_, n, chunk, h).transpose(1, 0, 3, 2)
    causal = jnp.tril(jnp.ones((chunk, chunk), bool))
    def step(carry, inp):
        c_mat, n_vec, m = carry
        qc, kc, vc, ic, fc = inp
        fcum = jnp.cumsum(fc, -1)
        log_d = jnp.where(causal, fcum[..., :, None] - fcum[..., None, :] + ic[..., None, :], -jnp.inf)
        log_inter = fcum + m[..., None]
        m_t = jnp.maximum(jnp.max(log_d, -1), log_inter)
        w_inter = jnp.exp(log_inter - m_t)
        scores = jnp.einsum('bhtk,bhsk->bhts', qc, kc) * jnp.exp(log_d - m_t[..., None])
        num = (w_inter[..., None] * jnp.einsum('bhtk,bhkv->bhtv', qc, c_mat)
               + jnp.einsum('bhts,bhsv->bhtv', scores, vc))
        den = w_inter * jnp.einsum('bhtk,bhk->bht', qc, n_vec) + jnp.sum(scores, -1)
        out = num / jnp.maximum(jnp.abs(den), jnp.exp(-m_t))[..., None]
        f_tot = fcum[..., -1]
        log_w = f_tot[..., None] - fcum + ic
        m_new = jnp.maximum(f_tot + m, jnp.max(log_w, -1))
        w_s = jnp.exp(log_w - m_new[..., None])
        w_old = jnp.exp(f_tot + m - m_new)
        c_mat = w_old[..., None, None] * c_mat + jnp.einsum('bhs,bhsk,bhsv->bhkv', w_s, kc, vc)
        n_vec = w_old[..., None] * n_vec + jnp.einsum('bhs,bhsk->bhk', w_s, kc)
        return (c_mat, n_vec, m_new), out
    init = (jnp.zeros((b_, h, dk, dv), F32), jnp.zeros((b_, h, dk), F32), jnp.zeros((b_, h), F32))
    xs = (to_chunks(q, chunk), to_chunks(k, chunk), to_chunks(v, chunk),
          gate_chunks(log_i), gate_chunks(log_f))
    _, out = lax.scan(step, init, xs)
    return from_chunks(out)


def mlstm_mix(pd, conv_w, conv_b, b_i, b_f):
    b_, s, _ = pd.shape
    o1 = 2 * D_QK_WIDTH
    qk, v, o, gi, gf = jnp.split(pd, [o1, o1 + D_WIDTH, o1 + 2 * D_WIDTH, o1 + 2 * D_WIDTH + D_HEADS], -1)
    qk = jax.nn.silu(causal_depthwise_conv(qk, conv_w, conv_b)).astype(F32)
    q, k = jnp.split(qk, 2, -1)
    hq = lambda t: t.reshape(b_, s, D_HEADS, D_DQK)
    log_i = (gi + b_i).astype(F32)
    log_f = jax.nn.log_sigmoid((gf + b_f).astype(F32))
    h = mlstm_chunk_scan(hq(q), hq(k) * D_DQK ** -0.5, v.astype(F32).reshape(b_, s, D_HEADS, D_DV),
                         log_i, log_f, D_CHUNK)
    return jax.nn.sigmoid(o.astype(F32)) * h.reshape(b_, s, D_WIDTH)


def grouped_expert_ffn(xf, expert_id, token_id, weight, w1, w3, w2):
    t, d = xf.shape
    n = expert_id.shape[0]
    e = w1.shape[0]
    blk = MOE_BLOCK
    rows = -(-(n + e * (blk - 1)) // blk) * blk
    order = jnp.argsort(expert_id)
    e_sorted = expert_id[order]
    counts = jnp.bincount(expert_id, length=e)
    padded = -(-counts // blk) * blk
    start = jnp.cumsum(counts) - counts
    pstart = jnp.cumsum(padded) - padded
    dest = pstart[e_sorted] + jnp.arange(n) - start[e_sorted]
    row_tok = jnp.full((rows,), t, jnp.int32).at[dest].set(token_id[order].astype(jnp.int32))
    row_w = jnp.zeros((rows,), F32).at[dest].set(weight[order].astype(F32))
    n_blocks = rows // blk
    block_exp = jnp.clip(jnp.searchsorted(jnp.cumsum(padded), jnp.arange(n_blocks) * blk, side='right'), 0, e - 1)
    x_pad = jnp.concatenate([xf, jnp.zeros((1, d), xf.dtype)], 0)
    xs = x_pad[row_tok].reshape(n_blocks, blk, d)
    def run(args):
        xb, eb = args
        hb = jax.nn.silu(xb @ w1[eb]) * (xb @ w3[eb])
        return hb @ w2[eb]
    ys = lax.map(run, (xs, block_exp)).reshape(rows, d)
    out = jnp.zeros((t + 1, d), F32).at[row_tok].add(ys.astype(F32) * row_w[:, None])
    return out[:t]


def hier_moe(x, wg, bg, we, be, w1, w3, w2):
    b_, s, d = x.shape
    t = b_ * s
    xf = x.reshape(t, d)
    g_logits = (xf @ wg).astype(F32) + bg
    g_prob = jax.nn.softmax(g_logits, -1)
    g_top = jnp.argmax(g_logits, -1)
    p_group = jnp.take_along_axis(g_prob, g_top[:, None], -1)[:, 0]
    e_all = jnp.einsum('td,gde->tge', xf, we).astype(F32) + be
    e_logits = jnp.take_along_axis(e_all, g_top[:, None, None], axis=1)[:, 0]
    top_v, top_i = lax.top_k(e_logits, TOP_K)
    gate = p_group[:, None] * jax.nn.softmax(top_v, -1)
    expert = g_top[:, None] * EXPERTS_PER_GROUP + top_i
    token = jnp.repeat(jnp.arange(t), TOP_K)
    y = grouped_expert_ffn(xf, expert.reshape(-1), token, gate.reshape(-1), w1, w3, w2)
    return y.reshape(b_, s, d).astype(x.dtype)


def setup_inputs(seed: int = 0) -> dict:
    key = jax.random.key(seed)
    ks = iter(jax.random.split(key, 40))
    nrm = lambda shape, scale: jax.random.normal(next(ks), shape, F32) * scale
    uni = lambda shape, lo, hi: jax.random.uniform(next(ks), shape, F32, lo, hi)
    return {
        'x': nrm((BATCH, SEQ, D_MODEL), 1.0),
        'ev_w_in': nrm((N_EVEN, D_MODEL, EVEN_COLS), D_MODEL ** -0.5),
        'ev_w_out': nrm((N_EVEN, MIX_WIDTH, D_MODEL), MIX_WIDTH ** -0.5 * DN_BETA),
        'rwkv_mu': uni((N_EVEN, A_COLS), 0.0, 1.0),
        'rwkv_w0': uni((N_EVEN, A_WIDTH), -6.0, -1.0),
        'rwkv_w2': nrm((N_EVEN, A_LORA_W, A_WIDTH), 0.5 * A_LORA_W ** -0.5),
        'rwkv_a0': nrm((N_EVEN, A_WIDTH), 0.1),
        'rwkv_a2': nrm((N_EVEN, A_LORA_A, A_WIDTH), 0.5 * A_LORA_A ** -0.5),
        'rwkv_g2': nrm((N_EVEN, A_LORA_G, A_WIDTH), A_LORA_G ** -0.5),
        'rwkv_kk': 0.85 + nrm((N_EVEN, A_WIDTH), 0.05),
        'rwkv_ka': 1.0 + nrm((N_EVEN, A_WIDTH), 0.05),
        'rwkv_rk': nrm((N_EVEN, A_HEADS, HEAD_DIM), 0.1),
        'rwkv_lnx_w': 1.0 + nrm((N_EVEN, A_WIDTH), 0.02),
        'rwkv_lnx_b': nrm((N_EVEN, A_WIDTH), 0.02),
        'hgrn_lb': nrm((N_EVEN, B_HEADS * B_DK), 0.5),
        'hgrn_norm_w': 1.0 + nrm((N_EVEN, B_DV), 0.02),
        'od_w_in': nrm((N_ODD, D_MODEL, ODD_COLS), D_MODEL ** -0.5),
        'od_w_out': nrm((N_ODD, MIX_WIDTH, D_MODEL), MIX_WIDTH ** -0.5 * DN_BETA),
        'mlstm_conv_w': nrm((N_ODD, D_CONV, 2 * D_QK_WIDTH), D_CONV ** -0.5),
        'mlstm_conv_b': nrm((N_ODD, 2 * D_QK_WIDTH), 0.02),
        'mlstm_b_i': nrm((N_ODD, D_HEADS), 0.1),
        'mlstm_b_f': jnp.linspace(3.0, 6.0, D_HEADS, dtype=F32) + nrm((N_ODD, D_HEADS), 0.1),
        'ln_w': 1.0 + nrm((DEPTH, 2, D_MODEL), 0.02),
        'ln_b': nrm((DEPTH, 2, D_MODEL), 0.02),
        'moe_wg': nrm((DEPTH, D_MODEL, N_GROUPS), D_MODEL ** -0.5),
        'moe_bg': nrm((DEPTH, N_GROUPS), 0.01),
        'moe_we': nrm((DEPTH, N_GROUPS, D_MODEL, EXPERTS_PER_GROUP), D_MODEL ** -0.5),
        'moe_be': nrm((DEPTH, N_GROUPS, EXPERTS_PER_GROUP), 0.01),
        'moe_w1': nrm((DEPTH, N_EXPERTS, D_MODEL, D_EXPERT), D_MODEL ** -0.5),
        'moe_w3': nrm((DEPTH, N_EXPERTS, D_MODEL, D_EXPERT), D_MODEL ** -0.5),
        'moe_w2': nrm((DEPTH, N_EXPERTS, D_EXPERT, D_MODEL), D_EXPERT ** -0.5 * DN_BETA),
    }


def reference(x, ev_w_in, ev_w_out, rwkv_mu, rwkv_w0, rwkv_w2, rwkv_a0, rwkv_a2, rwkv_g2,
              rwkv_kk, rwkv_ka, rwkv_rk, rwkv_lnx_w, rwkv_lnx_b, hgrn_lb, hgrn_norm_w,
              od_w_in, od_w_out, mlstm_conv_w, mlstm_conv_b, mlstm_b_i, mlstm_b_f,
              ln_w, ln_b, moe_wg, moe_bg, moe_we, moe_be, moe_w1, moe_w3, moe_w2):
    lb_all = jnp.cumsum(jax.nn.softmax(hgrn_lb.astype(F32), axis=0), axis=0)
    lb_all = lb_all - lb_all[:1]
    for layer in range(DEPTH):
        j = layer // 2
        if layer % 2 == 0:
            p = x @ ev_w_in[j]
            ya = rwkv7_mix(p[..., :A_COLS], rwkv_mu[j], rwkv_w0[j], rwkv_w2[j], rwkv_a0[j],
                           rwkv_a2[j], rwkv_g2[j], rwkv_kk[j], rwkv_ka[j], rwkv_rk[j],
                           rwkv_lnx_w[j], rwkv_lnx_b[j])
            yb = hgrn2_mix(p[..., A_COLS:], lb_all[j], hgrn_norm_w[j])
            mix = jnp.concatenate([ya, yb], -1).astype(x.dtype) @ ev_w_out[j]
        else:
            b_, s, _ = x.shape
            p = x @ od_w_in[j]
            qc, kc, vc, pd = jnp.split(p, [C_WIDTH, 2 * C_WIDTH, 3 * C_WIDTH], -1)
            hc = lambda t: t.reshape(b_, s, C_HEADS, HEAD_DIM)
            yc = dilated_attention(hc(qc), hc(kc), hc(vc)).reshape(b_, s, C_WIDTH)
            yd = mlstm_mix(pd, mlstm_conv_w[j], mlstm_conv_b[j], mlstm_b_i[j], mlstm_b_f[j])
            mix = jnp.concatenate([yc, yd], -1).astype(x.dtype) @ od_w_out[j]
        x = layer_norm(DN_ALPHA * x + mix, ln_w[layer, 0], ln_b[layer, 0]).astype(x.dtype)
        ffn = hier_moe(x, moe_wg[layer], moe_bg[layer], moe_we[layer], moe_be[layer],
                       moe_w1[layer], moe_w3[layer], moe_w2[layer])
        x = layer_norm(DN_ALPHA * x + ffn, ln_w[layer, 1], ln_b[layer, 1]).astype(x.dtype)
    return x
```

```python
import numpy as np
from contextlib import ExitStack
import concourse.bass as bass
import concourse.mybir as mybir
from concourse.bass_utils import run_bass_kernel_spmd

F32 = mybir.dt.float32
BF16 = mybir.dt.bfloat16
AF = mybir.ActivationFunctionType
ALU = mybir.AluOpType
AX = mybir.AxisListType

D = 2048
KC = 16
BLK = 512
DEPTH = 4
DN_ALPHA = (2 * DEPTH) ** 0.25
LN_EPS = 1e-5
ENGS = ("pe", "act", "dve", "pool", "sp")


class Sched:
    def __init__(self, nc, stack):
        self.nc = nc
        self.stack = stack
        self.q = {e: [] for e in ENGS}
        self.cnt = {}
        self.sems = {}
        self.seen = {e: {} for e in ENGS}
        self.lastw = {}
        self.readers = {}
        for e in ("pe", "act", "dve", "pool"):
            self.sem(e)
        self.nops = 0
        self.slotmap = {}
        self.free = []
        self.ndma = 0

    def slot_sem(self, slot):
        if slot not in self.slotmap:
            if self.free:
                sid = self.free.pop()
            else:
                sid = f"dma{self.ndma}"
                self.ndma += 1
                self.sem(sid)
            self.slotmap[slot] = sid
        return self.slotmap[slot]

    def release_slots(self):
        for sid in self.slotmap.values():
            if sid not in self.free:
                self.free.append(sid)
        self.slotmap = {}

    def sem(self, name):
        if name not in self.sems:
            self.sems[name] = self.stack.enter_context(self.nc.semaphore("s_" + str(name)))
            self.cnt[name] = 0
        return self.sems[name]

    def _deps(self, eng, reads, writes):
        need = {}

        def add(tok):
            s, v = tok
            if eng == "pe" and s == "pe":
                return
            if need.get(s, 0) < v:
                need[s] = v
        for k in reads:
            if k in self.lastw:
                add(self.lastw[k])
        for k in writes:
            if k in self.lastw:
                add(self.lastw[k])
            for t in self.readers.get(k, ()):
                add(t)
        waits = []
        for s, v in need.items():
            if self.seen[eng].get(s, 0) < v:
                self.seen[eng][s] = v
                waits.append((s, v))
        return waits

    def _commit(self, tok, reads, writes):
        for k in writes:
            self.lastw[k] = tok
            self.readers[k] = []
        for k in reads:
            if k in writes:
                continue
            lst = self.readers.setdefault(k, [])
            lst.append(tok)
            if len(lst) > 48:
                m = {}
                for s, v in lst:
                    m[s] = max(m.get(s, 0), v)
                self.readers[k] = list(m.items())

    def op(self, eng, fn, reads=(), writes=()):
        waits = self._deps(eng, reads, writes)
        self.cnt[eng] += 1
        tok = (eng, self.cnt[eng])
        self.q[eng].append((waits, fn, (eng, 1)))
        self._commit(tok, reads, writes)
        self.nops += 1
        return tok

    def dma(self, eng, slot, fn, reads=(), writes=()):
        slot = self.slot_sem(slot)
        waits = self._deps(eng, reads, writes)
        self.cnt[slot] += 16
        tok = (slot, self.cnt[slot])
        self.q[eng].append((waits, fn, (slot, 16)))
        self._commit(tok, reads, writes)
        self.nops += 1
        return tok

    def barrier(self, engs=ENGS):
        for e in engs:
            waits = []
            for s, v in self.cnt.items():
                if v > 0 and self.seen[e].get(s, 0) < v:
                    self.seen[e][s] = v
                    waits.append((s, v))
            if waits:
                self.q[e].append((waits, None, None))

    def emit(self, block):
        nc = self.nc
        names = {"pe": "tensor", "act": "scalar", "dve": "vector", "pool": "gpsimd", "sp": "sync"}
        for e in ENGS:
            items = self.q[e]

            def body(engine, items=items):
                for waits, fn, inc in items:
                    for s, v in waits:
                        engine.wait_ge(self.sems[s], v)
                    if fn is not None:
                        fn(engine).then_inc(self.sems[inc[0]], inc[1])
            getattr(block, names[e])(body)


class K:
    def __init__(self, S, depth=DEPTH, n_groups=4, dbg=None):
        self.S = S
        self.NB = S // BLK
        self.depth = depth
        self.NG = n_groups
        self.NE = n_groups * 8
        self.dbg = dbg or ()
        self.nc = bass.Bass("TRN2", target_bir_lowering=False)
        self.st = ExitStack()
        self.sch = Sched(self.nc, self.st)
        self.rr = 0

    def dram(self, name, shape, dt, kind="Internal"):
        return self.nc.dram_tensor(name, list(shape), dt, kind=kind).ap()

    def phase(self):
        self.sch.barrier()
        return _Phase(self)

    def ld(self, out, in_, reads=(), writes=(), eng=None, slot=None):
        eng = eng or "sp"
        slot = slot or ("d_" + str(writes[0] if writes else reads[0]))
        return self.sch.dma(eng, slot, lambda e: e.dma_start(out=out, in_=in_), reads=reads, writes=writes)

    def mm(self, out, lhsT, rhs, start, stop, reads, writes):
        return self.sch.op("pe", lambda e: e.matmul(out, lhsT=lhsT, rhs=rhs, start=start, stop=stop),
                           reads=reads, writes=writes)

    def tr(self, out, in_, ident, reads, writes):
        return self.sch.op("pe", lambda e: e.transpose(out=out, in_=in_, identity=ident), reads=reads, writes=writes)

    def act(self, out, in_, func, reads, writes, scale=None, bias=None, accum_out=None):
        kw = {}
        if scale is not None:
            kw["scale"] = scale
        if bias is not None:
            kw["bias"] = bias
        if accum_out is not None:
            kw["accum_out"] = accum_out
        return self.sch.op("act", lambda e: e.activation(out=out, in_=in_, func=func, **kw), reads=reads, writes=writes)

    def v(self, eng, name, reads, writes, **kw):
        return self.sch.op(eng, lambda e: getattr(e, name)(**kw), reads=reads, writes=writes)

    def evac_engine(self):
        self.rr += 1
        return "dve" if self.rr % 2 else "act"

    def copy(self, eng, out, in_, reads, writes):
        if eng == "act":
            return self.sch.op("act", lambda e: e.copy(out=out, in_=in_), reads=reads, writes=writes)
        return self.sch.op(eng, lambda e: e.tensor_copy(out=out, in_=in_), reads=reads, writes=writes)


class _Phase:
    def __init__(self, k):
        self.k = k

    def __enter__(self):
        self.stack = ExitStack()
        self.stack.__enter__()
        k = self.k
        nc = k.nc
        k.nphase = getattr(k, "nphase", 0) + 1
        pf = f"p{k.nphase}_"
        self.sb = lambda name, shape, dt=F32: self.stack.enter_context(nc.sbuf_tensor(pf + name, list(shape), dt))
        self.ps = lambda name, shape, dt=F32: self.stack.enter_context(nc.psum_tensor(pf + name, list(shape), dt))
        return self

    def __exit__(self, *a):
        self.k.sch.barrier()
        self.k.sch.release_slots()
        self.k.sch.lastw = {kk: vv for kk, vv in self.k.sch.lastw.items() if isinstance(kk, tuple) and kk and kk[0] == "D"}
        self.k.sch.readers = {kk: vv for kk, vv in self.k.sch.readers.items() if isinstance(kk, tuple) and kk and kk[0] == "D"}
        return self.stack.__exit__(*a)


def _declare(self):
    k = self
    S, NG, NE = k.S, k.NG, k.NE
    I = {}
    ext = lambda n, sh: k.dram(n, sh, F32, kind="ExternalInput")
    I["x"] = ext("x", [S, D])
    I["ev_w_in"] = ext("ev_w_in", [2, D, 7424])
    I["ev_w_out"] = ext("ev_w_out", [2, D, D])
    I["rwkv_mu"] = ext("rwkv_mu", [2, 3328])
    I["rwkv_w0"] = ext("rwkv_w0", [2, 1024])
    I["rwkv_w2"] = ext("rwkv_w2", [2, 64, 1024])
    I["rwkv_a0"] = ext("rwkv_a0", [2, 1024])
    I["rwkv_a2"] = ext("rwkv_a2", [2, 64, 1024])
    I["rwkv_g2"] = ext("rwkv_g2", [2, 128, 1024])
    I["rwkv_kk"] = ext("rwkv_kk", [2, 1024])
    I["rwkv_ka"] = ext("rwkv_ka", [2, 1024])
    I["rwkv_rk"] = ext("rwkv_rk", [2, 1024])
    I["rwkv_lnx_w"] = ext("rwkv_lnx_w", [2, 1024])
    I["rwkv_lnx_b"] = ext("rwkv_lnx_b", [2, 1024])
    I["hgrn_lb"] = ext("hgrn_lb", [2, 1024])
    I["hgrn_norm_w"] = ext("hgrn_norm_w", [2, 128])
    I["od_w_in"] = ext("od_w_in", [2, D, 6152])
    I["od_w_out"] = ext("od_w_out", [2, D, D])
    I["mlstm_conv_w"] = ext("mlstm_conv_w", [2, 4, 1024])
    I["mlstm_conv_b"] = ext("mlstm_conv_b", [2, 1024])
    I["mlstm_b_i"] = ext("mlstm_b_i", [2, 4])
    I["mlstm_b_f"] = ext("mlstm_b_f", [2, 4])
    I["ln_w"] = ext("ln_w", [DEPTH, 2, D])
    I["ln_b"] = ext("ln_b", [DEPTH, 2, D])
    I["moe_wg"] = ext("moe_wg", [DEPTH, D, NG])
    I["moe_bg"] = ext("moe_bg", [DEPTH, NG])
    I["moe_we"] = ext("moe_we", [DEPTH, NG, D, 8])
    I["moe_be"] = ext("moe_be", [DEPTH, NG * 8])
    I["moe_w1"] = ext("moe_w1", [DEPTH, NE, D, 512])
    I["moe_w3"] = ext("moe_w3", [DEPTH, NE, D, 512])
    I["moe_w2"] = ext("moe_w2", [DEPTH, NE, 512, D])
    k.I = I
    k.out = k.dram("out", [S, D], F32, kind="ExternalOutput")
    W = {}
    for L in k.layers:
        j = L // 2
        if L % 2 == 0:
            W["in", L] = k.dram(f"wbin{L}", [128, KC, 7424], BF16)
        else:
            W["in", L] = k.dram(f"wbin{L}", [128, KC, 6152], BF16)
        W["out", L] = k.dram(f"wbout{L}", [128, KC, D], BF16)
        W["w1", L] = k.dram(f"w1b{L}", [NE, 128, KC, 512], BF16)
        W["w3", L] = k.dram(f"w3b{L}", [NE, 128, KC, 512], BF16)
        W["w2", L] = k.dram(f"w2b{L}", [NE, 128, 4, D], BF16)
        W["wr", L] = k.dram(f"wrb{L}", [128, KC, NG + NE], BF16)
    k.W = W
    NB = k.NB
    A = {}
    A["xa"] = k.dram("xres_a", [S, D], F32)
    A["xb"] = k.dram("xres_b", [S, D], F32)
    A["xT"] = k.dram("xT", [NB, 128, KC, BLK], BF16)
    A["x1"] = k.dram("x1", [S, D], F32)
    A["x1T"] = k.dram("x1T", [NB, 128, KC, BLK], BF16)
    A["mixT"] = k.dram("mixT", [NB, 128, KC, BLK], BF16)
    A["gates"] = k.dram("gates", [S, NE], F32)
    k.A = A


def _precast(self):
    k = self
    sch = k.sch

    def cast(out, in_, key, slow=False):
        sch.dma("pool", "cast", lambda e: e.dma_start(out=out, in_=in_, allow_slow_non_contiguous=slow), writes=[key])
    for L in k.layers:
        j = L // 2
        win = k.I["ev_w_in" if L % 2 == 0 else "od_w_in"][j].rearrange("(kc p) c -> p kc c", p=128)
        C = 7424 if L % 2 == 0 else 6152
        c0 = 0
        while c0 < C:
            c1 = min(C, c0 + 1024)
            cast(k.W["in", L][:, :, c0:c1], win[:, :, c0:c1], ("D", "win", L, c0))
            c0 = c1
        k.win_keys = getattr(k, "win_keys", {})
        k.win_keys[L] = [("D", "win", L, c) for c in range(0, C, 1024)]
        wout = k.I["ev_w_out" if L % 2 == 0 else "od_w_out"][j].rearrange("(kc p) c -> p kc c", p=128)
        for q in range(2):
            cast(k.W["out", L][:, :, q * 1024:(q + 1) * 1024], wout[:, :, q * 1024:(q + 1) * 1024], ("D", "wout", L, q))
        NG, NE = k.NG, k.NE
        cast(k.W["wr", L][:, :, 0:NG], k.I["moe_wg"][L].rearrange("(kc p) c -> p kc c", p=128), ("D", "wr", L, 0), True)
        for g in range(NG):
            cast(k.W["wr", L][:, :, NG + 8 * g:NG + 8 * g + 8],
                 k.I["moe_we"][L, g].rearrange("(kc p) c -> p kc c", p=128), ("D", "wr", L, 1 + g), True)
        for e in range(NE):
            cast(k.W["w1", L][e], k.I["moe_w1"][L, e].rearrange("(kc p) c -> p kc c", p=128), ("D", "w1", L, e))
            cast(k.W["w3", L][e], k.I["moe_w3"][L, e].rearrange("(kc p) c -> p kc c", p=128), ("D", "w3", L, e))
            cast(k.W["w2", L][e], k.I["moe_w2"][L, e].rearrange("(kc p) c -> p kc c", p=128), ("D", "w2", L, e))


def _consts(self):
    k = self
    nc = k.nc
    sb = lambda name, shape, dt=F32: k.st.enter_context(nc.sbuf_tensor(name, list(shape), dt))
    k.c_onesf = sb("c_onesf", [128, 128])
    k.c_ident = sb("c_ident", [128, 128], BF16)
    k.c_identf = sb("c_identf", [128, 128])
    k.v("pool", "memset", [], ["c_onesf"], ap=k.c_onesf[:], constant=1.0)
    k.v("pool", "memset", [], ["c_identf"], ap=k.c_identf[:], constant=1.0)
    k.v("pool", "affine_select", ["c_identf"], ["c_identf"], out=k.c_identf[:], in_=k.c_identf[:], pattern=[[-1, 128]],
        compare_op=ALU.is_equal, fill=0.0, base=0, channel_multiplier=1)
    k.copy("dve", k.c_ident[:], k.c_identf[:], ["c_identf"], ["c_ident"])
    k.c_eps = sb("c_eps", [128, 4])
    for i, val in enumerate((LN_EPS, 1.0, 64e-5, 1e-6)):
        k.v("pool", "memset", [], ["c_eps"], ap=k.c_eps[:, i:i + 1], constant=val)


def _x0(self):
    k = self
    with k.phase() as P:
        xb = [P.sb(f"x0_xb{i}", [128, D], BF16) for i in range(2)]
        blk = [P.sb(f"x0_blk{i}", [128, KC, BLK], BF16) for i in range(2)]
        pt = [P.ps(f"x0_pt{i}", [128, KC, 128], BF16) for i in range(2)]
        n = 0
        for b in range(k.NB):
            bb = blk[b % 2]
            for sub in range(4):
                t0 = b * BLK + sub * 128
                xt = xb[n % 2]
                p = pt[n % 2]
                k.sch.dma("pool", ("x0ld", n % 2), lambda e, xt=xt, t0=t0: e.dma_start(out=xt[:], in_=k.I["x"][t0:t0 + 128, :]),
                          writes=[("x0_xb", n % 2)])
                for kc in range(KC):
                    k.tr(p[:, kc, :], xt[:, kc * 128:(kc + 1) * 128], k.c_ident[:], [("x0_xb", n % 2), "c_ident"], [("x0_pt", n % 2)])
                k.copy(k.evac_engine(), bb[:, :, sub * 128:(sub + 1) * 128], p[:], [("x0_pt", n % 2)], [("x0_blk", b % 2)])
                n += 1
            k.ld(k.A["xT"][b], bb[:], reads=[("x0_blk", b % 2)], writes=[("D", "xT", b)], slot=("x0st", b % 2))


K.declare = _declare
K.precast = _precast
K.consts = _consts
K.x0 = _x0


def _ln_tail(k, P, T, z, zk, lnw, lnb, out_tm, t0, blkbuf, blkkey, sub, n):
    st = T["stats"][n % 2]
    mv = T["mv"][n % 2]
    sk, mk = ("ln_st", n % 2), ("ln_mv", n % 2)
    for c in range(4):
        k.v("dve", "bn_stats", [zk], [sk], out=st[:, c, :], in_=z[:, c * 512:(c + 1) * 512])
    k.v("dve", "bn_aggr", [sk], [mk], out=mv[:, 0:2], in_=st[:].rearrange("p a b -> p (a b)"))
    k.act(mv[:, 3:4], mv[:, 1:2], AF.Sqrt, [mk], [mk], bias=k.c_eps[:, 0:1])
    k.v("dve", "reciprocal", [mk], [mk], out=mv[:, 2:3], in_=mv[:, 3:4])
    k.v("dve", "tensor_scalar", [zk, mk], [zk], out=z[:], in0=z[:], scalar1=mv[:, 0:1], scalar2=mv[:, 2:3],
        op0=ALU.subtract, op1=ALU.mult)
    k.v("pool", "tensor_tensor", [zk, "lnw"], [zk], out=z[:], in0=z[:], in1=lnw[:], op=ALU.mult)
    k.v("pool", "tensor_tensor", [zk, "lnb"], [zk], out=z[:], in0=z[:], in1=lnb[:], op=ALU.add)
    k.ld(out_tm[t0:t0 + 128, :], z[:], reads=[zk], writes=[("D", "tm", id(out_tm), t0)], slot=("lnst", n % 2))
    zb = T["zb"][n % 2]
    zbk = ("ln_zb", n % 2)
    k.act(zb[:], z[:], AF.Copy, [zk], [zbk])
    pt = T["pt"][n % len(T["pt"])]
    ptk = ("ln_pt", n % len(T["pt"]))
    for kc in range(KC):
        k.tr(pt[:, kc, :], zb[:, kc * 128:(kc + 1) * 128], k.c_ident[:], [zbk], [ptk])
    k.copy(k.evac_engine(), blkbuf[:, :, sub * 128:(sub + 1) * 128], pt[:], [ptk], [blkkey])


def _ln_alloc(P, npt=2):
    T = {}
    T["stats"] = [P.sb(f"ln_stats{i}", [128, 4, 6]) for i in range(2)]
    T["mv"] = [P.sb(f"ln_mv{i}", [128, 4]) for i in range(2)]
    T["zb"] = [P.sb(f"ln_zb{i}", [128, D], BF16) for i in range(2)]
    T["pt"] = [P.ps(f"ln_pt{i}", [128, KC, 128], BF16) for i in range(npt)]
    return T


def _phase_o(self, L, xin):
    k = self
    NG, NE = k.NG, k.NE
    NR = NG + NE
    with k.phase() as P:
        T = _ln_alloc(P)
        wout = P.sb("o_wout", [128, KC, D], BF16)
        wr = P.sb("o_wr", [128, KC, NR], BF16)
        lnw = P.sb("o_lnw", [128, D]); lnb = P.sb("o_lnb", [128, D])
        rb = P.sb("o_rb", [128, NR])
        mix = [P.sb(f"o_mix{i}", [128, KC, BLK], BF16) for i in range(2)]
        blk = [P.sb(f"o_blk{i}", [128, KC, BLK], BF16) for i in range(2)]
        xr = [P.sb(f"o_xr{i}", [128, D]) for i in range(2)]
        z = [P.sb(f"o_z{i}", [128, D]) for i in range(2)]
        rt = [P.sb(f"o_rt{i}", [128, 64 + 3 * NE]) for i in range(2)]
        po = [P.ps(f"o_po{i}", [128, 512]) for i in range(2)]
        pr = P.ps("o_pr", [128, 512])
        k.ld(wout[:, :, 0:1024], k.W["out", L][:, :, 0:1024], reads=[("D", "wout", L, 0)], writes=["wout0"])
        k.ld(wout[:, :, 1024:2048], k.W["out", L][:, :, 1024:2048], reads=[("D", "wout", L, 1)], writes=["wout1"])
        k.ld(wr[:], k.W["wr", L], reads=[("D", "wr", L, g) for g in range(NG + 1)], writes=["wr"])
        k.ld(lnw[:], k.I["ln_w"][L, 0:1, :].partition_broadcast(128), writes=["lnw"])
        k.ld(lnb[:], k.I["ln_b"][L, 0:1, :].partition_broadcast(128), writes=["lnb"])
        k.ld(rb[:, 0:NG], k.I["moe_bg"][L:L + 1, :].partition_broadcast(128), writes=["rb0"])
        k.ld(rb[:, NG:NR], k.I["moe_be"][L:L + 1, :].partition_broadcast(128), writes=["rb1"])
        n = 0
        npo = 0
        for b in range(k.NB):
            mb = mix[b % 2]
            k.ld(mb[:], k.A["mixT"][b], reads=[("D", "mixT", b, 0), ("D", "mixT", b, 1)], writes=[("o_mix", b % 2)], slot=("omix", b % 2))
            bb = blk[b % 2]
            bk = ("o_blk", b % 2)
            for sub in range(4):
                t0 = b * BLK + sub * 128
                x_t = xr[n % 2]; zt = z[n % 2]
                xk, zk = ("o_xr", n % 2), ("o_z", n % 2)
                k.ld(x_t[:], xin[t0:t0 + 128, :], reads=[("D", "tm", id(xin), t0)], writes=[xk], slot=("oxr", n % 2))
                for dc in range(4):
                    pp = po[npo % 2]; pk = ("o_po", npo % 2)
                    for kc in range(KC):
                        k.mm(pp[:], mb[:, kc, sub * 128:(sub + 1) * 128], wout[:, kc, dc * 512:(dc + 1) * 512],
                             kc == 0, kc == KC - 1, [("o_mix", b % 2), "wout0", "wout1"], [pk])
                    k.v("dve", "scalar_tensor_tensor", [xk, pk], [zk], out=zt[:, dc * 512:(dc + 1) * 512],
                        in0=x_t[:, dc * 512:(dc + 1) * 512], scalar=DN_ALPHA, in1=pp[:], op0=ALU.mult, op1=ALU.add)
                    npo += 1
                _ln_tail(k, P, T, zt, zk, lnw, lnb, k.A["x1"], t0, bb, bk, sub, n)
                r = rt[n % 2]; rk = ("o_rt", n % 2)
                for kc in range(KC):
                    k.mm(pr[:, 0:NR], bb[:, kc, sub * 128:(sub + 1) * 128], wr[:, kc, :], kc == 0, kc == KC - 1,
                         [bk, "wr"], ["o_pr"])
                lg = r[:, 0:NR]
                k.v("dve", "tensor_tensor", ["o_pr", "rb0", "rb1"], [rk], out=lg, in0=pr[:, 0:NR], in1=rb[:], op=ALU.add)
                sc = r[:, NR:NR + 16]
                gmax, gsum, pg, v1, v2, dd, c1, c2 = [sc[:, i:i + 1] for i in range(8)]
                gexp = r[:, NR + 16:NR + 16 + NG]
                gmask = r[:, NR + 20:NR + 20 + NG]
                o0 = 64
                ml = r[:, o0:o0 + NE]
                top = r[:, o0 + NE:o0 + NE + 8]
                gt = r[:, o0 + 2 * NE:o0 + 3 * NE]
                if NG > 1:
                    k.v("dve", "tensor_reduce", [rk], [rk], out=gmax, in_=r[:, 0:NG], axis=AX.X, op=ALU.max)
                else:
                    k.v("dve", "tensor_copy", [rk], [rk], out=gmax, in_=r[:, 0:1])
                k.v("dve", "tensor_scalar", [rk], [rk], out=gexp, in0=r[:, 0:NG], scalar1=gmax, scalar2=None, op0=ALU.subtract)
                k.act(gexp, gexp, AF.Exp, [rk], [rk], accum_out=gsum)
                k.v("dve", "reciprocal", [rk], [rk], out=pg, in_=gsum)
                k.v("dve", "tensor_scalar", [rk], [rk], out=gmask, in0=r[:, 0:NG], scalar1=gmax, scalar2=-1.0,
                    op0=ALU.is_equal, op1=ALU.add)
                k.v("dve", "scalar_tensor_tensor", [rk], [rk], out=ml.rearrange("p (g e) -> p g e", e=8),
                    in0=gmask.rearrange("p (g o) -> p g o", o=1).to_broadcast([128, NG, 8]), scalar=1e30,
                    in1=r[:, NG:NR].rearrange("p (g e) -> p g e", e=8), op0=ALU.mult, op1=ALU.add)
                k.v("dve", "max", [rk], [rk], out=top, in_=ml)
                k.v("dve", "tensor_tensor", [rk], [rk], out=dd, in0=top[:, 1:2], in1=top[:, 0:1], op=ALU.subtract)
                k.act(dd, dd, AF.Exp, [rk], [rk])
                k.v("dve", "tensor_scalar", [rk], [rk], out=dd, in0=dd, scalar1=1.0, scalar2=None, op0=ALU.add)
                k.v("dve", "reciprocal", [rk], [rk], out=dd, in_=dd)
                k.v("dve", "tensor_tensor", [rk], [rk], out=c1, in0=dd, in1=pg, op=ALU.mult)
                k.v("dve", "tensor_tensor", [rk], [rk], out=c2, in0=pg, in1=c1, op=ALU.subtract)
                k.v("dve", "tensor_scalar", [rk], [rk], out=gt, in0=ml, scalar1=top[:, 0:1], scalar2=c1,
                    op0=ALU.is_equal, op1=ALU.mult)
                k.v("dve", "tensor_scalar", [rk], [rk], out=ml, in0=ml, scalar1=top[:, 1:2], scalar2=c2,
                    op0=ALU.is_equal, op1=ALU.mult)
                k.v("dve", "tensor_tensor", [rk], [rk], out=gt, in0=gt, in1=ml, op=ALU.add)
                k.ld(k.A["gates"][t0:t0 + 128, :], gt, reads=[rk], writes=[("D", "gates", t0)], slot=("ogt", n % 2))
                n += 1
            k.ld(k.A["x1T"][b], bb[:], reads=[bk], writes=[("D", "x1T", b)], slot=("ox1T", b % 2))


def _phase_m(self, L, xout):
    k = self
    NE = k.NE
    with k.phase() as P:
        T = _ln_alloc(P, 1)
        lnw = P.sb("m_lnw", [128, D]); lnb = P.sb("m_lnb", [128, D])
        xb = [P.sb(f"m_xb{i}", [128, KC, BLK], BF16) for i in range(1)]
        blk = [P.sb(f"m_blk{i}", [128, KC, BLK], BF16) for i in range(1)]
        gt = [P.sb(f"m_gt{i}", [128, 4, NE]) for i in range(2)]
        yacc = P.sb("m_yacc", [128, 4, D])
        w1 = [P.sb(f"m_w1_{i}", [128, KC, 512], BF16) for i in range(2)]
        w3 = [P.sb(f"m_w3_{i}", [128, KC, 512], BF16) for i in range(2)]
        w2 = [P.sb(f"m_w2_{i}", [128, 4, D], BF16) for i in range(2)]
        h = [P.sb(f"m_h{i}", [128, 4, BLK], BF16) for i in range(2)]
        sl = [P.sb(f"m_sl{i}", [128, BLK]) for i in range(2)]
        xr = [P.sb(f"m_xr{i}", [128, D]) for i in range(1)]
        p1 = [P.ps(f"m_p1_{i}", [128, 512]) for i in range(2)]
        p3 = [P.ps(f"m_p3_{i}", [128, 512]) for i in range(2)]
        po = [P.ps(f"m_po{i}", [128, 512]) for i in range(2)]
        k.ld(lnw[:], k.I["ln_w"][L, 1:2, :].partition_broadcast(128), writes=["lnw"])
        k.ld(lnb[:], k.I["ln_b"][L, 1:2, :].partition_broadcast(128), writes=["lnb"])
        n = 0; ne = 0; nf = 0; npo = 0
        for b in range(k.NB):
            x_b = xb[0]; xbk = ("m_xb", 0)
            k.ld(x_b[:], k.A["x1T"][b], reads=[("D", "x1T", b)], writes=[xbk], slot=("mxb", 0))
            g = gt[b % 2]; gk = ("m_gt", b % 2)
            k.ld(g[:], k.A["gates"][b * BLK:(b + 1) * BLK, :].rearrange("(s p) e -> p s e", p=128),
                 reads=[("D", "gates", b * BLK + s * 128) for s in range(4)], writes=[gk], slot=("mgt", b % 2))
            for e in range(NE):
                i = ne % 2
                k.ld(w1[i][:], k.W["w1", L][e], reads=[("D", "w1", L, e)], writes=[("m_w1", i)], slot=("mw1", i))
                k.ld(w3[i][:], k.W["w3", L][e], reads=[("D", "w3", L, e)], writes=[("m_w3", i)], slot=("mw3", i))
                k.ld(w2[i][:], k.W["w2", L][e], reads=[("D", "w2", L, e)], writes=[("m_w2", i)], slot=("mw2", i))
                hh = h[i]; hk = ("m_h", i)
                for fc in range(4):
                    a1 = p1[nf % 2]; a3 = p3[nf % 2]
                    k1, k3 = ("m_p1", nf % 2), ("m_p3", nf % 2)
                    for kc in range(KC):
                        k.mm(a1[:], w1[i][:, kc, fc * 128:(fc + 1) * 128], x_b[:, kc, :], kc == 0, kc == KC - 1,
                             [("m_w1", i), xbk], [k1])
                    for kc in range(KC):
                        k.mm(a3[:], w3[i][:, kc, fc * 128:(fc + 1) * 128], x_b[:, kc, :], kc == 0, kc == KC - 1,
                             [("m_w3", i), xbk], [k3])
                    s_ = sl[nf % 2]; sk = ("m_sl", nf % 2)
                    k.act(s_[:], a1[:], AF.Silu, [k1], [sk])
                    k.v("dve", "tensor_tensor", [sk, k3], [hk], out=hh[:, fc, :], in0=s_[:], in1=a3[:], op=ALU.mult)
                    nf += 1
                for sub in range(4):
                    for dc in range(4):
                        pp = po[npo % 2]; pk = ("m_po", npo % 2)
                        for fc in range(4):
                            k.mm(pp[:], hh[:, fc, sub * 128:(sub + 1) * 128], w2[i][:, fc, dc * 512:(dc + 1) * 512],
                                 fc == 0, fc == 3, [hk, ("m_w2", i)], [pk])
                        ya = yacc[:, sub, dc * 512:(dc + 1) * 512]
                        yk = ("m_yacc", sub, dc)
                        if e == 0:
                            k.v("dve", "tensor_scalar", [pk, gk], [yk], out=ya, in0=pp[:], scalar1=g[:, sub, e:e + 1],
                                scalar2=None, op0=ALU.mult)
                        else:
                            k.v("dve", "scalar_tensor_tensor", [pk, gk, yk], [yk], out=ya, in0=pp[:],
                                scalar=g[:, sub, e:e + 1], in1=ya, op0=ALU.mult, op1=ALU.add)
                        npo += 1
                ne += 1
            bb = blk[0]; bk = ("m_blk", 0)
            for sub in range(4):
                t0 = b * BLK + sub * 128
                x_t = xr[0]; xk = ("m_xr", 0)
                k.ld(x_t[:], k.A["x1"][t0:t0 + 128, :], reads=[("D", "tm", id(k.A["x1"]), t0)], writes=[xk], slot=("mxr", 0))
                yks = [("m_yacc", sub, dc) for dc in range(4)]
                k.v("dve", "scalar_tensor_tensor", [xk] + yks, [xk], out=x_t[:], in0=x_t[:], scalar=DN_ALPHA,
                    in1=yacc[:, sub, :], op0=ALU.mult, op1=ALU.add)
                _ln_tail(k, P, T, x_t, xk, lnw, lnb, xout, t0, bb, bk, sub, n)
                n += 1
            k.ld(k.A["xT"][b], bb[:], reads=[bk], writes=[("D", "xT", b)], slot=("mxT", 0))


K.phase_o = _phase_o
K.phase_m = _phase_m


def build(S, depth=DEPTH, n_groups=4, mode="full", dbg=()):
    k = K(S, depth, n_groups, dbg)
    k.layers = list(range(depth))
    if mode in ("hgrn", "rwkv", "even", "odd", "dil", "mlstm"):
        k.layers = [(1 if mode in ("odd", "dil", "mlstm") else 0) + 2 * (depth - 1)]
    k.declare()
    k.precast()
    k.consts()
    k.x0()
    if mode in ("hgrn", "rwkv", "even", "odd", "dil", "mlstm"):
        L = 1 if mode in ("odd", "dil", "mlstm") else 0
        L += 2 * (depth - 1)
        dbgo = k.dram("dbg_mixT", [k.NB, 128, KC, BLK], BF16, kind="ExternalOutput")
        if L % 2 == 0:
            pT = k.inproj(L, 7424, 26, k.I["rwkv_mu"][L // 2])
            if mode in ("rwkv", "even"):
                k.rwkv(L, pT)
            if mode in ("hgrn", "even"):
                k.hgrn(L, pT)
        else:
            pT = k.inproj(L, 6152, 0, None)
            if mode in ("dil", "odd"):
                k.dil(L, pT)
            if mode in ("mlstm", "odd"):
                k.mlstm(L, pT)
        k.sch.barrier()
        halves = {"hgrn": [(8, 16)], "mlstm": [(8, 16)], "rwkv": [(0, 8)], "dil": [(0, 8)]}.get(mode, [(0, 16)])
        for b in range(k.NB):
            for (h0, h1) in halves:
                k.sch.dma("sp", "dbgst", lambda e, b=b, h0=h0, h1=h1: e.dma_start(out=dbgo[b][:, h0:h1, :], in_=k.A["mixT"][b][:, h0:h1, :]))
        k.sch.barrier()
        with k.nc.Block() as block:
            k.sch.emit(block)
        return k
    xin = k.I["x"]
    for L in range(depth):
        last = L == depth - 1
        xout = k.out if last else (k.A["xa"] if L % 2 == 0 else k.A["xb"])
        if mode == "tokenlocal":
            k.A["mixT"] = k.A["xT"]
            for b in range(k.NB):
                if ("D", "xT", b) in k.sch.lastw:
                    k.sch.lastw[("D", "mixT", b, 0)] = k.sch.lastw[("D", "xT", b)]
                    k.sch.lastw[("D", "mixT", b, 1)] = k.sch.lastw[("D", "xT", b)]
        else:
            if L % 2 == 0:
                k.even_mixer(L, xin)
            else:
                k.odd_mixer(L, xin)
        k.phase_o(L, xin)
        k.phase_m(L, xout)
        xin = xout
    k.sch.barrier()
    with k.nc.Block() as block:
        k.sch.emit(block)
    return k


class _PT:
    def __init__(self, groups):
        self.groups = groups

    def __getitem__(self, idx):
        c = idx[0]
        rest = tuple(idx[1:])
        if isinstance(c, slice):
            c0, c1 = c.start, c.stop
            for g0, g1, ap in self.groups:
                if g0 <= c0 and c1 <= g1:
                    return ap[(slice(c0 - g0, c1 - g0),) + rest]
            raise KeyError((c0, c1))
        for g0, g1, ap in self.groups:
            if g0 <= c < g1:
                return ap[(c - g0,) + rest]
        raise KeyError(c)


def _inproj(self, L, ncols, nlerp, mu_ap):
    k = self
    NCH = (ncols + 127) // 128
    S = k.S
    if ("pT", NCH) not in k.A:
        bounds = [0, 8, 16, 24, 26, 34, 42, 50, 58] if NCH == 58 else [0, 8, 16, 24, 32, 40, 48, 49]
        k.A["pT", NCH] = _PT([(g0, g1, k.dram(f"pT{NCH}_{g0}", [g1 - g0, 128, S], F32)) for g0, g1 in zip(bounds[:-1], bounds[1:])])
    pT = k.A["pT", NCH]
    with k.phase() as P:
        xb = [P.sb(f"ip_xb{i}", [128, KC, BLK], BF16) for i in range(2)]
        w = [P.sb(f"ip_w{i}", [128, KC, 512], BF16) for i in range(2)]
        raw = [P.sb(f"ip_raw{i}", [128, 520]) for i in range(2)]
        ob = [P.sb(f"ip_ob{i}", [128, 512]) for i in range(3)]
        dtmp = [P.sb(f"ip_d{i}", [128, 512]) for i in range(2)]
        pp = [P.ps(f"ip_pp{i}", [128, 512]) for i in range(4)]
        if nlerp:
            carry = P.sb("ip_carry", [128, nlerp])
            mu = P.sb("ip_mu", [128, nlerp])
            k.v("pool", "memset", [], ["ip_carry"], ap=carry[:], constant=0.0)
            k.sch.dma("sp", "ipmu", lambda e: e.dma_start(out=mu[:], in_=mu_ap.rearrange("(c p) -> p c", p=128),
                                                           allow_slow_non_contiguous=True), writes=["ip_mu"])
        nw = 0; nc_ = 0; nl = 0
        for b in range(k.NB):
            x_b = xb[b % 2]; xk = ("ip_xb", b % 2)
            k.ld(x_b[:], k.A["xT"][b], reads=[("D", "xT", b)], writes=[xk], slot=("ipx", b % 2))
            for c0 in range(0, ncols, 512):
                c1 = min(ncols, c0 + 512)
                wt = w[nw % 2]; wk = ("ip_w", nw % 2)
                k.ld(wt[:, :, 0:c1 - c0], k.W["in", L][:, :, c0:c1], reads=k.win_keys[L], writes=[wk], slot=("ipw", nw % 2))
                nw += 1
                for cc in range(c0 // 128, (c1 + 127) // 128):
                    m = min(128, ncols - cc * 128)
                    ps = pp[nc_ % 4]; pk = ("ip_pp", nc_ % 4)
                    off = cc * 128 - c0
                    for kc in range(KC):
                        k.mm(ps[0:m, :], wt[:, kc, off:off + m], x_b[:, kc, :], kc == 0, kc == KC - 1, [wk, xk], [pk])
                    o = ob[nc_ % 3]; ok_ = ("ip_ob", nc_ % 3)
                    if cc < nlerp:
                        r = raw[nl % 2]; rk = ("ip_raw", nl % 2)
                        d = dtmp[nl % 2]; dk = ("ip_d", nl % 2)
                        k.v("pool", "tensor_copy", ["ip_carry"], [rk], out=r[:, 0:1], in_=carry[:, cc:cc + 1])
                        k.act(r[:, 1:513], ps[:], AF.Copy, [pk], [rk])
                        k.v("pool", "tensor_copy", [rk], ["ip_carry"], out=carry[:, cc:cc + 1], in_=r[:, 512:513])
                        k.v("dve", "tensor_tensor", [rk], [dk], out=d[:], in0=r[:, 0:512], in1=r[:, 1:513], op=ALU.subtract)
                        k.v("dve", "scalar_tensor_tensor", [dk, rk, "ip_mu"], [ok_], out=o[:], in0=d[:], scalar=mu[:, cc:cc + 1],
                            in1=r[:, 1:513], op0=ALU.mult, op1=ALU.add)
                        nl += 1
                    else:
                        k.copy(k.evac_engine(), o[0:m, :], ps[0:m, :], [pk], [ok_])
                    k.ld(pT[cc, 0:m, b * BLK:(b + 1) * BLK], o[0:m, :], reads=[ok_], writes=[("D", "pT", cc, b)],
                         slot=("ipst", nc_ % 3))
                    nc_ += 1
    return pT


K.inproj = _inproj


def _blockmask(k, P, name, csz, incl=True):
    m = P.sb(name, [128, 128])
    k.v("pool", "memset", [], [name], ap=m[:], constant=1.0)
    k.v("pool", "affine_select", [name], [name], out=m[:], in_=m[:], pattern=[[1, 128]],
        compare_op=ALU.is_ge, fill=0.0, base=(0 if incl else -1), channel_multiplier=-1)
    for c in range(1, 128 // csz):
        k.v("pool", "affine_select", [name], [name], out=m[:, c * csz:(c + 1) * csz], in_=m[:, c * csz:(c + 1) * csz],
            pattern=[[0, csz]], compare_op=ALU.is_ge, fill=0.0, base=-c * csz, channel_multiplier=1)
    return m


def _hgrn(self, L, pT):
    k = self
    j = L // 2
    TB = 256
    C = 64
    CPT = 128 // C
    NCH = TB // C
    Q0, F0, I0, G0 = 26, 34, 42, 50
    with k.phase() as P:
        mask = _blockmask(k, P, "hg_mask", C)
        rmask = P.sb("hg_rmask", [128, TB])
        k.v("pool", "memset", [], ["hg_rmask"], ap=rmask[:], constant=1.0)
        k.v("pool", "memset", ["hg_rmask"], ["hg_rmask"], ap=rmask[:].rearrange("p (c j) -> p c j", j=C)[:, :, 0:1], constant=0.0)
        lb = P.sb("hg_lb", [128, 8]); oml = P.sb("hg_oml", [128, 8])
        if j == 0:
            k.v("pool", "memset", [], ["hg_lb"], ap=lb[:], constant=0.0)
            k.v("pool", "memset", [], ["hg_oml"], ap=oml[:], constant=1.0)
        else:
            l0 = P.sb("hg_l0", [128, 8])
            k.sch.dma("sp", "hgl0", lambda e: e.dma_start(out=l0[:], in_=k.I["hgrn_lb"][0].rearrange("(h p) -> p h", p=128),
                                                           allow_slow_non_contiguous=True), writes=["hg_l0"])
            k.sch.dma("sp", "hgl1", lambda e: e.dma_start(out=lb[:], in_=k.I["hgrn_lb"][1].rearrange("(h p) -> p h", p=128),
                                                           allow_slow_non_contiguous=True), writes=["hg_lb"])
            k.v("dve", "tensor_tensor", ["hg_l0", "hg_lb"], ["hg_lb"], out=lb[:], in0=lb[:], in1=l0[:], op=ALU.subtract)
            k.act(lb[:], lb[:], AF.Sigmoid, ["hg_lb"], ["hg_lb"])
            k.v("dve", "tensor_scalar", ["hg_lb"], ["hg_oml"], out=oml[:], in0=lb[:], scalar1=-1.0, scalar2=1.0,
                op0=ALU.mult, op1=ALU.add)
        nwb = P.sb("hg_nw", [128, 128])
        k.ld(nwb[:], k.I["hgrn_norm_w"][j:j + 1, :].partition_broadcast(128), writes=["hg_nw"])
        qf = [[P.sb(f"hg_{nm}{i}", [128, 8, TB]) for i in range(2)] for nm in ("q", "f", "i", "g")]
        logf = P.sb("hg_logf", [128, 8, TB]); kg = P.sb("hg_kg", [128, 8, TB]); bc = P.sb("hg_bc", [128, 8, TB])
        et = P.sb("hg_et", [128, 8, TB])
        eb = P.sb("hg_eb", [128, 8, NCH])
        qe = P.sb("hg_qe", [128, 8, TB], BF16); ke = P.sb("hg_ke", [128, 8, TB], BF16)
        kd = P.sb("hg_kd", [128, 8, TB], BF16); ib = P.sb("hg_ib", [128, 8, TB], BF16)
        S32 = P.sb("hg_S32", [128, 8, 128]); Sbf = P.sb("hg_Sbf", [128, 8, 8, 128], BF16)
        kdT = [P.sb(f"hg_kdT{i}", [128, 128], BF16) for i in range(3)]
        vT = [P.sb(f"hg_vT{i}", [128, 128], BF16) for i in range(3)]
        aT = [P.sb(f"hg_aT{i}", [128, 128], BF16) for i in range(3)]
        on = [P.sb(f"hg_on{i}", [128, 128], BF16) for i in range(3)]
        sc = [P.sb(f"hg_sc{i}", [128, 4]) for i in range(3)]
        junk = P.sb("hg_junk", [128, 128])
        mixb = [P.sb(f"hg_mix{i}", [128, 8, BLK], BF16) for i in range(2)]
        psA_ = [P.ps(f"hg_psA{i}", [128, 512]) for i in range(1)]
        psO_ = [P.ps(f"hg_psO{i}", [128, 512]) for i in range(2)]
        psS_ = [P.ps(f"hg_psS{i}", [128, 512]) for i in range(2)]
        psT_ = [P.ps(f"hg_psT{i}", [128, 1024], BF16) for i in range(3)]

        class _V:
            def __init__(self, lst):
                self.lst = lst

            def __getitem__(self, idx):
                p, i, c = idx
                return self.lst[i][p, 0:128]
        psA, psO, psS, psT = _V(psA_), _V(psO_), _V(psS_), _V(psT_)
        k.v("pool", "memset", [], ["hg_S32"], ap=S32[:], constant=0.0)
        k.v("pool", "memset", [], [("hg_Sbf", h, 0) for h in range(8)], ap=Sbf[:, :, 0, :], constant=0.0)
        nhb = k.S // TB
        cnt = {"a": 0, "o": 0, "s": 0, "t": 0, "u": 0}
        for hb in range(nhb):
            t0 = hb * TB
            i2 = hb % 2
            names = ("q", "f", "i", "g")
            tl = {}
            for ni, (nm, c0) in enumerate(zip(names, (Q0, F0, I0, G0))):
                tt = qf[ni][i2]
                key = ("hg_" + nm, i2)
                bks = sorted(set(t // BLK for t in (t0, t0 + TB - 1)))
                k.ld(tt[:], pT[c0:c0 + 8, :, t0:t0 + TB].rearrange("c p t -> p c t"),
                     reads=[("D", "pT", c0 + h, bb) for h in range(8) for bb in bks], writes=[key], slot=("hgld", ni, i2))
                tl[nm] = (tt, key)
            (q, qk), (f, fk), (iv, ik), (g, gk) = tl["q"], tl["f"], tl["i"], tl["g"]
            fl = lambda t: t[:].rearrange("p h t -> p (h t)")
            bcast = lambda t: t[:].rearrange("p (h o) -> p h o", o=1).to_broadcast([128, 8, TB])
            k.act(fl(f), fl(f), AF.Sigmoid, [fk], [fk])
            k.v("dve", "tensor_tensor", [fk, "hg_oml"], [fk], out=f[:], in0=f[:], in1=bcast(oml), op=ALU.mult)
            k.v("dve", "tensor_tensor", [fk, "hg_lb"], [fk], out=f[:], in0=f[:], in1=bcast(lb), op=ALU.add)
            k.act(fl(logf), fl(f), AF.Ln, [fk], ["hg_logf"])
            k.v("pool", "tensor_scalar", [fk], ["hg_kg"], out=fl(kg), in0=fl(f), scalar1=-1.0, scalar2=1.0, op0=ALU.mult, op1=ALU.add)
            k.act(fl(q), fl(q), AF.Silu, [qk], [qk])
            k.act(fl(g), fl(g), AF.Silu, [gk], [gk])
            k.v("pool", "tensor_copy", [ik], ["hg_ib"], out=fl(ib), in_=fl(iv))
            for h in range(8):
                k.v("dve", "tensor_tensor_scan", ["hg_logf", "hg_rmask"], [("hg_bc", h)], out=bc[:, h, :], data0=rmask[:],
                    data1=logf[:, h, :], initial=0.0, op0=ALU.mult, op1=ALU.add)
            bck = [("hg_bc", h) for h in range(8)]
            k.v("dve", "tensor_scalar", bck, bck, out=fl(bc), in0=fl(bc), scalar1=-80.0, scalar2=None, op0=ALU.max)
            k.act(fl(et), fl(bc), AF.Exp, bck, ["hg_et"])
            k.v("pool", "tensor_tensor", ["hg_et", qk], ["hg_qe"], out=fl(qe), in0=fl(q), in1=fl(et), op=ALU.mult)
            k.act(fl(et), fl(bc), AF.Exp, bck + ["hg_qe"], ["hg_et"], scale=-1.0)
            k.v("pool", "tensor_tensor", ["hg_et", "hg_kg"], ["hg_ke"], out=fl(ke), in0=fl(kg), in1=fl(et), op=ALU.mult)
            bl = bc[:].rearrange("p h (c j) -> p (h c) j", j=C)[:, :, C - 1:C]
            k.act(eb[:].rearrange("p h (c o) -> p (h c) o", o=1), bl, AF.Exp, bck, ["hg_eb"])
            k.v("dve", "tensor_tensor", bck + ["hg_ke"], ["hg_et"], out=et[:].rearrange("p h (c j) -> p (h c) j", j=C),
                in0=bl.to_broadcast([128, 8 * NCH, C]), in1=bc[:].rearrange("p h (c j) -> p (h c) j", j=C), op=ALU.subtract)
            k.act(fl(et), fl(et), AF.Exp, ["hg_et"], ["hg_et"])
            k.v("pool", "tensor_tensor", ["hg_et", "hg_kg"], ["hg_kd"], out=fl(kd), in0=fl(kg), in1=fl(et), op=ALU.mult)
            for ti in range(TB // 128):
                tg = hb * (TB // 128) + ti
                b = (tg * 128) // BLK
                sub = tg % 4
                mb = mixb[b % 2]; mbk = ("hg_mix", b % 2)
                ts = slice(ti * 128, (ti + 1) * 128)
                for h in range(8):
                    u = cnt["u"] % 3; cnt["u"] += 1
                    k.tr(psT[:, 0, :], kd[:, h, ts], k.c_ident[:], ["hg_kd"], [("hg_psT", 0)])
                    k.copy("act", kdT[u][:], psT[:, 0, :], [("hg_psT", 0)], [("hg_kdT", u)])
                    k.tr(psT[:, 1, :], ib[:, h, ts], k.c_ident[:], ["hg_ib"], [("hg_psT", 1)])
                    k.copy("dve", vT[u][:], psT[:, 1, :], [("hg_psT", 1)], [("hg_vT", u)])
                    ia = 0
                    k.mm(psA[:, ia, :], ke[:, h, ts], qe[:, h, ts], True, True, ["hg_ke", "hg_qe"], [("hg_psA", ia)])
                    k.v("dve", "tensor_tensor", [("hg_psA", ia), "hg_mask"], [("hg_aT", u)], out=aT[u][:], in0=psA[:, ia, :],
                        in1=mask[:], op=ALU.mult)
                    io = cnt["o"] % 2; cnt["o"] += 1
                    ok_ = ("hg_psO", io)
                    for c in range(CPT):
                        gch = tg * CPT + c
                        slot = gch % 8
                        k.mm(psO[c * C:(c + 1) * C, io, :], qe[:, h, ti * 128 + c * C: ti * 128 + (c + 1) * C], Sbf[:, h, slot, :],
                             True, False, ["hg_qe", ("hg_Sbf", h, slot)], [ok_])
                        is_ = cnt["s"] % 2; cnt["s"] += 1
                        k.mm(psS[:, is_, :], kdT[u][c * C:(c + 1) * C, :], vT[u][c * C:(c + 1) * C, :], True, True,
                             [("hg_kdT", u), ("hg_vT", u)], [("hg_psS", is_)])
                        k.v("dve", "scalar_tensor_tensor", [("hg_psS", is_), ("hg_S32", h), "hg_eb"], [("hg_S32", h)],
                            out=S32[:, h, :], in0=S32[:, h, :], scalar=eb[:, h, ti * CPT + c: ti * CPT + c + 1], in1=psS[:, is_, :],
                            op0=ALU.mult, op1=ALU.add)
                        nslot = (gch + 1) % 8
                        k.copy("act", Sbf[:, h, nslot, :], S32[:, h, :], [("hg_S32", h)], [("hg_Sbf", h, nslot)])
                    k.mm(psO[:, io, :], aT[u][:], vT[u][:], False, True, [("hg_aT", u), ("hg_vT", u)], [ok_])
                    s_ = sc[u]; sk = ("hg_sc", u)
                    k.act(junk[:], psO[:, io, :], AF.Square, [ok_], ["hg_junk", sk], accum_out=s_[:, 0:1])
                    k.act(s_[:, 1:2], s_[:, 0:1], AF.Sqrt, [sk], [sk], scale=1.0 / 128, bias=k.c_eps[:, 3:4])
                    k.v("dve", "reciprocal", [sk], [sk], out=s_[:, 2:3], in_=s_[:, 1:2])
                    k.v("dve", "scalar_tensor_tensor", [ok_, sk, "hg_nw"], [("hg_on", u)], out=on[u][:], in0=psO[:, io, :],
                        scalar=s_[:, 2:3], in1=nwb[:], op0=ALU.mult, op1=ALU.mult)
                    it = 2
                    k.tr(psT[:, it, :], on[u][:], k.c_ident[:], [("hg_on", u)], [("hg_psT", it)])
                    k.v("dve", "tensor_tensor", [("hg_psT", it), gk], [mbk], out=mb[:, h, sub * 128:(sub + 1) * 128],
                        in0=psT[:, it, :], in1=g[:, h, ts], op=ALU.mult)
                if sub == 3:
                    k.ld(k.A["mixT"][b][:, 8:16, :], mb[:], reads=[mbk], writes=[("D", "mixT", b, 1)], slot=("hgst", b % 2))


K.hgrn = _hgrn


def _lowmask(k, P, name, csz):
    m = P.sb(name, [128, 128])
    k.v("pool", "memset", [], [name], ap=m[:], constant=1.0)
    k.v("pool", "affine_select", [name], [name], out=m[:], in_=m[:], pattern=[[-1, 128]],
        compare_op=ALU.is_ge, fill=0.0, base=-1, channel_multiplier=1)
    for c in range(1, 128 // csz):
        k.v("pool", "affine_select", [name], [name], out=m[c * csz:(c + 1) * csz, :], in_=m[c * csz:(c + 1) * csz, :],
            pattern=[[1, 128]], compare_op=ALU.is_ge, fill=0.0, base=-c * csz, channel_multiplier=0)
    return m


def _rwkv(self, L, pT):
    k = self
    j = L // 2
    TB = 256
    C = 64
    NCH = TB // C
    I = k.I
    with k.phase() as P:
        mU = _blockmask(k, P, "rw_mU", C, True)
        mUs = _blockmask(k, P, "rw_mUs", C, False)
        mLs = _lowmask(k, P, "rw_mLs", C)
        rmask = P.sb("rw_rmask", [128, TB])
        k.v("pool", "memset", [], ["rw_rmask"], ap=rmask[:], constant=1.0)
        k.v("pool", "memset", ["rw_rmask"], ["rw_rmask"], ap=rmask[:].rearrange("p (c j) -> p c j", j=C)[:, :, 0:1], constant=0.0)
        bones = P.sb("rw_bones", [128, 128], BF16)
        k.v("pool", "memset", [], ["rw_bones"], ap=bones[:], constant=0.0)
        k.v("pool", "memset", ["rw_bones"], ["rw_bones"], ap=bones[0:64, 0:64], constant=1.0)
        k.v("pool", "memset", ["rw_bones"], ["rw_bones"], ap=bones[64:128, 64:128], constant=1.0)
        cst = P.sb("rw_cst", [128, 4])
        for i_, val in enumerate((1.0, -0.5, 1e-24, 0.0)):
            k.v("pool", "memset", [], ["rw_cst"], ap=cst[:, i_:i_ + 1], constant=val)
        prm = {}
        for nm in ("rwkv_w0", "rwkv_a0", "rwkv_kk", "rwkv_ka", "rwkv_rk"):
            t = P.sb("rw_" + nm, [128, 8])
            k.sch.dma("sp", ("rwprm", nm), lambda e, t=t, nm=nm: e.dma_start(out=t[:], in_=I[nm][j].rearrange("(c p) -> p c", p=128),
                                                                        allow_slow_non_contiguous=True), writes=["rw_" + nm])
            prm[nm] = t
        nw0 = P.sb("rw_nw0", [128, 8])
        k.v("dve", "tensor_scalar", ["rw_rwkv_w0"], ["rw_nw0"], out=nw0[:], in0=prm["rwkv_w0"][:], scalar1=-1.0, scalar2=None, op0=ALU.mult)
        lnw = P.sb("rw_lnw", [128, 1024]); lnb = P.sb("rw_lnb", [128, 1024])
        k.ld(lnw[:], I["rwkv_lnx_w"][j:j + 1, :].partition_broadcast(128), writes=["rw_lnw"])
        k.ld(lnb[:], I["rwkv_lnx_b"][j:j + 1, :].partition_broadcast(128), writes=["rw_lnb"])
        w2b = P.sb("rw_w2b", [128, 1024], BF16); a2b = P.sb("rw_a2b", [128, 1024], BF16); g2b = P.sb("rw_g2b", [128, 1024], BF16)
        k.sch.dma("pool", "rwlw", lambda e: e.dma_start(out=w2b[0:64, :], in_=I["rwkv_w2"][j]), writes=["rw_w2b"])
        k.sch.dma("pool", "rwla", lambda e: e.dma_start(out=a2b[64:128, :], in_=I["rwkv_a2"][j]), writes=["rw_a2b"])
        k.sch.dma("pool", "rwlg", lambda e: e.dma_start(out=g2b[:, :], in_=I["rwkv_g2"][j]), writes=["rw_g2b"])
        hsel = P.sb("rw_hsel", [128, 2], BF16)
        k.v("pool", "memset", [], ["rw_hsel"], ap=hsel[:], constant=0.0)
        k.v("pool", "memset", ["rw_hsel"], ["rw_hsel"], ap=hsel[0:64, 0:1], constant=1.0)
        k.v("pool", "memset", ["rw_hsel"], ["rw_hsel"], ap=hsel[64:128, 1:2], constant=1.0)
        F3 = [128, 8, TB]
        r_ = P.sb("rw_r", F3); k_ = P.sb("rw_k", F3); v_ = P.sb("rw_v", F3)
        xwa = P.sb("rw_xwa", [128, TB]); xg = P.sb("rw_xg", [128, TB])
        th = P.sb("rw_th", [128, TB], BF16); sg = P.sb("rw_sg", [128, TB], BF16)
        d_ = P.sb("rw_d", F3); alr = P.sb("rw_alr", F3); kkn = P.sb("rw_kkn", F3); kp = P.sb("rw_kp", F3)
        cum = P.sb("rw_cum", F3); et = P.sb("rw_et", F3); t1 = P.sb("rw_t1", F3)
        B3 = lambda nm: P.sb(nm, F3, BF16)
        at, bt, kt, rt, bh, kh, vb, prod, kk2 = [B3("rw_" + n) for n in ("at", "bt", "kt", "rt", "bh", "kh", "vb", "prod", "kk2")]
        pc = P.sb("rw_pc", [128, 8, NCH])
        tm = {n: [P.sb(f"rw_tm_{n}{i}", [128, 8, 128], BF16) for i in range(2)] for n in ("v", "a", "bh", "kh")}
        ytm = P.sb("rw_ytm", [128, 16, 64]); gtm = P.sb("rw_gtm", [128, 1024]); rkb = P.sb("rw_rkb", [128, 16])
        st = P.sb("rw_st", [128, 4, 16]); ysq = P.sb("rw_ysq", [128, 16, 64]); yb = P.sb("rw_yb", [128, 1024], BF16)
        mixb = [P.sb(f"rw_mix{i}", [128, 8, BLK], BF16) for i in range(2)]
        NR = 2
        dbl = {n: [P.sb(f"rw_{n}{i}", [128, 128], BF16) for i in range(2 * NR)] for n in ("X", "Y", "Pm", "Qm")}
        gm = {n: [P.sb(f"rw_{n}{i}", [128, 128], BF16) for i in range(NR)] for n in ("Kt", "RBt", "RKt")}
        zs = [P.sb(f"rw_zs{i}", [128, 64], BF16) for i in range(NR)]
        uloc = [P.sb(f"rw_uloc{i}", [128, 64]) for i in range(NR)]
        wt = [P.sb(f"rw_wt{i}", [128, 128], BF16) for i in range(NR)]
        ub = [P.sb(f"rw_ub{i}", [128, 64], BF16) for i in range(NR)]
        G32 = P.sb("rw_G32", [128, 8, 64]); Gbf = P.sb("rw_Gbf", [128, 8, 4, 64], BF16)
        k.v("pool", "memset", [], ["rw_G32"], ap=G32[:], constant=0.0)
        k.v("pool", "memset", [], [("rw_Gbf", h, 0) for h in range(16)], ap=Gbf[:, :, 0, :], constant=0.0)
        pg = [P.ps(f"rw_pg{i}", [128, 512]) for i in range(2)]
        pTt = [P.ps(f"rw_pT{i}", [128, 1024], BF16) for i in range(2)]
        pU = P.ps("rw_pU", [128, 512]); pG = P.ps("rw_pG", [128, 512]); pY = P.ps("rw_pY", [128, 512]); pM = P.ps("rw_pM", [128, 512])
        cn = {"g": 0, "t": 0, "u": 0}
        fl = lambda t: t[:].rearrange("p h t -> p (h t)")
        bc8 = lambda t: t[:].rearrange("p (h o) -> p h o", o=1).to_broadcast([128, 8, TB])
        c3 = lambda t: t[:].rearrange("p h (c j) -> p (h c) j", j=C)

        def gram(lhsT, rhs, mask, out, okey, rkeys):
            i = cn["g"] % 2; cn["g"] += 1
            k.mm(pg[i][:, 0:128], lhsT, rhs, True, True, rkeys, [("rw_pg", i)])
            k.v("dve", "tensor_tensor", [("rw_pg", i), mask[1]], [okey], out=out, in0=pg[i][:, 0:128], in1=mask[0][:], op=ALU.mult)

        nhb = k.S // TB
        for hb in range(nhb):
            t0 = hb * TB
            bks = sorted(set(t // BLK for t in (t0, t0 + TB - 1)))
            for nm, tt, c0 in (("r", r_, 0), ("k", k_, 8), ("v", v_, 16)):
                k.ld(tt[:], pT[c0:c0 + 8, :, t0:t0 + TB].rearrange("c p t -> p c t"),
                     reads=[("D", "pT", c0 + h, bb) for h in range(8) for bb in bks], writes=["rw_" + nm], slot=("rwld", nm))
            k.ld(xwa[:], pT[24, :, t0:t0 + TB], reads=[("D", "pT", 24, bb) for bb in bks], writes=["rw_xwa"], slot=("rwld", "xwa"))
            k.ld(xg[:], pT[25, :, t0:t0 + TB], reads=[("D", "pT", 25, bb) for bb in bks], writes=["rw_xg"], slot=("rwld", "xg"))
            k.act(th[0:64, :], xwa[0:64, :], AF.Tanh, ["rw_xwa"], ["rw_th"])
            k.act(th[64:128, :], xwa[64:128, :], AF.Copy, ["rw_xwa"], ["rw_th"])
            k.act(sg[:], xg[:], AF.Sigmoid, ["rw_xg"], ["rw_sg"])
            for hp in range(8):
                cs = slice(hp * 128, (hp + 1) * 128)
                k.mm(pM[:, 0:TB], w2b[0:64, cs], th[0:64, :], True, True, ["rw_w2b", "rw_th"], ["rw_pM"])
                k.act(d_[:, hp, :], pM[:, 0:TB], AF.Exp, ["rw_pM", "rw_nw0"], [("rw_d", hp)], scale=-1.0, bias=nw0[:, hp:hp + 1])
                k.mm(pM[:, 0:TB], a2b[64:128, cs], th[64:128, :], True, True, ["rw_a2b", "rw_th"], ["rw_pM"])
                k.act(alr[:, hp, :], pM[:, 0:TB], AF.Sigmoid, ["rw_pM", "rw_rwkv_a0"], [("rw_alr", hp)], bias=prm["rwkv_a0"][:, hp:hp + 1])
            dk = [("rw_d", hp) for hp in range(8)]; ak = [("rw_alr", hp) for hp in range(8)]
            k.act(fl(d_), fl(d_), AF.Ln, dk, dk, bias=cst[:, 0:1])
            k.act(fl(d_), fl(d_), AF.Exp, dk, dk, scale=-1.0, bias=cst[:, 1:2])
            k.v("dve", "tensor_tensor", ["rw_k", "rw_rwkv_kk"], ["rw_kkn"], out=kkn[:], in0=k_[:], in1=bc8(prm["rwkv_kk"]), op=ALU.mult)
            k.v("pool", "tensor_tensor", ["rw_kkn"], ["rw_kk2"], out=fl(kk2), in0=fl(kkn), in1=fl(kkn), op=ALU.mult)
            for hp in range(8):
                k.mm(pM[:, 0:TB], bones[:], kk2[:, hp, :], True, True, ["rw_bones", "rw_kk2"], ["rw_pM"])
                k.act(t1[:, hp, :], pM[:, 0:TB], AF.Sqrt, ["rw_pM"], [("rw_t1", hp)], bias=cst[:, 2:3])
            tk = [("rw_t1", hp) for hp in range(8)]
            k.v("dve", "reciprocal", tk, tk, out=fl(t1), in_=fl(t1))
            k.v("dve", "tensor_tensor", tk + ["rw_kkn"], ["rw_kkn"], out=fl(kkn), in0=fl(kkn), in1=fl(t1), op=ALU.mult)
            k.v("dve", "scalar_tensor_tensor", ak + ["rw_rwkv_ka"], tk, out=t1[:], in0=alr[:], scalar=-1.0, in1=bc8(prm["rwkv_ka"]),
                op0=ALU.add, op1=ALU.mult)
            k.v("dve", "scalar_tensor_tensor", tk + ["rw_k"], ["rw_kp"], out=fl(kp), in0=fl(t1), scalar=1.0, in1=fl(k_),
                op0=ALU.add, op1=ALU.mult)
            k.v("pool", "tensor_tensor", ["rw_r", "rw_kp"], tk, out=fl(t1), in0=fl(r_), in1=fl(kp), op=ALU.mult)
            k.v("pool", "tensor_tensor", tk + ["rw_rwkv_rk"], ["rw_prod"], out=prod[:], in0=t1[:], in1=bc8(prm["rwkv_rk"]), op=ALU.mult)
            k.v("pool", "tensor_copy", ["rw_v"], ["rw_vb"], out=fl(vb), in_=fl(v_))
            for hp in range(8):
                k.v("dve", "tensor_tensor_scan", dk + ["rw_rmask"], [("rw_cum", hp)], out=cum[:, hp, :], data0=rmask[:],
                    data1=d_[:, hp, :], initial=0.0, op0=ALU.mult, op1=ALU.add)
            ck = [("rw_cum", hp) for hp in range(8)]
            cl = c3(cum)[:, :, C - 1:C]
            k.act(pc[:].rearrange("p h (c o) -> p (h c) o", o=1), cl, AF.Exp, ck, ["rw_pc"], scale=-1.0)
            k.act(fl(et), fl(cum), AF.Exp, ck, ["rw_et"], scale=-1.0)
            k.v("pool", "tensor_tensor", ["rw_et", "rw_r"], ["rw_rt"], out=fl(rt), in0=fl(r_), in1=fl(et), op=ALU.mult)
            k.v("dve", "tensor_tensor", ["rw_kkn"] + ak, tk, out=fl(t1), in0=fl(kkn), in1=fl(alr), op=ALU.mult)
            k.act(fl(et), fl(cum), AF.Exp, ck + ["rw_rt"], ["rw_et"])
            k.v("pool", "tensor_tensor", ["rw_et"] + tk, ["rw_bt"], out=fl(bt), in0=fl(t1), in1=fl(et), op=ALU.mult)
            k.v("dve", "tensor_tensor", ["rw_et", "rw_kp"], ["rw_kt"], out=fl(kt), in0=fl(kp), in1=fl(et), op=ALU.mult)
            k.v("dve", "tensor_tensor", ck + ["rw_bt", "rw_kt"], ["rw_et"], out=c3(et), in0=c3(cum), in1=cl.to_broadcast([128, 8 * NCH, C]),
                op=ALU.subtract)
            k.act(fl(et), fl(et), AF.Exp, ["rw_et"], ["rw_et"])
            k.v("pool", "tensor_tensor", ["rw_et"] + tk, ["rw_bh"], out=fl(bh), in0=fl(t1), in1=fl(et), op=ALU.mult)
            k.v("dve", "tensor_tensor", ["rw_et", "rw_kp"], ["rw_kh"], out=fl(kh), in0=fl(kp), in1=fl(et), op=ALU.mult)
            k.v("dve", "tensor_tensor", ck + dk + ["rw_bh", "rw_kh"], ["rw_et"], out=fl(et), in0=fl(d_), in1=fl(cum), op=ALU.subtract)
            k.act(fl(et), fl(et), AF.Exp, ["rw_et"], ["rw_et"])
            k.v("dve", "scalar_tensor_tensor", ["rw_et", "rw_kkn"], ["rw_at"], out=fl(at), in0=fl(kkn), scalar=-1.0, in1=fl(et),
                op0=ALU.mult, op1=ALU.mult)
            for ti in range(TB // 128):
                tg = hb * (TB // 128) + ti
                b = (tg * 128) // BLK
                sub = tg % 4
                mb = mixb[b % 2]; mbk = ("rw_mix", b % 2)
                ts = slice(ti * 128, (ti + 1) * 128)
                i2 = tg % 2
                for n_i, (nm, src, skey) in enumerate((("v", vb, "rw_vb"), ("a", at, "rw_at"), ("bh", bh, "rw_bh"), ("kh", kh, "rw_kh"))):
                    ip = cn["t"] % 2; cn["t"] += 1
                    for hp in range(8):
                        k.tr(pTt[ip][:, hp * 128:(hp + 1) * 128], src[:, hp, ts], k.c_ident[:], [skey], [("rw_pT", ip)])
                    k.copy(k.evac_engine(), tm[nm][i2][:].rearrange("p h c -> p (h c)"), pTt[ip][:], [("rw_pT", ip)], [("rw_tm", nm, i2)])
                for hp in range(8):
                    k.mm(pM[:, 2 * hp:2 * hp + 2], prod[:, hp, ts], hsel[:], True, True, ["rw_prod", "rw_hsel"], ["rw_pM"])
                k.copy("act", rkb[:], pM[:, 0:16], ["rw_pM"], ["rw_rkb"])
                for q in range(2):
                    k.mm(pM[:, :], sg[:, ts], g2b[:, q * 512:(q + 1) * 512], True, True, ["rw_sg", "rw_g2b"], ["rw_pM"])
                    k.copy("act", gtm[:, q * 512:(q + 1) * 512], pM[:, :], ["rw_pM"], ["rw_gtm"])
                for hp in range(8):
                    for par in range(2):
                        h = 2 * hp + par
                        pr = slice(par * 64, (par + 1) * 64)
                        u = cn["u"] % NR; cn["u"] += 1
                        A_, B_, K_, R_ = at[pr, hp, ts], bt[pr, hp, ts], kt[pr, hp, ts], rt[pr, hp, ts]
                        X = dbl["X"]; Y = dbl["Y"]; Pm = dbl["Pm"]; Qm = dbl["Qm"]
                        xi = lambda lvl: 2 * u + (lvl % 2)
                        gram(B_, A_, (mUs, "rw_mUs"), X[xi(0)][:], ("rw_X", xi(0)), ["rw_bt", "rw_at"])
                        gram(A_, B_, (mLs, "rw_mLs"), Y[xi(0)][:], ("rw_Y", xi(0)), ["rw_bt", "rw_at"])
                        gram(K_, A_, (mUs, "rw_mUs"), gm["Kt"][u][:], ("rw_Kt", u), ["rw_kt", "rw_at"])
                        gram(B_, R_, (mU, "rw_mU"), gm["RBt"][u][:], ("rw_RBt", u), ["rw_bt", "rw_rt"])
                        gram(K_, R_, (mU, "rw_mU"), gm["RKt"][u][:], ("rw_RKt", u), ["rw_kt", "rw_rt"])
                        pcur, qcur = ("rw_X", xi(0)), ("rw_Y", xi(0))
                        Pc, Qc = X[xi(0)], Y[xi(0)]
                        for lvl in range(5):
                            xo, xn = xi(lvl), xi(lvl + 1)
                            last = lvl == 4
                            i = cn["g"] % 2; cn["g"] += 1
                            k.mm(pg[i][:, 0:128], Y[xo][:], X[xo][:], True, True, [("rw_Y", xo), ("rw_X", xo)], [("rw_pg", i)])
                            k.copy(k.evac_engine(), X[xn][:], pg[i][:, 0:128], [("rw_pg", i)], [("rw_X", xn)])
                            if not last:
                                i = cn["g"] % 2; cn["g"] += 1
                                k.mm(pg[i][:, 0:128], X[xo][:], Y[xo][:], True, True, [("rw_Y", xo), ("rw_X", xo)], [("rw_pg", i)])
                                k.copy(k.evac_engine(), Y[xn][:], pg[i][:, 0:128], [("rw_pg", i)], [("rw_Y", xn)])
                            pn = 2 * u + (lvl % 2)
                            i = cn["g"] % 2; cn["g"] += 1
                            k.mm(pg[i][:, 0:128], Qc[:], X[xn][:], True, False, [qcur, ("rw_X", xn)], [("rw_pg", i)])
                            k.mm(pg[i][:, 0:128], k.c_ident[:], X[xn][:], False, False, [("rw_X", xn)], [("rw_pg", i)])
                            k.mm(pg[i][:, 0:128], k.c_ident[:], Pc[:], False, True, [pcur], [("rw_pg", i)])
                            k.copy(k.evac_engine(), Pm[pn][:], pg[i][:, 0:128], [("rw_pg", i)], [("rw_Pm", pn)])
                            if not last:
                                i = cn["g"] % 2; cn["g"] += 1
                                k.mm(pg[i][:, 0:128], Pc[:], Y[xn][:], True, False, [pcur, ("rw_Y", xn)], [("rw_pg", i)])
                                k.mm(pg[i][:, 0:128], k.c_ident[:], Y[xn][:], False, False, [("rw_Y", xn)], [("rw_pg", i)])
                                k.mm(pg[i][:, 0:128], k.c_ident[:], Qc[:], False, True, [qcur], [("rw_pg", i)])
                                k.copy(k.evac_engine(), Qm[pn][:], pg[i][:, 0:128], [("rw_pg", i)], [("rw_Qm", pn)])
                                Qc, qcur = Qm[pn], ("rw_Qm", pn)
                            Pc, pcur = Pm[pn], ("rw_Pm", pn)
                        Tt, tkey = Pc, pcur
                        Vh = tm["v"][i2][:, hp, pr]; vkey = ("rw_tm", "v", i2)
                        i = cn["g"] % 2; cn["g"] += 1
                        k.mm(pg[i][:, 0:64], gm["Kt"][u][:], Vh, True, True, [("rw_Kt", u), vkey], [("rw_pg", i)])
                        k.copy(k.evac_engine(), zs[u][:], pg[i][:, 0:64], [("rw_pg", i)], [("rw_zs", u)])
                        i = cn["g"] % 2; cn["g"] += 1
                        k.mm(pg[i][:, 0:64], k.c_ident[:], zs[u][:], True, False, [("rw_zs", u)], [("rw_pg", i)])
                        k.mm(pg[i][:, 0:64], Tt[:], zs[u][:], False, True, [tkey, ("rw_zs", u)], [("rw_pg", i)])
                        k.copy(k.evac_engine(), uloc[u][:], pg[i][:, 0:64], [("rw_pg", i)], [("rw_uloc", u)])
                        i = cn["g"] % 2; cn["g"] += 1
                        k.mm(pg[i][pr, 0:128], tm["a"][i2][:, hp, pr], Tt[:], True, True, [("rw_tm", "a", i2), tkey], [("rw_pg", i)])
                        k.v("dve", "tensor_tensor", [("rw_pg", i), "rw_at"], [("rw_wt", u)], out=wt[u][pr, :], in0=pg[i][pr, 0:128], in1=A_,
                            op=ALU.add)
                        for c in range(2):
                            gch = tg * 2 + c
                            slot = gch % 4
                            cr = slice(c * 64, (c + 1) * 64)
                            gk = ("rw_Gbf", h, slot)
                            k.mm(pU[cr, 0:64], wt[u][pr, cr], Gbf[pr, hp, slot, :], True, True, [("rw_wt", u), gk], ["rw_pU"])
                            k.v("dve", "tensor_tensor", ["rw_pU", ("rw_uloc", u)], [("rw_ub", u)], out=ub[u][cr, :], in0=pU[cr, 0:64],
                                in1=uloc[u][cr, :], op=ALU.add)
                            k.mm(pY[cr, 0:64], rt[pr, hp, ti * 128 + c * 64: ti * 128 + (c + 1) * 64], Gbf[pr, hp, slot, :], True, False,
                                 ["rw_rt", gk], ["rw_pY"])
                            k.mm(pG[pr, 0:64], tm["bh"][i2][cr, hp, pr], ub[u][cr, :], True, False, [("rw_tm", "bh", i2), ("rw_ub", u)], ["rw_pG"])
                            k.mm(pG[pr, 0:64], tm["kh"][i2][cr, hp, pr], tm["v"][i2][cr, hp, pr], False, True, [("rw_tm", "kh", i2), vkey], ["rw_pG"])
                            k.v("dve", "scalar_tensor_tensor", ["rw_pG", ("rw_G32", h), "rw_pc"], [("rw_G32", h)], out=G32[pr, hp, :],
                                in0=G32[pr, hp, :], scalar=pc[pr, hp, ti * 2 + c: ti * 2 + c + 1], in1=pG[pr, 0:64], op0=ALU.mult, op1=ALU.add)
                            ns = (gch + 1) % 4
                            k.copy("act", Gbf[pr, hp, ns, :], G32[pr, hp, :], [("rw_G32", h)], [("rw_Gbf", h, ns)])
                        k.mm(pY[:, 0:64], gm["RBt"][u][:], ub[u][:], False, False, [("rw_RBt", u), ("rw_ub", u)], ["rw_pY"])
                        k.mm(pY[:, 0:64], gm["RKt"][u][:], Vh, False, True, [("rw_RKt", u), vkey], ["rw_pY"])
                        k.copy("act", ytm[:, h, :], pY[:, 0:64], ["rw_pY"], ["rw_ytm"])
                yk = ["rw_ytm"]
                k.v("dve", "tensor_reduce", yk, ["rw_st"], out=st[:, 0, :], in_=ytm[:], axis=AX.X, op=ALU.add)
                k.v("dve", "tensor_scalar", ["rw_st"], ["rw_st"], out=st[:, 0, :], in0=st[:, 0, :], scalar1=1.0 / 64, scalar2=None, op0=ALU.mult)
                b16 = lambda a: a.rearrange("p (h o) -> p h o", o=1).to_broadcast([128, 16, 64])
                k.v("dve", "tensor_tensor", yk + ["rw_st"], yk, out=ytm[:], in0=ytm[:], in1=b16(st[:, 0, :]), op=ALU.subtract)
                k.v("pool", "tensor_tensor", yk, ["rw_ysq"], out=ysq[:], in0=ytm[:], in1=ytm[:], op=ALU.mult)
                k.v("dve", "tensor_reduce", ["rw_ysq"], ["rw_st"], out=st[:, 1, :], in_=ysq[:], axis=AX.X, op=ALU.add)
                k.act(st[:, 2, :], st[:, 1, :], AF.Sqrt, ["rw_st"], ["rw_st"], scale=1.0 / 64, bias=k.c_eps[:, 2:3])
                k.v("dve", "reciprocal", ["rw_st"], ["rw_st"], out=st[:, 3, :], in_=st[:, 2, :])
                k.v("dve", "tensor_tensor", yk + ["rw_st"], yk, out=ytm[:], in0=ytm[:], in1=b16(st[:, 3, :]), op=ALU.mult)
                yf = ytm[:].rearrange("p h c -> p (h c)")
                k.v("pool", "tensor_tensor", yk + ["rw_lnw"], yk, out=yf, in0=yf, in1=lnw[:], op=ALU.mult)
                k.v("pool", "tensor_tensor", yk + ["rw_lnb"], yk, out=yf, in0=yf, in1=lnb[:], op=ALU.add)
                k.v("dve", "tensor_tensor", ["rw_rkb", ("rw_tm", "v", i2)], ["rw_ysq"], out=ysq[:], in0=b16(rkb[:]),
                    in1=tm["v"][i2][:].rearrange("p h (a c) -> p (h a) c", a=2), op=ALU.mult)
                k.v("dve", "tensor_tensor", yk + ["rw_ysq"], yk, out=ytm[:], in0=ytm[:], in1=ysq[:], op=ALU.add)
                k.v("dve", "tensor_tensor", yk + ["rw_gtm"], ["rw_yb"], out=yb[:], in0=yf, in1=gtm[:], op=ALU.mult)
                ip = cn["t"] % 2; cn["t"] += 1
                for hp in range(8):
                    k.tr(pTt[ip][:, hp * 128:(hp + 1) * 128], yb[:, hp * 128:(hp + 1) * 128], k.c_ident[:], ["rw_yb"], [("rw_pT", ip)])
                k.copy(k.evac_engine(), mb[:, :, sub * 128:(sub + 1) * 128], pTt[ip][:].rearrange("p (h c) -> p h c", c=128), [("rw_pT", ip)], [mbk])
                if sub == 3:
                    k.ld(k.A["mixT"][b][:, 0:8, :], mb[:], reads=[mbk], writes=[("D", "mixT", b, 0)], slot=("rwst", b % 2))


K.rwkv = _rwkv


def _mlstm(self, L, pT):
    k = self
    j = L // 2
    TB = 256
    I = k.I
    NT = TB // 128
    with k.phase() as P:
        mU = _blockmask(k, P, "ml_mU", 128, True)
        ones4 = P.sb("ml_ones4", [4, TB])
        k.v("pool", "memset", [], ["ml_ones4"], ap=ones4[:], constant=1.0)
        selh = P.sb("ml_selh", [4, 4, 128])
        k.v("pool", "memset", [], ["ml_selh"], ap=selh[:], constant=1.0)
        k.v("pool", "affine_select", ["ml_selh"], ["ml_selh"], out=selh[:], in_=selh[:], pattern=[[-1, 4], [0, 128]],
            compare_op=ALU.is_equal, fill=0.0, base=0, channel_multiplier=1)
        cw = P.sb("ml_cw", [128, 8, 4]); cb = P.sb("ml_cb", [128, 8])
        for i_ in range(4):
            k.sch.dma("sp", ("mlcw", i_), lambda e, i_=i_: e.dma_start(out=cw[:, :, i_], in_=I["mlstm_conv_w"][j, i_].rearrange("(c p) -> p c", p=128),
                                                                   allow_slow_non_contiguous=True), writes=["ml_cw"])
        k.sch.dma("sp", "mlcb", lambda e: e.dma_start(out=cb[:], in_=I["mlstm_conv_b"][j].rearrange("(c p) -> p c", p=128),
                                                       allow_slow_non_contiguous=True), writes=["ml_cb"])
        bi = P.sb("ml_bi", [4, 1]); bf = P.sb("ml_bf", [4, 1]); nbf = P.sb("ml_nbf", [4, 1]); one1 = P.sb("ml_one1", [4, 1])
        k.sch.dma("sp", "mlbi", lambda e: e.dma_start(out=bi[:], in_=I["mlstm_b_i"][j].rearrange("(h o) -> h o", o=1)), writes=["ml_bi"])
        k.sch.dma("sp", "mlbf", lambda e: e.dma_start(out=bf[:], in_=I["mlstm_b_f"][j].rearrange("(h o) -> h o", o=1)), writes=["ml_bf"])
        k.v("dve", "tensor_scalar", ["ml_bf"], ["ml_nbf"], out=nbf[:], in0=bf[:], scalar1=-1.0, scalar2=None, op0=ALU.mult)
        k.v("pool", "memset", [], ["ml_one1"], ap=one1[:], constant=1.0)
        qk = P.sb("ml_qk", [128, 8, TB + 3]); acc = P.sb("ml_acc", [128, 8, TB]); tmp = P.sb("ml_tmp", [128, 8, TB])
        qkb = P.sb("ml_qkb", [128, 8, TB], BF16)
        vv = P.sb("ml_v", [128, 8, TB]); vb = P.sb("ml_vb", [128, 8, TB], BF16)
        og = P.sb("ml_og", [128, 8, TB])
        gi = P.sb("ml_gi", [4, TB]); gf = P.sb("ml_gf", [4, TB])
        Fs = [P.sb(f"ml_F{i}", [4, TB]) for i in range(2)]; Ms = [P.sb(f"ml_M{i}", [4, TB]) for i in range(2)]
        cv = P.sb("ml_cv", [4, TB]); rw = P.sb("ml_rw", [4, 3, TB]); ec = P.sb("ml_ec", [4, NT]); mprev = P.sb("ml_mprev", [4, NT])
        zero1 = P.sb("ml_zero1", [4, 1])
        k.v("pool", "memset", [], ["ml_zero1"], ap=zero1[:], constant=0.0)
        tok = [P.sb(f"ml_tok{i}", [128, 12]) for i in range(2)]
        ecb = [P.sb(f"ml_ecb{i}", [128, 4]) for i in range(2)]
        ktm = [P.sb(f"ml_ktm{i}", [128, 128], BF16) for i in range(2)]
        vx = [P.sb(f"ml_vx{i}", [128, 257], BF16) for i in range(2)]
        aT = [P.sb(f"ml_aT{i}", [128, 128], BF16) for i in range(2)]
        sc = [P.sb(f"ml_sc{i}", [128, 8]) for i in range(2)]
        hb_ = [P.sb(f"ml_hb{i}", [128, 256], BF16) for i in range(2)]
        C32 = P.sb("ml_C32", [128, 4, 257]); Cbf = P.sb("ml_Cbf", [128, 4, 2, 257], BF16)
        k.v("pool", "memset", [], ["ml_C32"], ap=C32[:], constant=0.0)
        k.v("pool", "memset", [], [("ml_Cbf", h, 0) for h in range(4)], ap=Cbf[:, :, 0, :], constant=0.0)
        mixb = [P.sb(f"ml_mix{i}", [128, 8, BLK], BF16) for i in range(2)]
        pTk = P.ps("ml_pTk", [128, 1024], BF16); pTv = P.ps("ml_pTv", [128, 1024], BF16); pTh = P.ps("ml_pTh", [128, 1024], BF16)
        pA = P.ps("ml_pA", [128, 512]); pO = [P.ps(f"ml_pO{i}", [128, 512]) for i in range(2)]; pC = P.ps("ml_pC", [128, 512])
        pS = P.ps("ml_pS", [128, 512])
        fl = lambda t: t[:].rearrange("p h t -> p (h t)")
        nhb = k.S // TB
        cn = {"u": 0, "o": 0}
        for hb in range(nhb):
            t0 = hb * TB
            bks = sorted(set(t // BLK for t in (max(t0 - 3, 0), t0 + TB - 1)))
            rk = lambda c0, n: [("D", "pT", c0 + h, bb) for h in range(n) for bb in bks]
            if hb == 0:
                k.v("pool", "memset", [], ["ml_qk"], ap=qk[:, :, 0:3], constant=0.0)
                k.ld(qk[:, :, 3:], pT[24:32, :, 0:TB].rearrange("c p t -> p c t"), reads=rk(24, 8), writes=["ml_qk"], slot="mlqk")
            else:
                k.ld(qk[:], pT[24:32, :, t0 - 3:t0 + TB].rearrange("c p t -> p c t"), reads=rk(24, 8), writes=["ml_qk"], slot="mlqk")
            k.ld(vv[:], pT[32:40, :, t0:t0 + TB].rearrange("c p t -> p c t"), reads=rk(32, 8), writes=["ml_v"], slot="mlv")
            k.ld(og[:], pT[40:48, :, t0:t0 + TB].rearrange("c p t -> p c t"), reads=rk(40, 8), writes=["ml_og"], slot="mlog")
            k.ld(gi[:], pT[48, 0:4, t0:t0 + TB], reads=rk(48, 1), writes=["ml_gi"], slot="mlgi")
            k.ld(gf[:], pT[48, 4:8, t0:t0 + TB], reads=rk(48, 1), writes=["ml_gf"], slot="mlgf")
            wb = lambda i: cw[:, :, i:i + 1].to_broadcast([128, 8, TB])
            k.v("dve", "tensor_tensor", ["ml_qk", "ml_cw"], ["ml_acc"], out=acc[:], in0=qk[:, :, 3:3 + TB], in1=wb(3), op=ALU.mult)
            for i in range(3):
                k.v("pool", "tensor_tensor", ["ml_qk", "ml_cw"], ["ml_tmp"], out=tmp[:], in0=qk[:, :, i:i + TB], in1=wb(i), op=ALU.mult)
                k.v("dve", "tensor_tensor", ["ml_tmp", "ml_acc"], ["ml_acc"], out=fl(acc), in0=fl(acc), in1=fl(tmp), op=ALU.add)
            for c in range(8):
                k.act(qkb[:, c, :], acc[:, c, :], AF.Silu, ["ml_acc", "ml_cb"], [("ml_qkb", c)], bias=cb[:, c:c + 1])
            qkk = [("ml_qkb", c) for c in range(8)]
            k.v("pool", "tensor_copy", ["ml_v"], ["ml_vb"], out=fl(vb), in_=fl(vv))
            k.act(fl(og), fl(og), AF.Sigmoid, ["ml_og"], ["ml_og"])
            Fc, Fp = Fs[hb % 2], Fs[(hb + 1) % 2]
            Mc, Mp = Ms[hb % 2], Ms[(hb + 1) % 2]
            fk, fpk, mk, mpk = ("ml_F", hb % 2), ("ml_F", (hb + 1) % 2), ("ml_M", hb % 2), ("ml_M", (hb + 1) % 2)
            k.act(gf[:], gf[:], AF.Exp, ["ml_gf", "ml_nbf"], ["ml_gf"], scale=-1.0, bias=nbf[:, 0:1])
            k.act(gf[:], gf[:], AF.Ln, ["ml_gf", "ml_one1"], ["ml_gf"], bias=one1[:, 0:1])
            k.v("dve", "tensor_scalar", ["ml_gf"], ["ml_gf"], out=gf[:], in0=gf[:], scalar1=-1.0, scalar2=None, op0=ALU.mult)
            k.v("dve", "tensor_tensor_scan", ["ml_gf", "ml_ones4", fpk], [fk], out=Fc[:], data0=ones4[:], data1=gf[:],
                initial=(0.0 if hb == 0 else Fp[:, TB - 1:TB]), op0=ALU.mult, op1=ALU.add)
            k.v("dve", "scalar_tensor_tensor", ["ml_gi", "ml_bi", fk], ["ml_cv"], out=cv[:], in0=gi[:], scalar=bi[:, 0:1], in1=Fc[:],
                op0=ALU.add, op1=ALU.subtract)
            k.v("dve", "tensor_tensor_scan", ["ml_cv", "ml_ones4", mpk], [mk], out=Mc[:], data0=ones4[:], data1=cv[:],
                initial=(0.0 if hb == 0 else Mp[:, TB - 1:TB]), op0=ALU.mult, op1=ALU.max)
            for ti in range(NT):
                if ti == 0:
                    src = zero1[:, 0:1] if hb == 0 else Mp[:, TB - 1:TB]
                    sk = ["ml_zero1"] if hb == 0 else [mpk]
                else:
                    src = Mc[:, ti * 128 - 1:ti * 128]; sk = [mk]
                k.v("dve", "tensor_copy", sk, ["ml_mprev"], out=mprev[:, ti:ti + 1], in_=src)
            Mc3 = Mc[:].rearrange("p (c j) -> p c j", j=128)
            mpb = mprev[:].rearrange("p (c o) -> p c o", o=1).to_broadcast([4, NT, 128])
            mlast = Mc3[:, :, 127:128]
            r3 = lambda i: rw[:, i, :].rearrange("p (c j) -> p c j", j=128)
            cv3 = cv[:].rearrange("p (c j) -> p c j", j=128)
            k.v("dve", "tensor_tensor", [mk, "ml_mprev"], ["ml_rw"], out=r3(0), in0=mpb, in1=Mc3, op=ALU.subtract)
            k.v("dve", "tensor_tensor", ["ml_cv", "ml_mprev"], ["ml_rw"], out=r3(1), in0=cv3, in1=mpb, op=ALU.subtract)
            k.v("dve", "tensor_tensor", ["ml_cv", mk], ["ml_rw"], out=r3(2), in0=cv3, in1=mlast.to_broadcast([4, NT, 128]), op=ALU.subtract)
            k.act(rw[:].rearrange("p a t -> p (a t)"), rw[:].rearrange("p a t -> p (a t)"), AF.Exp, ["ml_rw"], ["ml_rw"])
            k.v("dve", "tensor_tensor", [mk, "ml_mprev"], ["ml_ec"], out=ec[:].rearrange("p (c o) -> p c o", o=1),
                in0=mprev[:].rearrange("p (c o) -> p c o", o=1), in1=mlast, op=ALU.subtract)
            k.act(ec[:], ec[:], AF.Exp, ["ml_ec"], ["ml_ec"])
            k.v("dve", "tensor_tensor", [fk, mk, "ml_rw"], ["ml_cv"], out=cv[:], in0=Fc[:], in1=Mc[:], op=ALU.add)
            k.act(cv[:], cv[:], AF.Exp, ["ml_cv"], ["ml_cv"], scale=-1.0)
            for ti in range(NT):
                tg = hb * NT + ti
                b = (tg * 128) // BLK
                sub = tg % 4
                mb = mixb[b % 2]; mbk = ("ml_mix", b % 2)
                ts = slice(ti * 128, (ti + 1) * 128)
                i2 = tg % 2
                tk_ = tok[i2]; tkk = ("ml_tok", i2)
                for a in range(3):
                    k.tr(pS[:, 4 * a:4 * a + 4], rw[:, a, ts], k.c_identf[0:4, 0:4], ["ml_rw", "c_identf"], ["ml_pS"])
                k.tr(pS[:, 12:16], cv[:, ts], k.c_identf[0:4, 0:4], ["ml_cv", "c_identf"], ["ml_pS"])
                for h in range(4):
                    k.mm(pS[:, 16 + h:17 + h], selh[:, h, :], ec[:, ti:ti + 1], True, True, ["ml_selh", "ml_ec"], ["ml_pS"])
                tkb = P.sb if False else None
                k.copy("act", tk_[:, 0:12], pS[:, 0:12], ["ml_pS"], [tkk])
                eb_ = ecb[i2]; ebk = ("ml_ecb", i2)
                k.copy("dve", eb_[:, 0:4], pS[:, 16:20], ["ml_pS"], [ebk])
                en_ = sc[i2]; enk = ("ml_sc", i2)
                k.copy("dve", en_[:, 0:4], pS[:, 12:16], ["ml_pS"], [enk])
                for h in range(4):
                    u = cn["u"] % 2; cn["u"] += 1
                    slot = tg % 2
                    k.tr(pTk[:, 0:128], qkb[:, 4 + h, ts], k.c_ident[:], qkk, ["ml_pTk"])
                    k.v("dve", "tensor_scalar", ["ml_pTk", tkk], [("ml_ktm", u)], out=ktm[u][:], in0=pTk[:, 0:128],
                        scalar1=tk_[:, 8 + h:9 + h], scalar2=128 ** -0.5, op0=ALU.mult, op1=ALU.mult)
                    for half in range(2):
                        k.tr(pTv[:, half * 128:(half + 1) * 128], vb[:, 2 * h + half, ts], k.c_ident[:], ["ml_vb"], ["ml_pTv"])
                    k.copy("act", vx[u][:, 0:256], pTv[:, 0:256], ["ml_pTv"], [("ml_vx", u)])
                    k.v("pool", "memset", [("ml_vx", u)], [("ml_vx", u)], ap=vx[u][:, 256:257], constant=1.0)
                    k.mm(pA[:, 0:128], qkb[:, 4 + h, ts], qkb[:, h, ts], True, True, qkk, ["ml_pA"])
                    k.v("dve", "tensor_scalar", ["ml_pA", tkk], [("ml_aT", u)], out=aT[u][:], in0=pA[:, 0:128],
                        scalar1=tk_[:, 4 + h:5 + h], scalar2=128 ** -0.5, op0=ALU.mult, op1=ALU.mult)
                    k.v("pool", "tensor_tensor", [("ml_aT", u), "ml_mU"], [("ml_aT", u)], out=aT[u][:], in0=aT[u][:], in1=mU[:], op=ALU.mult)
                    io = cn["o"] % 2; cn["o"] += 1
                    po = pO[io]; pok = ("ml_pO", io)
                    k.mm(po[:, 0:257], qkb[:, h, ts], Cbf[:, h, slot, :], True, False, qkk + [("ml_Cbf", h, slot)], [pok])
                    k.mm(po[:, 0:257], aT[u][:], vx[u][:], False, True, [("ml_aT", u), ("ml_vx", u)], [pok])
                    k.mm(pC[:, 0:257], ktm[u][:], vx[u][:], True, True, [("ml_ktm", u), ("ml_vx", u)], ["ml_pC"])
                    k.v("dve", "scalar_tensor_tensor", ["ml_pC", ("ml_C32", h), ebk], [("ml_C32", h)], out=C32[:, h, :], in0=C32[:, h, :],
                        scalar=eb_[:, h:h + 1], in1=pC[:, 0:257], op0=ALU.mult, op1=ALU.add)
                    k.copy("act", Cbf[:, h, 1 - slot, :], C32[:, h, :], [("ml_C32", h)], [("ml_Cbf", h, 1 - slot)])
                    s_ = sc[i2]
                    k.act(s_[:, 4:5], po[:, 256:257], AF.Abs, [pok, tkk, enk], [enk], scale=tk_[:, h:h + 1])
                    k.v("dve", "tensor_tensor", [enk], [enk], out=s_[:, 5:6], in0=s_[:, 4:5], in1=en_[:, h:h + 1], op=ALU.max)
                    k.v("dve", "reciprocal", [enk], [enk], out=s_[:, 6:7], in_=s_[:, 5:6])
                    k.v("dve", "tensor_tensor", [enk, tkk], [enk], out=s_[:, 7:8], in0=s_[:, 6:7], in1=tk_[:, h:h + 1], op=ALU.mult)
                    k.v("dve", "tensor_scalar", [pok, enk], [("ml_hb", u)], out=hb_[u][:], in0=po[:, 0:256], scalar1=s_[:, 7:8], scalar2=None,
                        op0=ALU.mult)
                    for half in range(2):
                        k.tr(pTh[:, half * 128:(half + 1) * 128], hb_[u][:, half * 128:(half + 1) * 128], k.c_ident[:], [("ml_hb", u)], ["ml_pTh"])
                    k.v("dve", "tensor_tensor", ["ml_pTh", "ml_og"], [mbk], out=mb[:, 2 * h:2 * h + 2, sub * 128:(sub + 1) * 128],
                        in0=pTh[:, 0:256].rearrange("p (a c) -> p a c", c=128), in1=og[:, 2 * h:2 * h + 2, ts], op=ALU.mult)
                if sub == 3:
                    k.ld(k.A["mixT"][b][:, 8:16, :], mb[:], reads=[mbk], writes=[("D", "mixT", b, 1)], slot=("mlst", b % 2))


K.mlstm = _mlstm


def _dil(self, L, pT):
    k = self
    S = k.S
    SB = min(2048, S)
    NSB = S // SB
    PAIRS = [(1, 0), (4, 1), (16, 2)]
    if "Vtm" not in k.A:
        k.A["Vtm"] = k.dram("dl_Vtm", [S, 1024], BF16)
        k.A["OP"] = k.dram("dl_OP", [3, S, 16 * 66], F32)
    Vtm, OP = k.A["Vtm"], k.A["OP"]
    with k.phase() as P:
        vf = [P.sb(f"dv_vf{i}", [128, 8, BLK]) for i in range(2)]
        vb = [P.sb(f"dv_vb{i}", [128, 8, BLK], BF16) for i in range(2)]
        vt = [P.sb(f"dv_vt{i}", [128, 1024], BF16) for i in range(2)]
        pt = [P.ps(f"dv_pt{i}", [128, 1024], BF16) for i in range(2)]
        n = 0
        for b in range(k.NB):
            i = b % 2
            k.ld(vf[i][:], pT[16:24, :, b * BLK:(b + 1) * BLK].rearrange("c p t -> p c t"), reads=[("D", "pT", 16 + h, b) for h in range(8)],
                 writes=[("dv_vf", i)], slot=("dvld", i))
            k.act(vb[i][:].rearrange("p h t -> p (h t)"), vf[i][:].rearrange("p h t -> p (h t)"), AF.Copy, [("dv_vf", i)], [("dv_vb", i)])
            for sub in range(4):
                ii = n % 2; n += 1
                for hp in range(8):
                    k.tr(pt[ii][:, hp * 128:(hp + 1) * 128], vb[i][:, hp, sub * 128:(sub + 1) * 128], k.c_ident[:], [("dv_vb", i)], [("dv_pt", ii)])
                k.copy("dve", vt[ii][:], pt[ii][:], [("dv_pt", ii)], [("dv_vt", ii)])
                t0 = b * BLK + sub * 128
                k.ld(Vtm[t0:t0 + 128, :], vt[ii][:], reads=[("dv_vt", ii)], writes=[("D", "Vtm", t0 // 128)], slot=("dvst", ii))
    with k.phase() as P:
        maskb = P.sb("dl_maskb", [128, 256])
        k.v("pool", "memset", [], ["dl_maskb"], ap=maskb[:], constant=0.0)
        k.v("pool", "affine_select", ["dl_maskb"], ["dl_maskb"], out=maskb[:, 0:128], in_=maskb[:, 0:128], pattern=[[1, 128]],
            compare_op=ALU.is_ge, fill=-30000.0, base=0, channel_multiplier=-1)
        k.v("pool", "affine_select", ["dl_maskb"], ["dl_maskb"], out=maskb[:, 128:256], in_=maskb[:, 128:256], pattern=[[-1, 128]],
            compare_op=ALU.is_ge, fill=-30000.0, base=0, channel_multiplier=1)
        qsb = P.sb("dl_q", [128, 8, SB], BF16)
        ksb = [P.sb(f"dl_k{i}", [128, 8, SB], BF16) for i in range(2)]
        stg = [P.sb(f"dl_stg{i}", [128, 8, BLK]) for i in range(2)]
        vcur = [P.sb(f"dl_vc{i}", [128, 1024], BF16) for i in range(2)]
        vprv = [P.sb(f"dl_vp{i}", [128, 1024], BF16) for i in range(2)]
        ou = [P.sb(f"dl_ou{i}", [128, 16, 66]) for i in range(2)]
        ssb = [P.sb(f"dl_ssb{i}", [128, 256]) for i in range(2)]
        nmx = [P.sb(f"dl_nmx{i}", [128, 1]) for i in range(2)]
        pb = [P.sb(f"dl_pb{i}", [128, 256], BF16) for i in range(2)]
        ptb = [P.sb(f"dl_ptb{i}", [128, 256], BF16) for i in range(2)]
        pSc = [P.ps(f"dl_pSc{i}", [128, 512]) for i in range(2)]
        pPT = [P.ps(f"dl_pPT{i}", [128, 1024], BF16) for i in range(2)]
        pOv = [P.ps(f"dl_pOv{i}", [128, 512]) for i in range(2)]
        ns = 0; nu = 0; nh = 0
        for sb in range(NSB):
            kb = ksb[sb % 2]; kbk = ("dl_k", sb % 2)
            kp = ksb[(sb + 1) % 2]; kpk = ("dl_k", (sb + 1) % 2)
            for piece in range(SB // BLK):
                b = sb * (SB // BLK) + piece
                for (dst, dkey, c0, scale) in ((qsb, "dl_q", 0, 0.125), (kb, kbk, 8, 1.0)):
                    sg_ = stg[ns % 2]; sk = ("dl_stg", ns % 2); ns += 1
                    k.ld(sg_[:], pT[c0:c0 + 8, :, b * BLK:(b + 1) * BLK].rearrange("c p t -> p c t"),
                         reads=[("D", "pT", c0 + h, b) for h in range(8)], writes=[sk], slot=("dlld", ns % 2))
                    k.act(dst[:, :, piece * BLK:(piece + 1) * BLK], sg_[:], AF.Copy, [sk], [dkey], scale=scale)
            for (d, pi) in PAIRS:
                nblk = SB // (128 * d)
                qv = qsb[:].rearrange("p h (m i d) -> p h m i d", i=128, d=d)
                kcv = kb[:].rearrange("p h (m i d) -> p h m i d", i=128, d=d)
                kpv = kp[:].rearrange("p h (m i d) -> p h m i d", i=128, d=d)
                Vd = Vtm.rearrange("(n d) c -> d n c", d=d)
                Od = OP[pi].rearrange("(n d) c -> d n c", d=d)
                for r in range(d):
                    for m in range(nblk):
                        n0 = (sb * SB + m * 128 * d) // d
                        hasprev = not (sb == 0 and m == 0)
                        iu = nu % 2; nu += 1
                        vc = vcur[iu]; vck = ("dl_vc", iu)
                        vp = vprv[iu]; vpk = ("dl_vp", iu)
                        tiles = lambda n_: sorted(set(((n_ + i_) * d + r) // 128 for i_ in range(128)))
                        k.ld(vc[:], Vd[r, n0:n0 + 128, :], reads=[("D", "Vtm", t_) for t_ in tiles(n0)], writes=[vck], slot=("dlvc", iu))
                        if hasprev:
                            k.ld(vp[:], Vd[r, n0 - 128:n0, :], reads=[("D", "Vtm", t_) for t_ in tiles(n0 - 128)], writes=[vpk], slot=("dlvp", iu))
                        o_u = ou[iu]; ouk = ("dl_ou", iu)
                        W = 256 if hasprev else 128
                        c_lo = 0 if hasprev else 128
                        for hp in range(8):
                            for par in range(2):
                                h = 2 * hp + par
                                pr = slice(par * 64, (par + 1) * 64)
                                ih = nh % 2; nh += 1
                                psc = pSc[ih]; psk = ("dl_pSc", ih)
                                qT = qv[pr, hp, m, :, r]
                                if hasprev:
                                    kprev = kcv[pr, hp, m - 1, :, r] if m > 0 else kpv[pr, hp, nblk - 1, :, r]
                                    k.mm(psc[:, 0:128], qT, kprev, True, True, ["dl_q", kbk if m > 0 else kpk], [psk])
                                k.mm(psc[:, 128:256], qT, kcv[pr, hp, m, :, r], True, True, ["dl_q", kbk], [psk])
                                s_ = ssb[ih]; ssk = ("dl_ssb", ih)
                                k.v("dve", "tensor_tensor", [psk, "dl_maskb"], [ssk], out=s_[:, c_lo:256], in0=psc[:, c_lo:256], in1=maskb[:, c_lo:256], op=ALU.add)
                                k.v("dve", "tensor_reduce", [ssk], [ouk], out=o_u[:, h, 64:65], in_=s_[:, c_lo:256], axis=AX.X, op=ALU.max)
                                k.v("dve", "tensor_scalar", [ouk], [("dl_nmx", ih)], out=nmx[ih][:], in0=o_u[:, h, 64:65], scalar1=-1.0, scalar2=None, op0=ALU.mult)
                                p_ = pb[ih]; pbk = ("dl_pb", ih)
                                k.act(p_[:, c_lo:256], s_[:, c_lo:256], AF.Exp, [ssk, ("dl_nmx", ih)], [pbk, ouk], bias=nmx[ih][:, 0:1], accum_out=o_u[:, h, 65:66])
                                ppt = pPT[ih]; ppk = ("dl_pPT", ih)
                                if hasprev:
                                    k.tr(ppt[:, 0:128], p_[:, 0:128], k.c_ident[:], [pbk], [ppk])
                                k.tr(ppt[:, 128:256], p_[:, 128:256], k.c_ident[:], [pbk], [ppk])
                                pt_ = ptb[ih]; ptk = ("dl_ptb", ih)
                                k.copy("dve" if ih else "act", pt_[:, c_lo:256], ppt[:, c_lo:256], [ppk], [ptk])
                                pov = pOv[ih]; pok = ("dl_pOv", ih)
                                if hasprev:
                                    k.mm(pov[:, 0:64], pt_[:, 0:128], vp[:, h * 64:(h + 1) * 64], True, False, [ptk, vpk], [pok])
                                k.mm(pov[:, 0:64], pt_[:, 128:256], vc[:, h * 64:(h + 1) * 64], not hasprev, True, [ptk, vck], [pok])
                                k.copy("act", o_u[:, h, 0:64], pov[:, 0:64], [pok], [ouk])
                        k.ld(Od[r, n0:n0 + 128, :], o_u[:].rearrange("p h c -> p (h c)"), reads=[ouk],
                             writes=[("D", "OP", pi, t_) for t_ in tiles(n0)], slot=("dlou", iu))
    with k.phase() as P:
        o3 = [[P.sb(f"dc_o{p}_{i}", [128, 16, 66]) for p in range(3)] for i in range(2)]
        mall = P.sb("dc_mall", [128, 16]); wts = P.sb("dc_w", [128, 3, 16]); den = P.sb("dc_den", [128, 16]); tmp16 = P.sb("dc_t16", [128, 16])
        num = P.sb("dc_num", [128, 16, 64]); tmp = P.sb("dc_tmp", [128, 16, 64]); yb = P.sb("dc_yb", [128, 1024], BF16)
        mixb = [P.sb(f"dc_mix{i}", [128, 8, BLK], BF16) for i in range(2)]
        pt = [P.ps(f"dc_pt{i}", [128, 1024], BF16) for i in range(2)]
        b16 = lambda a: a.rearrange("p (h o) -> p h o", o=1).to_broadcast([128, 16, 64])
        for tg in range(S // 128):
            i = tg % 2
            b = tg // 4; sub = tg % 4
            mb = mixb[b % 2]; mbk = ("dc_mix", b % 2)
            oks = []
            for p in range(3):
                k.ld(o3[i][p][:].rearrange("p h c -> p (h c)"), OP[p, tg * 128:(tg + 1) * 128, :], reads=[("D", "OP", p, tg)],
                     writes=[("dc_o", p, i)], slot=("dcld", p, i))
                oks.append(("dc_o", p, i))
            mv = lambda p: o3[i][p][:, :, 64]
            lv = lambda p: o3[i][p][:, :, 65]
            k.v("dve", "tensor_tensor", oks, ["dc_mall"], out=mall[:], in0=mv(0), in1=mv(1), op=ALU.max)
            k.v("dve", "tensor_tensor", oks + ["dc_mall"], ["dc_mall"], out=mall[:], in0=mall[:], in1=mv(2), op=ALU.max)
            for p in range(3):
                k.v("dve", "tensor_tensor", oks + ["dc_mall"], ["dc_w"], out=wts[:, p, :], in0=mv(p), in1=mall[:], op=ALU.subtract)
            k.act(wts[:].rearrange("p a h -> p (a h)"), wts[:].rearrange("p a h -> p (a h)"), AF.Exp, ["dc_w"], ["dc_w"])
            k.v("dve", "tensor_tensor", oks + ["dc_w"], ["dc_den"], out=den[:], in0=wts[:, 0, :], in1=lv(0), op=ALU.mult)
            k.v("dve", "tensor_tensor", oks + ["dc_w"], ["dc_num"], out=num[:], in0=o3[i][0][:, :, 0:64], in1=b16(wts[:, 0, :]), op=ALU.mult)
            for p in (1, 2):
                k.v("dve", "tensor_tensor", oks + ["dc_w"], ["dc_t16"], out=tmp16[:], in0=wts[:, p, :], in1=lv(p), op=ALU.mult)
                k.v("dve", "tensor_tensor", ["dc_t16", "dc_den"], ["dc_den"], out=den[:], in0=den[:], in1=tmp16[:], op=ALU.add)
                k.v("pool", "tensor_tensor", oks + ["dc_w"], ["dc_tmp"], out=tmp[:], in0=o3[i][p][:, :, 0:64], in1=b16(wts[:, p, :]), op=ALU.mult)
                k.v("dve", "tensor_tensor", ["dc_tmp", "dc_num"], ["dc_num"], out=num[:], in0=num[:], in1=tmp[:], op=ALU.add)
            k.v("dve", "reciprocal", ["dc_den"], ["dc_den"], out=den[:], in_=den[:])
            k.v("dve", "tensor_tensor", ["dc_num", "dc_den"], ["dc_yb"], out=yb[:].rearrange("p (h c) -> p h c", c=64), in0=num[:], in1=b16(den[:]),
                op=ALU.mult)
            for hp in range(8):
                k.tr(pt[i][:, hp * 128:(hp + 1) * 128], yb[:, hp * 128:(hp + 1) * 128], k.c_ident[:], ["dc_yb"], [("dc_pt", i)])
            k.copy(k.evac_engine(), mb[:, :, sub * 128:(sub + 1) * 128], pt[i][:].rearrange("p (h c) -> p h c", c=128), [("dc_pt", i)], [mbk])
            if sub == 3:
                k.ld(k.A["mixT"][b][:, 0:8, :], mb[:], reads=[mbk], writes=[("D", "mixT", b, 0)], slot=("dcst", b % 2))


K.dil = _dil


def _even_mixer(self, L, xin):
    pT = self.inproj(L, 7424, 26, self.I["rwkv_mu"][L // 2])
    self.rwkv(L, pT)
    self.hgrn(L, pT)


def _odd_mixer(self, L, xin):
    pT = self.inproj(L, 6152, 0, None)
    self.dil(L, pT)
    self.mlstm(L, pT)


K.even_mixer = _even_mixer
K.odd_mixer = _odd_mixer

_INPUT_NAMES = ["ev_w_in", "ev_w_out", "rwkv_mu", "rwkv_w0", "rwkv_w2", "rwkv_a0", "rwkv_a2", "rwkv_g2", "rwkv_kk", "rwkv_ka",
                "rwkv_rk", "rwkv_lnx_w", "rwkv_lnx_b", "hgrn_lb", "hgrn_norm_w", "od_w_in", "od_w_out", "mlstm_conv_w",
                "mlstm_conv_b", "mlstm_b_i", "mlstm_b_f", "ln_w", "ln_b", "moe_wg", "moe_bg", "moe_we", "moe_be", "moe_w1",
                "moe_w3", "moe_w2"]


def make_in_maps(inputs, n_seq):
    f = lambda a: np.ascontiguousarray(np.asarray(a, dtype=np.float32))
    shared = {}
    for n in _INPUT_NAMES:
        a = f(inputs[n])
        if n == "rwkv_rk":
            a = a.reshape(a.shape[0], -1)
        if n == "moe_be":
            a = a.reshape(a.shape[0], -1)
        shared[n] = a
    x = f(inputs["x"])
    maps = []
    for c in range(n_seq):
        m = dict(shared)
        m["x"] = x[c]
        maps.append(m)
    return maps


def kernel(**inputs):
    x = np.asarray(inputs["x"])
    B, S, _ = x.shape
    NG = np.asarray(inputs["moe_wg"]).shape[-1]
    k = build(S, DEPTH, NG, mode="full")
    in_maps = make_in_maps(inputs, B)
    res = run_bass_kernel_spmd(k.nc, in_maps, core_ids=list(range(B)))
    out = np.stack([np.asarray(res.results[c]["out"], dtype=np.float32) for c in range(B)], axis=0)
    return out
```

```python
import numpy as np
from contextlib import ExitStack
import concourse.bass as bass
import concourse.mybir as mybir
from concourse.bass_utils import run_bass_kernel_spmd

F32 = mybir.dt.float32
BF16 = mybir.dt.bfloat16
AF = mybir.ActivationFunctionType
ALU = mybir.AluOpType
AX = mybir.AxisListType

D = 2048
KC = 16
BLK = 512
DEPTH = 4
DN_ALPHA = (2 * DEPTH) ** 0.25
LN_EPS = 1e-5
ENGS = ("pe", "act", "dve", "pool", "sp")
SPARSE_MOE = True
I32 = mybir.dt.int32


class Sched:
    def __init__(self, nc, stack):
        self.nc = nc
        self.stack = stack
        self.q = {e: [] for e in ENGS}
        self.cnt = {}
        self.sems = {}
        self.seen = {e: {} for e in ENGS}
        self.lastw = {}
        self.readers = {}
        for e in ("pe", "act", "dve", "pool"):
            self.sem(e)
        self.nops = 0
        self.slotmap = {}
        self.free = {}
        self.ndma = 0

    def slot_sem(self, slot, eng="sp"):
        cls = "sw" if eng == "pool" else "hw"
        key = (cls, slot)
        if key not in self.slotmap:
            fl = self.free.setdefault(cls, []) if isinstance(self.free, dict) else None
            if fl:
                sid = fl.pop()
            else:
                sid = f"dma{cls}{self.ndma}"
                self.ndma += 1
                self.sem(sid)
            self.slotmap[key] = sid
        return self.slotmap[key]

    def release_slots(self):
        for (cls, _), sid in self.slotmap.items():
            fl = self.free.setdefault(cls, [])
            if sid not in fl:
                fl.append(sid)
        self.slotmap = {}

    def sem(self, name):
        if name not in self.sems:
            self.sems[name] = self.stack.enter_context(self.nc.semaphore("s_" + str(name)))
            self.cnt[name] = 0
        return self.sems[name]

    def _deps(self, eng, reads, writes):
        need = {}

        def add(tok):
            s, v = tok
            if eng == "pe" and s == "pe":
                return
            if need.get(s, 0) < v:
                need[s] = v
        for k in reads:
            if k in self.lastw:
                add(self.lastw[k])
        for k in writes:
            if k in self.lastw:
                add(self.lastw[k])
            for t in self.readers.get(k, ()):
                add(t)
        waits = []
        for s, v in need.items():
            if self.seen[eng].get(s, 0) < v:
                self.seen[eng][s] = v
                waits.append((s, v))
        return waits

    def _commit(self, tok, reads, writes):
        for k in writes:
            self.lastw[k] = tok
            self.readers[k] = []
        for k in reads:
            if k in writes:
                continue
            lst = self.readers.setdefault(k, [])
            lst.append(tok)
            if len(lst) > 48:
                m = {}
                for s, v in lst:
                    m[s] = max(m.get(s, 0), v)
                self.readers[k] = list(m.items())

    def op(self, eng, fn, reads=(), writes=()):
        waits = self._deps(eng, reads, writes)
        self.cnt[eng] += 1
        tok = (eng, self.cnt[eng])
        self.q[eng].append((waits, fn, (eng, 1)))
        self._commit(tok, reads, writes)
        self.nops += 1
        return tok

    def dma(self, eng, slot, fn, reads=(), writes=()):
        slot = self.slot_sem(slot, eng)
        waits = self._deps(eng, reads, writes)
        self.cnt[slot] += 16
        tok = (slot, self.cnt[slot])
        self.q[eng].append((waits, fn, (slot, 16)))
        self._commit(tok, reads, writes)
        self.nops += 1
        return tok

    def barrier(self, engs=ENGS):
        for e in engs:
            waits = []
            for s, v in self.cnt.items():
                if v > 0 and self.seen[e].get(s, 0) < v:
                    self.seen[e][s] = v
                    waits.append((s, v))
            if waits:
                self.q[e].append((waits, None, None))

    def emit(self, block):
        nc = self.nc
        names = {"pe": "tensor", "act": "scalar", "dve": "vector", "pool": "gpsimd", "sp": "sync"}
        for e in ENGS:
            items = self.q[e]

            def body(engine, items=items):
                for waits, fn, inc in items:
                    for s, v in waits:
                        engine.wait_ge(self.sems[s], v)
                    if fn is not None:
                        fn(engine).then_inc(self.sems[inc[0]], inc[1])
            getattr(block, names[e])(body)


class K:
    def __init__(self, S, depth=DEPTH, n_groups=4, dbg=None):
        self.S = S
        self.NB = S // BLK
        self.depth = depth
        self.NG = n_groups
        self.NE = n_groups * 8
        self.dbg = dbg or ()
        self.nc = bass.Bass("TRN2", target_bir_lowering=False)
        self.st = ExitStack()
        self.sch = Sched(self.nc, self.st)
        self.rr = 0

    def dram(self, name, shape, dt, kind="Internal"):
        return self.nc.dram_tensor(name, list(shape), dt, kind=kind).ap()

    def phase(self):
        self.sch.barrier()
        return _Phase(self)

    def ld(self, out, in_, reads=(), writes=(), eng=None, slot=None):
        eng = eng or "sp"
        slot = slot or ("d_" + str(writes[0] if writes else reads[0]))
        return self.sch.dma(eng, slot, lambda e: e.dma_start(out=out, in_=in_), reads=reads, writes=writes)

    def mm(self, out, lhsT, rhs, start, stop, reads, writes):
        return self.sch.op("pe", lambda e: e.matmul(out, lhsT=lhsT, rhs=rhs, start=start, stop=stop),
                           reads=reads, writes=writes)

    def tr(self, out, in_, ident, reads, writes):
        return self.sch.op("pe", lambda e: e.transpose(out=out, in_=in_, identity=ident), reads=reads, writes=writes)

    def act(self, out, in_, func, reads, writes, scale=None, bias=None, accum_out=None):
        kw = {}
        if scale is not None:
            kw["scale"] = scale
        if bias is not None:
            kw["bias"] = bias
        if accum_out is not None:
            kw["accum_out"] = accum_out
        return self.sch.op("act", lambda e: e.activation(out=out, in_=in_, func=func, **kw), reads=reads, writes=writes)

    def v(self, eng, name, reads, writes, **kw):
        return self.sch.op(eng, lambda e: getattr(e, name)(**kw), reads=reads, writes=writes)

    def evac_engine(self):
        self.rr += 1
        return "dve" if self.rr % 2 else "act"

    def copy(self, eng, out, in_, reads, writes):
        if eng == "act":
            return self.sch.op("act", lambda e: e.copy(out=out, in_=in_), reads=reads, writes=writes)
        return self.sch.op(eng, lambda e: e.tensor_copy(out=out, in_=in_), reads=reads, writes=writes)


class _Phase:
    def __init__(self, k):
        self.k = k

    def __enter__(self):
        self.stack = ExitStack()
        self.stack.__enter__()
        k = self.k
        nc = k.nc
        k.nphase = getattr(k, "nphase", 0) + 1
        pf = f"p{k.nphase}_"
        self.sb = lambda name, shape, dt=F32: self.stack.enter_context(nc.sbuf_tensor(pf + name, list(shape), dt))
        self.ps = lambda name, shape, dt=F32: self.stack.enter_context(nc.psum_tensor(pf + name, list(shape), dt))
        return self

    def __exit__(self, *a):
        self.k.sch.barrier()
        self.k.sch.release_slots()
        self.k.sch.lastw = {kk: vv for kk, vv in self.k.sch.lastw.items() if isinstance(kk, tuple) and kk and kk[0] == "D"}
        self.k.sch.readers = {kk: vv for kk, vv in self.k.sch.readers.items() if isinstance(kk, tuple) and kk and kk[0] == "D"}
        return self.stack.__exit__(*a)


def _declare(self):
    k = self
    S, NG, NE = k.S, k.NG, k.NE
    I = {}
    ext = lambda n, sh: k.dram(n, sh, F32, kind="ExternalInput")
    I["x"] = ext("x", [S, D])
    I["ev_w_in"] = ext("ev_w_in", [2, D, 7424])
    I["ev_w_out"] = ext("ev_w_out", [2, D, D])
    I["rwkv_mu"] = ext("rwkv_mu", [2, 3328])
    I["rwkv_w0"] = ext("rwkv_w0", [2, 1024])
    I["rwkv_w2"] = ext("rwkv_w2", [2, 64, 1024])
    I["rwkv_a0"] = ext("rwkv_a0", [2, 1024])
    I["rwkv_a2"] = ext("rwkv_a2", [2, 64, 1024])
    I["rwkv_g2"] = ext("rwkv_g2", [2, 128, 1024])
    I["rwkv_kk"] = ext("rwkv_kk", [2, 1024])
    I["rwkv_ka"] = ext("rwkv_ka", [2, 1024])
    I["rwkv_rk"] = ext("rwkv_rk", [2, 1024])
    I["rwkv_lnx_w"] = ext("rwkv_lnx_w", [2, 1024])
    I["rwkv_lnx_b"] = ext("rwkv_lnx_b", [2, 1024])
    I["hgrn_lb"] = ext("hgrn_lb", [2, 1024])
    I["hgrn_norm_w"] = ext("hgrn_norm_w", [2, 128])
    I["od_w_in"] = ext("od_w_in", [2, D, 6152])
    I["od_w_out"] = ext("od_w_out", [2, D, D])
    I["mlstm_conv_w"] = ext("mlstm_conv_w", [2, 4, 1024])
    I["mlstm_conv_b"] = ext("mlstm_conv_b", [2, 1024])
    I["mlstm_b_i"] = ext("mlstm_b_i", [2, 4])
    I["mlstm_b_f"] = ext("mlstm_b_f", [2, 4])
    I["ln_w"] = ext("ln_w", [DEPTH, 2, D])
    I["ln_b"] = ext("ln_b", [DEPTH, 2, D])
    I["moe_wg"] = ext("moe_wg", [DEPTH, D, NG])
    I["moe_bg"] = ext("moe_bg", [DEPTH, NG])
    I["moe_we"] = ext("moe_we", [DEPTH, NG, D, 8])
    I["moe_be"] = ext("moe_be", [DEPTH, NG * 8])
    I["moe_w1"] = ext("moe_w1", [DEPTH, NE, D, 512])
    I["moe_w3"] = ext("moe_w3", [DEPTH, NE, D, 512])
    I["moe_w2"] = ext("moe_w2", [DEPTH, NE, 512, D])
    k.I = I
    k.out = k.dram("out", [S, D], F32, kind="ExternalOutput")
    W = {}
    for L in k.layers:
        j = L // 2
        if L % 2 == 0:
            W["in", L] = k.dram(f"wbin{L}", [128, KC, 7424], BF16)
        else:
            W["in", L] = k.dram(f"wbin{L}", [128, KC, 6152], BF16)
        W["out", L] = k.dram(f"wbout{L}", [128, KC, D], BF16)
        W["w1", L] = k.dram(f"w1b{L}", [NE, 128, KC, 512], BF16)
        W["w3", L] = k.dram(f"w3b{L}", [NE, 128, KC, 512], BF16)
        W["w2", L] = k.dram(f"w2b{L}", [NE, 128, 4, D], BF16)
        W["wr", L] = k.dram(f"wrb{L}", [128, KC, NG + NE], BF16)
    k.W = W
    NB = k.NB
    A = {}
    A["xa"] = k.dram("xres_a", [S, D], F32)
    A["xb"] = k.dram("xres_b", [S, D], F32)
    A["xT"] = k.dram("xT", [NB, 128, KC, BLK], BF16)
    A["x1"] = k.dram("x1", [S, D], F32)
    A["x1T"] = k.dram("x1T", [NB, 128, KC, BLK], BF16)
    A["mixT"] = k.dram("mixT", [NB, 128, KC, BLK], BF16)
    A["gates"] = k.dram("gates", [S, NE], F32)
    A["x1b"] = k.dram("x1b", [S, D], BF16)
    k.NBLK = (2 * S + NE * 127 + 127) // 128
    A["xs"] = k.dram("xsorted", [k.NBLK * 128, D], BF16)
    A["ys"] = [k.dram(f"ysorted{i}", [k.NBLK * 128, D // 2], F32) for i in range(2)]
    A["rank"] = k.dram("rank", [S, NE], F32)
    A["pgt"] = k.dram("pgt", [S, 4], F32)
    A["widx"] = k.dram("widx", [128, k.NBLK], mybir.dt.int32)
    k.A = A


def _precast(self):
    k = self
    sch = k.sch

    def cast(out, in_, key, slow=False):
        sch.dma("pool", "cast", lambda e: e.dma_start(out=out, in_=in_, allow_slow_non_contiguous=slow), writes=[key])
    for L in k.layers:
        j = L // 2
        win = k.I["ev_w_in" if L % 2 == 0 else "od_w_in"][j].rearrange("(kc p) c -> p kc c", p=128)
        C = 7424 if L % 2 == 0 else 6152
        c0 = 0
        while c0 < C:
            c1 = min(C, c0 + 1024)
            cast(k.W["in", L][:, :, c0:c1], win[:, :, c0:c1], ("D", "win", L, c0))
            c0 = c1
        k.win_keys = getattr(k, "win_keys", {})
        k.win_keys[L] = [("D", "win", L, c) for c in range(0, C, 1024)]
        wout = k.I["ev_w_out" if L % 2 == 0 else "od_w_out"][j].rearrange("(kc p) c -> p kc c", p=128)
        for q in range(2):
            cast(k.W["out", L][:, :, q * 1024:(q + 1) * 1024], wout[:, :, q * 1024:(q + 1) * 1024], ("D", "wout", L, q))
        NG, NE = k.NG, k.NE
        cast(k.W["wr", L][:, :, 0:NG], k.I["moe_wg"][L].rearrange("(kc p) c -> p kc c", p=128), ("D", "wr", L, 0), True)
        for g in range(NG):
            cast(k.W["wr", L][:, :, NG + 8 * g:NG + 8 * g + 8],
                 k.I["moe_we"][L, g].rearrange("(kc p) c -> p kc c", p=128), ("D", "wr", L, 1 + g), True)
        for e in range(NE):
            cast(k.W["w1", L][e], k.I["moe_w1"][L, e].rearrange("(kc p) c -> p kc c", p=128), ("D", "w1", L, e))
            cast(k.W["w3", L][e], k.I["moe_w3"][L, e].rearrange("(kc p) c -> p kc c", p=128), ("D", "w3", L, e))
            cast(k.W["w2", L][e], k.I["moe_w2"][L, e].rearrange("(kc p) c -> p kc c", p=128), ("D", "w2", L, e))


def _consts(self):
    k = self
    nc = k.nc
    sb = lambda name, shape, dt=F32: k.st.enter_context(nc.sbuf_tensor(name, list(shape), dt))
    k.c_onesf = sb("c_onesf", [128, 128])
    k.c_ident = sb("c_ident", [128, 128], BF16)
    k.c_identf = sb("c_identf", [128, 128])
    k.v("pool", "memset", [], ["c_onesf"], ap=k.c_onesf[:], constant=1.0)
    k.v("pool", "memset", [], ["c_identf"], ap=k.c_identf[:], constant=1.0)
    k.v("pool", "affine_select", ["c_identf"], ["c_identf"], out=k.c_identf[:], in_=k.c_identf[:], pattern=[[-1, 128]],
        compare_op=ALU.is_equal, fill=0.0, base=0, channel_multiplier=1)
    k.copy("dve", k.c_ident[:], k.c_identf[:], ["c_identf"], ["c_ident"])
    k.c_eps = sb("c_eps", [128, 4])
    for i, val in enumerate((LN_EPS, 1.0, 64e-5, 1e-6)):
        k.v("pool", "memset", [], ["c_eps"], ap=k.c_eps[:, i:i + 1], constant=val)


def _x0(self):
    k = self
    with k.phase() as P:
        xb = [P.sb(f"x0_xb{i}", [128, D], BF16) for i in range(2)]
        blk = [P.sb(f"x0_blk{i}", [128, KC, BLK], BF16) for i in range(2)]
        pt = [P.ps(f"x0_pt{i}", [128, KC, 128], BF16) for i in range(2)]
        n = 0
        for b in range(k.NB):
            bb = blk[b % 2]
            for sub in range(4):
                t0 = b * BLK + sub * 128
                xt = xb[n % 2]
                p = pt[n % 2]
                k.sch.dma("pool", ("x0ld", n % 2), lambda e, xt=xt, t0=t0: e.dma_start(out=xt[:], in_=k.I["x"][t0:t0 + 128, :]),
                          writes=[("x0_xb", n % 2)])
                for kc in range(KC):
                    k.tr(p[:, kc, :], xt[:, kc * 128:(kc + 1) * 128], k.c_ident[:], [("x0_xb", n % 2), "c_ident"], [("x0_pt", n % 2)])
                k.copy(k.evac_engine(), bb[:, :, sub * 128:(sub + 1) * 128], p[:], [("x0_pt", n % 2)], [("x0_blk", b % 2)])
                n += 1
            k.ld(k.A["xT"][b], bb[:], reads=[("x0_blk", b % 2)], writes=[("D", "xT", b)], slot=("x0st", b % 2))


K.declare = _declare
K.precast = _precast
K.consts = _consts
K.x0 = _x0


def _ln_tail(k, P, T, z, zk, lnw, lnb, out_tm, t0, blkbuf, blkkey, sub, n, tm_bf=None):
    st = T["stats"][n % 2]
    mv = T["mv"][n % 2]
    sk, mk = ("ln_st", n % 2), ("ln_mv", n % 2)
    for c in range(4):
        k.v("dve", "bn_stats", [zk], [sk], out=st[:, c, :], in_=z[:, c * 512:(c + 1) * 512])
    k.v("dve", "bn_aggr", [sk], [mk], out=mv[:, 0:2], in_=st[:].rearrange("p a b -> p (a b)"))
    k.act(mv[:, 3:4], mv[:, 1:2], AF.Sqrt, [mk], [mk], bias=k.c_eps[:, 0:1])
    k.v("dve", "reciprocal", [mk], [mk], out=mv[:, 2:3], in_=mv[:, 3:4])
    k.v("dve", "tensor_scalar", [zk, mk], [zk], out=z[:], in0=z[:], scalar1=mv[:, 0:1], scalar2=mv[:, 2:3],
        op0=ALU.subtract, op1=ALU.mult)
    k.v("pool", "tensor_tensor", [zk, "lnw"], [zk], out=z[:], in0=z[:], in1=lnw[:], op=ALU.mult)
    k.v("pool", "tensor_tensor", [zk, "lnb"], [zk], out=z[:], in0=z[:], in1=lnb[:], op=ALU.add)
    k.ld(out_tm[t0:t0 + 128, :], z[:], reads=[zk], writes=[("D", "tm", id(out_tm), t0)], slot=("lnst", n % 2))
    zb = T["zb"][n % 2]
    zbk = ("ln_zb", n % 2)
    k.act(zb[:], z[:], AF.Copy, [zk], [zbk])
    if tm_bf is not None:
        k.ld(tm_bf[t0:t0 + 128, :], zb[:], reads=[zbk], writes=[("D", "x1b", t0)], slot=("lnstb", n % 2))
    pt = T["pt"][n % len(T["pt"])]
    ptk = ("ln_pt", n % len(T["pt"]))
    for kc in range(KC):
        k.tr(pt[:, kc, :], zb[:, kc * 128:(kc + 1) * 128], k.c_ident[:], [zbk], [ptk])
    k.copy(k.evac_engine(), blkbuf[:, :, sub * 128:(sub + 1) * 128], pt[:], [ptk], [blkkey])


def _ln_alloc(P, npt=2):
    T = {}
    T["stats"] = [P.sb(f"ln_stats{i}", [128, 4, 6]) for i in range(2)]
    T["mv"] = [P.sb(f"ln_mv{i}", [128, 4]) for i in range(2)]
    T["zb"] = [P.sb(f"ln_zb{i}", [128, D], BF16) for i in range(2)]
    T["pt"] = [P.ps(f"ln_pt{i}", [128, KC, 128], BF16) for i in range(npt)]
    return T


def _phase_o(self, L, xin):
    k = self
    NG, NE = k.NG, k.NE
    NR = NG + NE
    with k.phase() as P:
        T = _ln_alloc(P)
        wout = P.sb("o_wout", [128, KC, D], BF16)
        wr = P.sb("o_wr", [128, KC, NR], BF16)
        lnw = P.sb("o_lnw", [128, D]); lnb = P.sb("o_lnb", [128, D])
        rb = P.sb("o_rb", [128, NR])
        mix = [P.sb(f"o_mix{i}", [128, KC, BLK], BF16) for i in range(2)]
        blk = [P.sb(f"o_blk{i}", [128, KC, BLK], BF16) for i in range(2)]
        xr = [P.sb(f"o_xr{i}", [128, D]) for i in range(2)]
        z = [P.sb(f"o_z{i}", [128, D]) for i in range(2)]
        rt = [P.sb(f"o_rt{i}", [128, 64 + 3 * NE]) for i in range(2)]
        po = [P.ps(f"o_po{i}", [128, 512]) for i in range(2)]
        pr = P.ps("o_pr", [128, 512])
        k.ld(wout[:, :, 0:1024], k.W["out", L][:, :, 0:1024], reads=[("D", "wout", L, 0)], writes=["wout0"])
        k.ld(wout[:, :, 1024:2048], k.W["out", L][:, :, 1024:2048], reads=[("D", "wout", L, 1)], writes=["wout1"])
        k.ld(wr[:], k.W["wr", L], reads=[("D", "wr", L, g) for g in range(NG + 1)], writes=["wr"])
        k.ld(lnw[:], k.I["ln_w"][L, 0:1, :].partition_broadcast(128), writes=["lnw"])
        k.ld(lnb[:], k.I["ln_b"][L, 0:1, :].partition_broadcast(128), writes=["lnb"])
        k.ld(rb[:, 0:NG], k.I["moe_bg"][L:L + 1, :].partition_broadcast(128), writes=["rb0"])
        k.ld(rb[:, NG:NR], k.I["moe_be"][L:L + 1, :].partition_broadcast(128), writes=["rb1"])
        n = 0
        npo = 0
        for b in range(k.NB):
            mb = mix[b % 2]
            k.ld(mb[:], k.A["mixT"][b], reads=[("D", "mixT", b, 0), ("D", "mixT", b, 1)], writes=[("o_mix", b % 2)], slot=("omix", b % 2))
            bb = blk[b % 2]
            bk = ("o_blk", b % 2)
            for sub in range(4):
                t0 = b * BLK + sub * 128
                x_t = xr[n % 2]; zt = z[n % 2]
                xk, zk = ("o_xr", n % 2), ("o_z", n % 2)
                k.ld(x_t[:], xin[t0:t0 + 128, :], reads=[("D", "tm", id(xin), t0)], writes=[xk], slot=("oxr", n % 2))
                for dc in range(4):
                    pp = po[npo % 2]; pk = ("o_po", npo % 2)
                    for kc in range(KC):
                        k.mm(pp[:], mb[:, kc, sub * 128:(sub + 1) * 128], wout[:, kc, dc * 512:(dc + 1) * 512],
                             kc == 0, kc == KC - 1, [("o_mix", b % 2), "wout0", "wout1"], [pk])
                    k.v("dve", "scalar_tensor_tensor", [xk, pk], [zk], out=zt[:, dc * 512:(dc + 1) * 512],
                        in0=x_t[:, dc * 512:(dc + 1) * 512], scalar=DN_ALPHA, in1=pp[:], op0=ALU.mult, op1=ALU.add)
                    npo += 1
                _ln_tail(k, P, T, zt, zk, lnw, lnb, k.A["x1"], t0, bb, bk, sub, n, tm_bf=k.A["x1b"])
                r = rt[n % 2]; rk = ("o_rt", n % 2)
                for kc in range(KC):
                    k.mm(pr[:, 0:NR], bb[:, kc, sub * 128:(sub + 1) * 128], wr[:, kc, :], kc == 0, kc == KC - 1,
                         [bk, "wr"], ["o_pr"])
                lg = r[:, 0:NR]
                k.v("dve", "tensor_tensor", ["o_pr", "rb0", "rb1"], [rk], out=lg, in0=pr[:, 0:NR], in1=rb[:], op=ALU.add)
                sc = r[:, NR:NR + 16]
                gmax, gsum, pg, v1, v2, dd, c1, c2 = [sc[:, i:i + 1] for i in range(8)]
                gexp = r[:, NR + 16:NR + 16 + NG]
                gmask = r[:, NR + 20:NR + 20 + NG]
                o0 = 64
                ml = r[:, o0:o0 + NE]
                top = r[:, o0 + NE:o0 + NE + 8]
                gt = r[:, o0 + 2 * NE:o0 + 3 * NE]
                if NG > 1:
                    k.v("dve", "tensor_reduce", [rk], [rk], out=gmax, in_=r[:, 0:NG], axis=AX.X, op=ALU.max)
                else:
                    k.v("dve", "tensor_copy", [rk], [rk], out=gmax, in_=r[:, 0:1])
                k.v("dve", "tensor_scalar", [rk], [rk], out=gexp, in0=r[:, 0:NG], scalar1=gmax, scalar2=None, op0=ALU.subtract)
                k.act(gexp, gexp, AF.Exp, [rk], [rk], accum_out=gsum)
                k.v("dve", "reciprocal", [rk], [rk], out=pg, in_=gsum)
                k.v("dve", "tensor_scalar", [rk], [rk], out=gmask, in0=r[:, 0:NG], scalar1=gmax, scalar2=-1.0,
                    op0=ALU.is_equal, op1=ALU.add)
                k.v("dve", "scalar_tensor_tensor", [rk], [rk], out=ml.rearrange("p (g e) -> p g e", e=8),
                    in0=gmask.rearrange("p (g o) -> p g o", o=1).to_broadcast([128, NG, 8]), scalar=1e30,
                    in1=r[:, NG:NR].rearrange("p (g e) -> p g e", e=8), op0=ALU.mult, op1=ALU.add)
                k.v("dve", "max", [rk], [rk], out=top, in_=ml)
                k.v("dve", "tensor_tensor", [rk], [rk], out=dd, in0=top[:, 1:2], in1=top[:, 0:1], op=ALU.subtract)
                k.act(dd, dd, AF.Exp, [rk], [rk])
                k.v("dve", "tensor_scalar", [rk], [rk], out=dd, in0=dd, scalar1=1.0, scalar2=None, op0=ALU.add)
                k.v("dve", "reciprocal", [rk], [rk], out=dd, in_=dd)
                k.v("dve", "tensor_tensor", [rk], [rk], out=c1, in0=dd, in1=pg, op=ALU.mult)
                k.v("dve", "tensor_tensor", [rk], [rk], out=c2, in0=pg, in1=c1, op=ALU.subtract)
                k.v("dve", "tensor_scalar", [rk], [rk], out=gt, in0=ml, scalar1=top[:, 0:1], scalar2=c1,
                    op0=ALU.is_equal, op1=ALU.mult)
                k.v("dve", "tensor_scalar", [rk], [rk], out=ml, in0=ml, scalar1=top[:, 1:2], scalar2=c2,
                    op0=ALU.is_equal, op1=ALU.mult)
                k.v("dve", "tensor_tensor", [rk], [rk], out=gt, in0=gt, in1=ml, op=ALU.add)
                k.ld(k.A["gates"][t0:t0 + 128, :], gt, reads=[rk], writes=[("D", "gates", t0)], slot=("ogt", n % 2))
                n += 1
            k.ld(k.A["x1T"][b], bb[:], reads=[bk], writes=[("D", "x1T", b)], slot=("ox1T", b % 2))


def _phase_m(self, L, xout):
    k = self
    NE = k.NE
    with k.phase() as P:
        T = _ln_alloc(P, 1)
        lnw = P.sb("m_lnw", [128, D]); lnb = P.sb("m_lnb", [128, D])
        xb = [P.sb(f"m_xb{i}", [128, KC, BLK], BF16) for i in range(1)]
        blk = [P.sb(f"m_blk{i}", [128, KC, BLK], BF16) for i in range(1)]
        gt = [P.sb(f"m_gt{i}", [128, 4, NE]) for i in range(2)]
        yacc = P.sb("m_yacc", [128, 4, D])
        w1 = [P.sb(f"m_w1_{i}", [128, KC, 512], BF16) for i in range(2)]
        w3 = [P.sb(f"m_w3_{i}", [128, KC, 512], BF16) for i in range(2)]
        w2 = [P.sb(f"m_w2_{i}", [128, 4, D], BF16) for i in range(2)]
        h = [P.sb(f"m_h{i}", [128, 4, BLK], BF16) for i in range(2)]
        sl = [P.sb(f"m_sl{i}", [128, BLK]) for i in range(2)]
        xr = [P.sb(f"m_xr{i}", [128, D]) for i in range(1)]
        p1 = [P.ps(f"m_p1_{i}", [128, 512]) for i in range(2)]
        p3 = [P.ps(f"m_p3_{i}", [128, 512]) for i in range(2)]
        po = [P.ps(f"m_po{i}", [128, 512]) for i in range(2)]
        k.ld(lnw[:], k.I["ln_w"][L, 1:2, :].partition_broadcast(128), writes=["lnw"])
        k.ld(lnb[:], k.I["ln_b"][L, 1:2, :].partition_broadcast(128), writes=["lnb"])
        n = 0; ne = 0; nf = 0; npo = 0
        for b in range(k.NB):
            x_b = xb[0]; xbk = ("m_xb", 0)
            k.ld(x_b[:], k.A["x1T"][b], reads=[("D", "x1T", b)], writes=[xbk], slot=("mxb", 0))
            g = gt[b % 2]; gk = ("m_gt", b % 2)
            k.ld(g[:], k.A["gates"][b * BLK:(b + 1) * BLK, :].rearrange("(s p) e -> p s e", p=128),
                 reads=[("D", "gates", b * BLK + s * 128) for s in range(4)], writes=[gk], slot=("mgt", b % 2))
            for e in range(NE):
                i = ne % 2
                k.ld(w1[i][:], k.W["w1", L][e], reads=[("D", "w1", L, e)], writes=[("m_w1", i)], slot=("mw1", i))
                k.ld(w3[i][:], k.W["w3", L][e], reads=[("D", "w3", L, e)], writes=[("m_w3", i)], slot=("mw3", i))
                k.ld(w2[i][:], k.W["w2", L][e], reads=[("D", "w2", L, e)], writes=[("m_w2", i)], slot=("mw2", i))
                hh = h[i]; hk = ("m_h", i)
                for fc in range(4):
                    a1 = p1[nf % 2]; a3 = p3[nf % 2]
                    k1, k3 = ("m_p1", nf % 2), ("m_p3", nf % 2)
                    for kc in range(KC):
                        k.mm(a1[:], w1[i][:, kc, fc * 128:(fc + 1) * 128], x_b[:, kc, :], kc == 0, kc == KC - 1,
                             [("m_w1", i), xbk], [k1])
                    for kc in range(KC):
                        k.mm(a3[:], w3[i][:, kc, fc * 128:(fc + 1) * 128], x_b[:, kc, :], kc == 0, kc == KC - 1,
                             [("m_w3", i), xbk], [k3])
                    s_ = sl[nf % 2]; sk = ("m_sl", nf % 2)
                    k.act(s_[:], a1[:], AF.Silu, [k1], [sk])
                    k.v("dve", "tensor_tensor", [sk, k3], [hk], out=hh[:, fc, :], in0=s_[:], in1=a3[:], op=ALU.mult)
                    nf += 1
                for sub in range(4):
                    for dc in range(4):
                        pp = po[npo % 2]; pk = ("m_po", npo % 2)
                        for fc in range(4):
                            k.mm(pp[:], hh[:, fc, sub * 128:(sub + 1) * 128], w2[i][:, fc, dc * 512:(dc + 1) * 512],
                                 fc == 0, fc == 3, [hk, ("m_w2", i)], [pk])
                        ya = yacc[:, sub, dc * 512:(dc + 1) * 512]
                        yk = ("m_yacc", sub, dc)
                        if e == 0:
                            k.v("dve", "tensor_scalar", [pk, gk], [yk], out=ya, in0=pp[:], scalar1=g[:, sub, e:e + 1],
                                scalar2=None, op0=ALU.mult)
                        else:
                            k.v("dve", "scalar_tensor_tensor", [pk, gk, yk], [yk], out=ya, in0=pp[:],
                                scalar=g[:, sub, e:e + 1], in1=ya, op0=ALU.mult, op1=ALU.add)
                        npo += 1
                ne += 1
            bb = blk[0]; bk = ("m_blk", 0)
            for sub in range(4):
                t0 = b * BLK + sub * 128
                x_t = xr[0]; xk = ("m_xr", 0)
                k.ld(x_t[:], k.A["x1"][t0:t0 + 128, :], reads=[("D", "tm", id(k.A["x1"]), t0)], writes=[xk], slot=("mxr", 0))
                yks = [("m_yacc", sub, dc) for dc in range(4)]
                k.v("dve", "scalar_tensor_tensor", [xk] + yks, [xk], out=x_t[:], in0=x_t[:], scalar=DN_ALPHA,
                    in1=yacc[:, sub, :], op0=ALU.mult, op1=ALU.add)
                _ln_tail(k, P, T, x_t, xk, lnw, lnb, xout, t0, bb, bk, sub, n)
                n += 1
            k.ld(k.A["xT"][b], bb[:], reads=[bk], writes=[("D", "xT", b)], slot=("mxT", 0))


K.phase_o = _phase_o
K.phase_m = _phase_m


def build(S, depth=DEPTH, n_groups=4, mode="full", dbg=()):
    k = K(S, depth, n_groups, dbg)
    k.layers = list(range(depth))
    if mode in ("hgrn", "rwkv", "even", "odd", "dil", "mlstm"):
        k.layers = [(1 if mode in ("odd", "dil", "mlstm") else 0) + 2 * (depth - 1)]
    k.declare()
    k.precast()
    k.consts()
    k.x0()
    if mode in ("hgrn", "rwkv", "even", "odd", "dil", "mlstm"):
        L = 1 if mode in ("odd", "dil", "mlstm") else 0
        L += 2 * (depth - 1)
        dbgo = k.dram("dbg_mixT", [k.NB, 128, KC, BLK], BF16, kind="ExternalOutput")
        if L % 2 == 0:
            pT = k.inproj(L, 7424, 26, k.I["rwkv_mu"][L // 2])
            if mode in ("rwkv", "even"):
                k.rwkv(L, pT)
            if mode in ("hgrn", "even"):
                k.hgrn(L, pT)
        else:
            pT = k.inproj(L, 6152, 0, None)
            if mode in ("dil", "odd"):
                k.dil(L, pT)
            if mode in ("mlstm", "odd"):
                k.mlstm(L, pT)
        k.sch.barrier()
        halves = {"hgrn": [(8, 16)], "mlstm": [(8, 16)], "rwkv": [(0, 8)], "dil": [(0, 8)]}.get(mode, [(0, 16)])
        for b in range(k.NB):
            for (h0, h1) in halves:
                k.sch.dma("sp", "dbgst", lambda e, b=b, h0=h0, h1=h1: e.dma_start(out=dbgo[b][:, h0:h1, :], in_=k.A["mixT"][b][:, h0:h1, :]))
        k.sch.barrier()
        with k.nc.Block() as block:
            k.sch.emit(block)
        return k
    xin = k.I["x"]
    for L in range(depth):
        last = L == depth - 1
        xout = k.out if last else (k.A["xa"] if L % 2 == 0 else k.A["xb"])
        if mode == "tokenlocal":
            k.A["mixT"] = k.A["xT"]
            for b in range(k.NB):
                if ("D", "xT", b) in k.sch.lastw:
                    k.sch.lastw[("D", "mixT", b, 0)] = k.sch.lastw[("D", "xT", b)]
                    k.sch.lastw[("D", "mixT", b, 1)] = k.sch.lastw[("D", "xT", b)]
        else:
            if L % 2 == 0:
                k.even_mixer(L, xin)
            else:
                k.odd_mixer(L, xin)
        k.phase_o(L, xin)
        if SPARSE_MOE:
            k.phase_s(L)
            k.phase_m2(L)
            k.phase_m3(L, xout)
        else:
            k.phase_m(L, xout)
        xin = xout
    k.sch.barrier()
    with k.nc.Block() as block:
        k.sch.emit(block)
    return k


class _PT:
    def __init__(self, groups):
        self.groups = groups

    def __getitem__(self, idx):
        c = idx[0]
        rest = tuple(idx[1:])
        if isinstance(c, slice):
            c0, c1 = c.start, c.stop
            for g0, g1, ap in self.groups:
                if g0 <= c0 and c1 <= g1:
                    return ap[(slice(c0 - g0, c1 - g0),) + rest]
            raise KeyError((c0, c1))
        for g0, g1, ap in self.groups:
            if g0 <= c < g1:
                return ap[(c - g0,) + rest]
        raise KeyError(c)


def _inproj(self, L, ncols, nlerp, mu_ap):
    k = self
    NCH = (ncols + 127) // 128
    S = k.S
    if ("pT", NCH) not in k.A:
        bounds = [0, 8, 16, 24, 26, 34, 42, 50, 58] if NCH == 58 else [0, 8, 16, 24, 32, 40, 48, 49]
        k.A["pT", NCH] = _PT([(g0, g1, k.dram(f"pT{NCH}_{g0}", [g1 - g0, 128, S], F32)) for g0, g1 in zip(bounds[:-1], bounds[1:])])
    pT = k.A["pT", NCH]
    with k.phase() as P:
        xb = [P.sb(f"ip_xb{i}", [128, KC, BLK], BF16) for i in range(2)]
        w = [P.sb(f"ip_w{i}", [128, KC, 512], BF16) for i in range(2)]
        raw = [P.sb(f"ip_raw{i}", [128, 520]) for i in range(2)]
        ob = [P.sb(f"ip_ob{i}", [128, 512]) for i in range(3)]
        dtmp = [P.sb(f"ip_d{i}", [128, 512]) for i in range(2)]
        pp = [P.ps(f"ip_pp{i}", [128, 512]) for i in range(4)]
        if nlerp:
            carry = P.sb("ip_carry", [128, nlerp])
            mu = P.sb("ip_mu", [128, nlerp])
            k.v("pool", "memset", [], ["ip_carry"], ap=carry[:], constant=0.0)
            k.sch.dma("sp", "ipmu", lambda e: e.dma_start(out=mu[:], in_=mu_ap.rearrange("(c p) -> p c", p=128),
                                                           allow_slow_non_contiguous=True), writes=["ip_mu"])
        nw = 0; nc_ = 0; nl = 0
        for b in range(k.NB):
            x_b = xb[b % 2]; xk = ("ip_xb", b % 2)
            k.ld(x_b[:], k.A["xT"][b], reads=[("D", "xT", b)], writes=[xk], slot=("ipx", b % 2))
            for c0 in range(0, ncols, 512):
                c1 = min(ncols, c0 + 512)
                wt = w[nw % 2]; wk = ("ip_w", nw % 2)
                k.ld(wt[:, :, 0:c1 - c0], k.W["in", L][:, :, c0:c1], reads=k.win_keys[L], writes=[wk], slot=("ipw", nw % 2))
                nw += 1
                for cc in range(c0 // 128, (c1 + 127) // 128):
                    m = min(128, ncols - cc * 128)
                    ps = pp[nc_ % 4]; pk = ("ip_pp", nc_ % 4)
                    off = cc * 128 - c0
                    for kc in range(KC):
                        k.mm(ps[0:m, :], wt[:, kc, off:off + m], x_b[:, kc, :], kc == 0, kc == KC - 1, [wk, xk], [pk])
                    o = ob[nc_ % 3]; ok_ = ("ip_ob", nc_ % 3)
                    if cc < nlerp:
                        r = raw[nl % 2]; rk = ("ip_raw", nl % 2)
                        d = dtmp[nl % 2]; dk = ("ip_d", nl % 2)
                        k.v("pool", "tensor_copy", ["ip_carry"], [rk], out=r[:, 0:1], in_=carry[:, cc:cc + 1])
                        k.act(r[:, 1:513], ps[:], AF.Copy, [pk], [rk])
                        k.v("pool", "tensor_copy", [rk], ["ip_carry"], out=carry[:, cc:cc + 1], in_=r[:, 512:513])
                        k.v("dve", "tensor_tensor", [rk], [dk], out=d[:], in0=r[:, 0:512], in1=r[:, 1:513], op=ALU.subtract)
                        k.v("dve", "scalar_tensor_tensor", [dk, rk, "ip_mu"], [ok_], out=o[:], in0=d[:], scalar=mu[:, cc:cc + 1],
                            in1=r[:, 1:513], op0=ALU.mult, op1=ALU.add)
                        nl += 1
                    else:
                        k.copy(k.evac_engine(), o[0:m, :], ps[0:m, :], [pk], [ok_])
                    k.ld(pT[cc, 0:m, b * BLK:(b + 1) * BLK], o[0:m, :], reads=[ok_], writes=[("D", "pT", cc, b)],
                         slot=("ipst", nc_ % 3))
                    nc_ += 1
    return pT


K.inproj = _inproj


def _blockmask(k, P, name, csz, incl=True):
    m = P.sb(name, [128, 128])
    k.v("pool", "memset", [], [name], ap=m[:], constant=1.0)
    k.v("pool", "affine_select", [name], [name], out=m[:], in_=m[:], pattern=[[1, 128]],
        compare_op=ALU.is_ge, fill=0.0, base=(0 if incl else -1), channel_multiplier=-1)
    for c in range(1, 128 // csz):
        k.v("pool", "affine_select", [name], [name], out=m[:, c * csz:(c + 1) * csz], in_=m[:, c * csz:(c + 1) * csz],
            pattern=[[0, csz]], compare_op=ALU.is_ge, fill=0.0, base=-c * csz, channel_multiplier=1)
    return m


def _hgrn(self, L, pT):
    k = self
    j = L // 2
    TB = 256
    C = 64
    CPT = 128 // C
    NCH = TB // C
    Q0, F0, I0, G0 = 26, 34, 42, 50
    with k.phase() as P:
        mask = _blockmask(k, P, "hg_mask", C)
        rmask = P.sb("hg_rmask", [128, TB])
        k.v("pool", "memset", [], ["hg_rmask"], ap=rmask[:], constant=1.0)
        k.v("pool", "memset", ["hg_rmask"], ["hg_rmask"], ap=rmask[:].rearrange("p (c j) -> p c j", j=C)[:, :, 0:1], constant=0.0)
        lb = P.sb("hg_lb", [128, 8]); oml = P.sb("hg_oml", [128, 8])
        if j == 0:
            k.v("pool", "memset", [], ["hg_lb"], ap=lb[:], constant=0.0)
            k.v("pool", "memset", [], ["hg_oml"], ap=oml[:], constant=1.0)
        else:
            l0 = P.sb("hg_l0", [128, 8])
            k.sch.dma("sp", "hgl0", lambda e: e.dma_start(out=l0[:], in_=k.I["hgrn_lb"][0].rearrange("(h p) -> p h", p=128),
                                                           allow_slow_non_contiguous=True), writes=["hg_l0"])
            k.sch.dma("sp", "hgl1", lambda e: e.dma_start(out=lb[:], in_=k.I["hgrn_lb"][1].rearrange("(h p) -> p h", p=128),
                                                           allow_slow_non_contiguous=True), writes=["hg_lb"])
            k.v("dve", "tensor_tensor", ["hg_l0", "hg_lb"], ["hg_lb"], out=lb[:], in0=lb[:], in1=l0[:], op=ALU.subtract)
            k.act(lb[:], lb[:], AF.Sigmoid, ["hg_lb"], ["hg_lb"])
            k.v("dve", "tensor_scalar", ["hg_lb"], ["hg_oml"], out=oml[:], in0=lb[:], scalar1=-1.0, scalar2=1.0,
                op0=ALU.mult, op1=ALU.add)
        nwb = P.sb("hg_nw", [128, 128])
        k.ld(nwb[:], k.I["hgrn_norm_w"][j:j + 1, :].partition_broadcast(128), writes=["hg_nw"])
        qf = [[P.sb(f"hg_{nm}{i}", [128, 8, TB]) for i in range(2)] for nm in ("q", "f", "i", "g")]
        logf = P.sb("hg_logf", [128, 8, TB]); kg = P.sb("hg_kg", [128, 8, TB]); bc = P.sb("hg_bc", [128, 8, TB])
        et = P.sb("hg_et", [128, 8, TB])
        eb = P.sb("hg_eb", [128, 8, NCH])
        qe = P.sb("hg_qe", [128, 8, TB], BF16); ke = P.sb("hg_ke", [128, 8, TB], BF16)
        kd = P.sb("hg_kd", [128, 8, TB], BF16); ib = P.sb("hg_ib", [128, 8, TB], BF16)
        S32 = P.sb("hg_S32", [128, 8, 128]); Sbf = P.sb("hg_Sbf", [128, 8, 8, 128], BF16)
        kdT = [P.sb(f"hg_kdT{i}", [128, 128], BF16) for i in range(3)]
        vT = [P.sb(f"hg_vT{i}", [128, 128], BF16) for i in range(3)]
        aT = [P.sb(f"hg_aT{i}", [128, 128], BF16) for i in range(3)]
        on = [P.sb(f"hg_on{i}", [128, 128], BF16) for i in range(3)]
        sc = [P.sb(f"hg_sc{i}", [128, 4]) for i in range(3)]
        junk = P.sb("hg_junk", [128, 128])
        mixb = [P.sb(f"hg_mix{i}", [128, 8, BLK], BF16) for i in range(2)]
        psA_ = [P.ps(f"hg_psA{i}", [128, 512]) for i in range(1)]
        psO_ = [P.ps(f"hg_psO{i}", [128, 512]) for i in range(2)]
        psS_ = [P.ps(f"hg_psS{i}", [128, 512]) for i in range(2)]
        psT_ = [P.ps(f"hg_psT{i}", [128, 1024], BF16) for i in range(3)]

        class _V:
            def __init__(self, lst):
                self.lst = lst

            def __getitem__(self, idx):
                p, i, c = idx
                return self.lst[i][p, 0:128]
        psA, psO, psS, psT = _V(psA_), _V(psO_), _V(psS_), _V(psT_)
        k.v("pool", "memset", [], ["hg_S32"], ap=S32[:], constant=0.0)
        k.v("pool", "memset", [], [("hg_Sbf", h, 0) for h in range(8)], ap=Sbf[:, :, 0, :], constant=0.0)
        nhb = k.S // TB
        cnt = {"a": 0, "o": 0, "s": 0, "t": 0, "u": 0}
        for hb in range(nhb):
            t0 = hb * TB
            i2 = hb % 2
            names = ("q", "f", "i", "g")
            tl = {}
            for ni, (nm, c0) in enumerate(zip(names, (Q0, F0, I0, G0))):
                tt = qf[ni][i2]
                key = ("hg_" + nm, i2)
                bks = sorted(set(t // BLK for t in (t0, t0 + TB - 1)))
                k.ld(tt[:], pT[c0:c0 + 8, :, t0:t0 + TB].rearrange("c p t -> p c t"),
                     reads=[("D", "pT", c0 + h, bb) for h in range(8) for bb in bks], writes=[key], slot=("hgld", ni, i2))
                tl[nm] = (tt, key)
            (q, qk), (f, fk), (iv, ik), (g, gk) = tl["q"], tl["f"], tl["i"], tl["g"]
            fl = lambda t: t[:].rearrange("p h t -> p (h t)")
            bcast = lambda t: t[:].rearrange("p (h o) -> p h o", o=1).to_broadcast([128, 8, TB])
            k.act(fl(f), fl(f), AF.Sigmoid, [fk], [fk])
            k.v("dve", "tensor_tensor", [fk, "hg_oml"], [fk], out=f[:], in0=f[:], in1=bcast(oml), op=ALU.mult)
            k.v("dve", "tensor_tensor", [fk, "hg_lb"], [fk], out=f[:], in0=f[:], in1=bcast(lb), op=ALU.add)
            k.act(fl(logf), fl(f), AF.Ln, [fk], ["hg_logf"])
            k.v("pool", "tensor_scalar", [fk], ["hg_kg"], out=fl(kg), in0=fl(f), scalar1=-1.0, scalar2=1.0, op0=ALU.mult, op1=ALU.add)
            k.act(fl(q), fl(q), AF.Silu, [qk], [qk])
            k.act(fl(g), fl(g), AF.Silu, [gk], [gk])
            k.v("pool", "tensor_copy", [ik], ["hg_ib"], out=fl(ib), in_=fl(iv))
            for h in range(8):
                k.v("dve", "tensor_tensor_scan", ["hg_logf", "hg_rmask"], [("hg_bc", h)], out=bc[:, h, :], data0=rmask[:],
                    data1=logf[:, h, :], initial=0.0, op0=ALU.mult, op1=ALU.add)
            bck = [("hg_bc", h) for h in range(8)]
            k.v("dve", "tensor_scalar", bck, bck, out=fl(bc), in0=fl(bc), scalar1=-80.0, scalar2=None, op0=ALU.max)
            k.act(fl(et), fl(bc), AF.Exp, bck, ["hg_et"])
            k.v("pool", "tensor_tensor", ["hg_et", qk], ["hg_qe"], out=fl(qe), in0=fl(q), in1=fl(et), op=ALU.mult)
            k.act(fl(et), fl(bc), AF.Exp, bck + ["hg_qe"], ["hg_et"], scale=-1.0)
            k.v("pool", "tensor_tensor", ["hg_et", "hg_kg"], ["hg_ke"], out=fl(ke), in0=fl(kg), in1=fl(et), op=ALU.mult)
            bl = bc[:].rearrange("p h (c j) -> p (h c) j", j=C)[:, :, C - 1:C]
            k.act(eb[:].rearrange("p h (c o) -> p (h c) o", o=1), bl, AF.Exp, bck, ["hg_eb"])
            k.v("dve", "tensor_tensor", bck + ["hg_ke"], ["hg_et"], out=et[:].rearrange("p h (c j) -> p (h c) j", j=C),
                in0=bl.to_broadcast([128, 8 * NCH, C]), in1=bc[:].rearrange("p h (c j) -> p (h c) j", j=C), op=ALU.subtract)
            k.act(fl(et), fl(et), AF.Exp, ["hg_et"], ["hg_et"])
            k.v("pool", "tensor_tensor", ["hg_et", "hg_kg"], ["hg_kd"], out=fl(kd), in0=fl(kg), in1=fl(et), op=ALU.mult)
            for ti in range(TB // 128):
                tg = hb * (TB // 128) + ti
                b = (tg * 128) // BLK
                sub = tg % 4
                mb = mixb[b % 2]; mbk = ("hg_mix", b % 2)
                ts = slice(ti * 128, (ti + 1) * 128)
                for h in range(8):
                    u = cnt["u"] % 3; cnt["u"] += 1
                    k.tr(psT[:, 0, :], kd[:, h, ts], k.c_ident[:], ["hg_kd"], [("hg_psT", 0)])
                    k.copy("act", kdT[u][:], psT[:, 0, :], [("hg_psT", 0)], [("hg_kdT", u)])
                    k.tr(psT[:, 1, :], ib[:, h, ts], k.c_ident[:], ["hg_ib"], [("hg_psT", 1)])
                    k.copy("dve", vT[u][:], psT[:, 1, :], [("hg_psT", 1)], [("hg_vT", u)])
                    ia = 0
                    k.mm(psA[:, ia, :], ke[:, h, ts], qe[:, h, ts], True, True, ["hg_ke", "hg_qe"], [("hg_psA", ia)])
                    k.v("dve", "tensor_tensor", [("hg_psA", ia), "hg_mask"], [("hg_aT", u)], out=aT[u][:], in0=psA[:, ia, :],
                        in1=mask[:], op=ALU.mult)
                    io = cnt["o"] % 2; cnt["o"] += 1
                    ok_ = ("hg_psO", io)
                    for c in range(CPT):
                        gch = tg * CPT + c
                        slot = gch % 8
                        k.mm(psO[c * C:(c + 1) * C, io, :], qe[:, h, ti * 128 + c * C: ti * 128 + (c + 1) * C], Sbf[:, h, slot, :],
                             True, False, ["hg_qe", ("hg_Sbf", h, slot)], [ok_])
                        is_ = cnt["s"] % 2; cnt["s"] += 1
                        k.mm(psS[:, is_, :], kdT[u][c * C:(c + 1) * C, :], vT[u][c * C:(c + 1) * C, :], True, True,
                             [("hg_kdT", u), ("hg_vT", u)], [("hg_psS", is_)])
                        k.v("dve", "scalar_tensor_tensor", [("hg_psS", is_), ("hg_S32", h), "hg_eb"], [("hg_S32", h)],
                            out=S32[:, h, :], in0=S32[:, h, :], scalar=eb[:, h, ti * CPT + c: ti * CPT + c + 1], in1=psS[:, is_, :],
                            op0=ALU.mult, op1=ALU.add)
                        nslot = (gch + 1) % 8
                        k.copy("act", Sbf[:, h, nslot, :], S32[:, h, :], [("hg_S32", h)], [("hg_Sbf", h, nslot)])
                    k.mm(psO[:, io, :], aT[u][:], vT[u][:], False, True, [("hg_aT", u), ("hg_vT", u)], [ok_])
                    s_ = sc[u]; sk = ("hg_sc", u)
                    k.act(junk[:], psO[:, io, :], AF.Square, [ok_], ["hg_junk", sk], accum_out=s_[:, 0:1])
                    k.act(s_[:, 1:2], s_[:, 0:1], AF.Sqrt, [sk], [sk], scale=1.0 / 128, bias=k.c_eps[:, 3:4])
                    k.v("dve", "reciprocal", [sk], [sk], out=s_[:, 2:3], in_=s_[:, 1:2])
                    k.v("dve", "scalar_tensor_tensor", [ok_, sk, "hg_nw"], [("hg_on", u)], out=on[u][:], in0=psO[:, io, :],
                        scalar=s_[:, 2:3], in1=nwb[:], op0=ALU.mult, op1=ALU.mult)
                    it = 2
                    k.tr(psT[:, it, :], on[u][:], k.c_ident[:], [("hg_on", u)], [("hg_psT", it)])
                    k.v("dve", "tensor_tensor", [("hg_psT", it), gk], [mbk], out=mb[:, h, sub * 128:(sub + 1) * 128],
                        in0=psT[:, it, :], in1=g[:, h, ts], op=ALU.mult)
                if sub == 3:
                    k.ld(k.A["mixT"][b][:, 8:16, :], mb[:], reads=[mbk], writes=[("D", "mixT", b, 1)], slot=("hgst", b % 2))


K.hgrn = _hgrn


def _lowmask(k, P, name, csz):
    m = P.sb(name, [128, 128])
    k.v("pool", "memset", [], [name], ap=m[:], constant=1.0)
    k.v("pool", "affine_select", [name], [name], out=m[:], in_=m[:], pattern=[[-1, 128]],
        compare_op=ALU.is_ge, fill=0.0, base=-1, channel_multiplier=1)
    for c in range(1, 128 // csz):
        k.v("pool", "affine_select", [name], [name], out=m[c * csz:(c + 1) * csz, :], in_=m[c * csz:(c + 1) * csz, :],
            pattern=[[1, 128]], compare_op=ALU.is_ge, fill=0.0, base=-c * csz, channel_multiplier=0)
    return m


def _rwkv(self, L, pT):
    k = self
    j = L // 2
    TB = 256
    C = 64
    NCH = TB // C
    I = k.I
    with k.phase() as P:
        mU = _blockmask(k, P, "rw_mU", C, True)
        mUs = _blockmask(k, P, "rw_mUs", C, False)
        mLs = _lowmask(k, P, "rw_mLs", C)
        rmask = P.sb("rw_rmask", [128, TB])
        k.v("pool", "memset", [], ["rw_rmask"], ap=rmask[:], constant=1.0)
        k.v("pool", "memset", ["rw_rmask"], ["rw_rmask"], ap=rmask[:].rearrange("p (c j) -> p c j", j=C)[:, :, 0:1], constant=0.0)
        bones = P.sb("rw_bones", [128, 128], BF16)
        k.v("pool", "memset", [], ["rw_bones"], ap=bones[:], constant=0.0)
        k.v("pool", "memset", ["rw_bones"], ["rw_bones"], ap=bones[0:64, 0:64], constant=1.0)
        k.v("pool", "memset", ["rw_bones"], ["rw_bones"], ap=bones[64:128, 64:128], constant=1.0)
        cst = P.sb("rw_cst", [128, 4])
        for i_, val in enumerate((1.0, -0.5, 1e-24, 0.0)):
            k.v("pool", "memset", [], ["rw_cst"], ap=cst[:, i_:i_ + 1], constant=val)
        prm = {}
        for nm in ("rwkv_w0", "rwkv_a0", "rwkv_kk", "rwkv_ka", "rwkv_rk"):
            t = P.sb("rw_" + nm, [128, 8])
            k.sch.dma("sp", ("rwprm", nm), lambda e, t=t, nm=nm: e.dma_start(out=t[:], in_=I[nm][j].rearrange("(c p) -> p c", p=128),
                                                                        allow_slow_non_contiguous=True), writes=["rw_" + nm])
            prm[nm] = t
        nw0 = P.sb("rw_nw0", [128, 8])
        k.v("dve", "tensor_scalar", ["rw_rwkv_w0"], ["rw_nw0"], out=nw0[:], in0=prm["rwkv_w0"][:], scalar1=-1.0, scalar2=None, op0=ALU.mult)
        lnw = P.sb("rw_lnw", [128, 1024]); lnb = P.sb("rw_lnb", [128, 1024])
        k.ld(lnw[:], I["rwkv_lnx_w"][j:j + 1, :].partition_broadcast(128), writes=["rw_lnw"])
        k.ld(lnb[:], I["rwkv_lnx_b"][j:j + 1, :].partition_broadcast(128), writes=["rw_lnb"])
        w2b = P.sb("rw_w2b", [128, 1024], BF16); a2b = P.sb("rw_a2b", [128, 1024], BF16); g2b = P.sb("rw_g2b", [128, 1024], BF16)
        k.sch.dma("pool", "rwlw", lambda e: e.dma_start(out=w2b[0:64, :], in_=I["rwkv_w2"][j]), writes=["rw_w2b"])
        k.sch.dma("pool", "rwla", lambda e: e.dma_start(out=a2b[64:128, :], in_=I["rwkv_a2"][j]), writes=["rw_a2b"])
        k.sch.dma("pool", "rwlg", lambda e: e.dma_start(out=g2b[:, :], in_=I["rwkv_g2"][j]), writes=["rw_g2b"])
        hsel = P.sb("rw_hsel", [128, 2], BF16)
        k.v("pool", "memset", [], ["rw_hsel"], ap=hsel[:], constant=0.0)
        k.v("pool", "memset", ["rw_hsel"], ["rw_hsel"], ap=hsel[0:64, 0:1], constant=1.0)
        k.v("pool", "memset", ["rw_hsel"], ["rw_hsel"], ap=hsel[64:128, 1:2], constant=1.0)
        F3 = [128, 8, TB]
        r_ = P.sb("rw_r", F3); k_ = P.sb("rw_k", F3); v_ = P.sb("rw_v", F3)
        xwa = P.sb("rw_xwa", [128, TB]); xg = P.sb("rw_xg", [128, TB])
        th = P.sb("rw_th", [128, TB], BF16); sg = P.sb("rw_sg", [128, TB], BF16)
        d_ = P.sb("rw_d", F3); alr = P.sb("rw_alr", F3); kkn = P.sb("rw_kkn", F3); kp = P.sb("rw_kp", F3)
        cum = P.sb("rw_cum", F3); et = P.sb("rw_et", F3); t1 = P.sb("rw_t1", F3)
        B3 = lambda nm: P.sb(nm, F3, BF16)
        at, bt, kt, rt, bh, kh, vb, prod, kk2 = [B3("rw_" + n) for n in ("at", "bt", "kt", "rt", "bh", "kh", "vb", "prod", "kk2")]
        pc = P.sb("rw_pc", [128, 8, NCH])
        tm = {n: [P.sb(f"rw_tm_{n}{i}", [128, 8, 128], BF16) for i in range(2)] for n in ("v", "a", "bh", "kh")}
        ytm = P.sb("rw_ytm", [128, 16, 64]); gtm = P.sb("rw_gtm", [128, 1024]); rkb = P.sb("rw_rkb", [128, 16])
        st = P.sb("rw_st", [128, 4, 16]); ysq = P.sb("rw_ysq", [128, 16, 64]); yb = P.sb("rw_yb", [128, 1024], BF16)
        mixb = [P.sb(f"rw_mix{i}", [128, 8, BLK], BF16) for i in range(2)]
        NR = 2
        dbl = {n: [P.sb(f"rw_{n}{i}", [128, 128], BF16) for i in range(2 * NR)] for n in ("X", "Y", "Pm", "Qm")}
        gm = {n: [P.sb(f"rw_{n}{i}", [128, 128], BF16) for i in range(NR)] for n in ("Kt", "RBt", "RKt")}
        zs = [P.sb(f"rw_zs{i}", [128, 64], BF16) for i in range(NR)]
        uloc = [P.sb(f"rw_uloc{i}", [128, 64]) for i in range(NR)]
        wt = [P.sb(f"rw_wt{i}", [128, 128], BF16) for i in range(NR)]
        ub = [P.sb(f"rw_ub{i}", [128, 64], BF16) for i in range(NR)]
        G32 = P.sb("rw_G32", [128, 8, 64]); Gbf = P.sb("rw_Gbf", [128, 8, 4, 64], BF16)
        k.v("pool", "memset", [], ["rw_G32"], ap=G32[:], constant=0.0)
        k.v("pool", "memset", [], [("rw_Gbf", h, 0) for h in range(16)], ap=Gbf[:, :, 0, :], constant=0.0)
        pg = [P.ps(f"rw_pg{i}", [128, 512]) for i in range(2)]
        pTt = [P.ps(f"rw_pT{i}", [128, 1024], BF16) for i in range(2)]
        pU = P.ps("rw_pU", [128, 512]); pG = P.ps("rw_pG", [128, 512]); pY = P.ps("rw_pY", [128, 512]); pM = P.ps("rw_pM", [128, 512])
        cn = {"g": 0, "t": 0, "u": 0}
        fl = lambda t: t[:].rearrange("p h t -> p (h t)")
        bc8 = lambda t: t[:].rearrange("p (h o) -> p h o", o=1).to_broadcast([128, 8, TB])
        c3 = lambda t: t[:].rearrange("p h (c j) -> p (h c) j", j=C)

        def gram(lhsT, rhs, mask, out, okey, rkeys):
            i = cn["g"] % 2; cn["g"] += 1
            k.mm(pg[i][:, 0:128], lhsT, rhs, True, True, rkeys, [("rw_pg", i)])
            k.v("dve", "tensor_tensor", [("rw_pg", i), mask[1]], [okey], out=out, in0=pg[i][:, 0:128], in1=mask[0][:], op=ALU.mult)

        nhb = k.S // TB
        for hb in range(nhb):
            t0 = hb * TB
            bks = sorted(set(t // BLK for t in (t0, t0 + TB - 1)))
            for nm, tt, c0 in (("r", r_, 0), ("k", k_, 8), ("v", v_, 16)):
                k.ld(tt[:], pT[c0:c0 + 8, :, t0:t0 + TB].rearrange("c p t -> p c t"),
                     reads=[("D", "pT", c0 + h, bb) for h in range(8) for bb in bks], writes=["rw_" + nm], slot=("rwld", nm))
            k.ld(xwa[:], pT[24, :, t0:t0 + TB], reads=[("D", "pT", 24, bb) for bb in bks], writes=["rw_xwa"], slot=("rwld", "xwa"))
            k.ld(xg[:], pT[25, :, t0:t0 + TB], reads=[("D", "pT", 25, bb) for bb in bks], writes=["rw_xg"], slot=("rwld", "xg"))
            k.act(th[0:64, :], xwa[0:64, :], AF.Tanh, ["rw_xwa"], ["rw_th"])
            k.act(th[64:128, :], xwa[64:128, :], AF.Copy, ["rw_xwa"], ["rw_th"])
            k.act(sg[:], xg[:], AF.Sigmoid, ["rw_xg"], ["rw_sg"])
            for hp in range(8):
                cs = slice(hp * 128, (hp + 1) * 128)
                k.mm(pM[:, 0:TB], w2b[0:64, cs], th[0:64, :], True, True, ["rw_w2b", "rw_th"], ["rw_pM"])
                k.act(d_[:, hp, :], pM[:, 0:TB], AF.Exp, ["rw_pM", "rw_nw0"], [("rw_d", hp)], scale=-1.0, bias=nw0[:, hp:hp + 1])
                k.mm(pM[:, 0:TB], a2b[64:128, cs], th[64:128, :], True, True, ["rw_a2b", "rw_th"], ["rw_pM"])
                k.act(alr[:, hp, :], pM[:, 0:TB], AF.Sigmoid, ["rw_pM", "rw_rwkv_a0"], [("rw_alr", hp)], bias=prm["rwkv_a0"][:, hp:hp + 1])
            dk = [("rw_d", hp) for hp in range(8)]; ak = [("rw_alr", hp) for hp in range(8)]
            k.act(fl(d_), fl(d_), AF.Ln, dk, dk, bias=cst[:, 0:1])
            k.act(fl(d_), fl(d_), AF.Exp, dk, dk, scale=-1.0, bias=cst[:, 1:2])
            k.v("dve", "tensor_tensor", ["rw_k", "rw_rwkv_kk"], ["rw_kkn"], out=kkn[:], in0=k_[:], in1=bc8(prm["rwkv_kk"]), op=ALU.mult)
            k.v("pool", "tensor_tensor", ["rw_kkn"], ["rw_kk2"], out=fl(kk2), in0=fl(kkn), in1=fl(kkn), op=ALU.mult)
            for hp in range(8):
                k.mm(pM[:, 0:TB], bones[:], kk2[:, hp, :], True, True, ["rw_bones", "rw_kk2"], ["rw_pM"])
                k.act(t1[:, hp, :], pM[:, 0:TB], AF.Sqrt, ["rw_pM"], [("rw_t1", hp)], bias=cst[:, 2:3])
            tk = [("rw_t1", hp) for hp in range(8)]
            k.v("dve", "reciprocal", tk, tk, out=fl(t1), in_=fl(t1))
            k.v("dve", "tensor_tensor", tk + ["rw_kkn"], ["rw_kkn"], out=fl(kkn), in0=fl(kkn), in1=fl(t1), op=ALU.mult)
            k.v("dve", "scalar_tensor_tensor", ak + ["rw_rwkv_ka"], tk, out=t1[:], in0=alr[:], scalar=-1.0, in1=bc8(prm["rwkv_ka"]),
                op0=ALU.add, op1=ALU.mult)
            k.v("dve", "scalar_tensor_tensor", tk + ["rw_k"], ["rw_kp"], out=fl(kp), in0=fl(t1), scalar=1.0, in1=fl(k_),
                op0=ALU.add, op1=ALU.mult)
            k.v("pool", "tensor_tensor", ["rw_r", "rw_kp"], tk, out=fl(t1), in0=fl(r_), in1=fl(kp), op=ALU.mult)
            k.v("pool", "tensor_tensor", tk + ["rw_rwkv_rk"], ["rw_prod"], out=prod[:], in0=t1[:], in1=bc8(prm["rwkv_rk"]), op=ALU.mult)
            k.v("pool", "tensor_copy", ["rw_v"], ["rw_vb"], out=fl(vb), in_=fl(v_))
            for hp in range(8):
                k.v("dve", "tensor_tensor_scan", dk + ["rw_rmask"], [("rw_cum", hp)], out=cum[:, hp, :], data0=rmask[:],
                    data1=d_[:, hp, :], initial=0.0, op0=ALU.mult, op1=ALU.add)
            ck = [("rw_cum", hp) for hp in range(8)]
            cl = c3(cum)[:, :, C - 1:C]
            k.act(pc[:].rearrange("p h (c o) -> p (h c) o", o=1), cl, AF.Exp, ck, ["rw_pc"], scale=-1.0)
            k.act(fl(et), fl(cum), AF.Exp, ck, ["rw_et"], scale=-1.0)
            k.v("pool", "tensor_tensor", ["rw_et", "rw_r"], ["rw_rt"], out=fl(rt), in0=fl(r_), in1=fl(et), op=ALU.mult)
            k.v("dve", "tensor_tensor", ["rw_kkn"] + ak, tk, out=fl(t1), in0=fl(kkn), in1=fl(alr), op=ALU.mult)
            k.act(fl(et), fl(cum), AF.Exp, ck + ["rw_rt"], ["rw_et"])
            k.v("pool", "tensor_tensor", ["rw_et"] + tk, ["rw_bt"], out=fl(bt), in0=fl(t1), in1=fl(et), op=ALU.mult)
            k.v("dve", "tensor_tensor", ["rw_et", "rw_kp"], ["rw_kt"], out=fl(kt), in0=fl(kp), in1=fl(et), op=ALU.mult)
            k.v("dve", "tensor_tensor", ck + ["rw_bt", "rw_kt"], ["rw_et"], out=c3(et), in0=c3(cum), in1=cl.to_broadcast([128, 8 * NCH, C]),
                op=ALU.subtract)
            k.act(fl(et), fl(et), AF.Exp, ["rw_et"], ["rw_et"])
            k.v("pool", "tensor_tensor", ["rw_et"] + tk, ["rw_bh"], out=fl(bh), in0=fl(t1), in1=fl(et), op=ALU.mult)
            k.v("dve", "tensor_tensor", ["rw_et", "rw_kp"], ["rw_kh"], out=fl(kh), in0=fl(kp), in1=fl(et), op=ALU.mult)
            k.v("dve", "tensor_tensor", ck + dk + ["rw_bh", "rw_kh"], ["rw_et"], out=fl(et), in0=fl(d_), in1=fl(cum), op=ALU.subtract)
            k.act(fl(et), fl(et), AF.Exp, ["rw_et"], ["rw_et"])
            k.v("dve", "scalar_tensor_tensor", ["rw_et", "rw_kkn"], ["rw_at"], out=fl(at), in0=fl(kkn), scalar=-1.0, in1=fl(et),
                op0=ALU.mult, op1=ALU.mult)
            for ti in range(TB // 128):
                tg = hb * (TB // 128) + ti
                b = (tg * 128) // BLK
                sub = tg % 4
                mb = mixb[b % 2]; mbk = ("rw_mix", b % 2)
                ts = slice(ti * 128, (ti + 1) * 128)
                i2 = tg % 2
                for n_i, (nm, src, skey) in enumerate((("v", vb, "rw_vb"), ("a", at, "rw_at"), ("bh", bh, "rw_bh"), ("kh", kh, "rw_kh"))):
                    ip = cn["t"] % 2; cn["t"] += 1
                    for hp in range(8):
                        k.tr(pTt[ip][:, hp * 128:(hp + 1) * 128], src[:, hp, ts], k.c_ident[:], [skey], [("rw_pT", ip)])
                    k.copy(k.evac_engine(), tm[nm][i2][:].rearrange("p h c -> p (h c)"), pTt[ip][:], [("rw_pT", ip)], [("rw_tm", nm, i2)])
                for hp in range(8):
                    k.mm(pM[:, 2 * hp:2 * hp + 2], prod[:, hp, ts], hsel[:], True, True, ["rw_prod", "rw_hsel"], ["rw_pM"])
                k.copy("act", rkb[:], pM[:, 0:16], ["rw_pM"], ["rw_rkb"])
                for q in range(2):
                    k.mm(pM[:, :], sg[:, ts], g2b[:, q * 512:(q + 1) * 512], True, True, ["rw_sg", "rw_g2b"], ["rw_pM"])
                    k.copy("act", gtm[:, q * 512:(q + 1) * 512], pM[:, :], ["rw_pM"], ["rw_gtm"])
                for hp in range(8):
                    for par in range(2):
                        h = 2 * hp + par
                        pr = slice(par * 64, (par + 1) * 64)
                        u = cn["u"] % NR; cn["u"] += 1
                        A_, B_, K_, R_ = at[pr, hp, ts], bt[pr, hp, ts], kt[pr, hp, ts], rt[pr, hp, ts]
                        X = dbl["X"]; Y = dbl["Y"]; Pm = dbl["Pm"]; Qm = dbl["Qm"]
                        xi = lambda lvl: 2 * u + (lvl % 2)
                        gram(B_, A_, (mUs, "rw_mUs"), X[xi(0)][:], ("rw_X", xi(0)), ["rw_bt", "rw_at"])
                        gram(A_, B_, (mLs, "rw_mLs"), Y[xi(0)][:], ("rw_Y", xi(0)), ["rw_bt", "rw_at"])
                        gram(K_, A_, (mUs, "rw_mUs"), gm["Kt"][u][:], ("rw_Kt", u), ["rw_kt", "rw_at"])
                        gram(B_, R_, (mU, "rw_mU"), gm["RBt"][u][:], ("rw_RBt", u), ["rw_bt", "rw_rt"])
                        gram(K_, R_, (mU, "rw_mU"), gm["RKt"][u][:], ("rw_RKt", u), ["rw_kt", "rw_rt"])
                        pcur, qcur = ("rw_X", xi(0)), ("rw_Y", xi(0))
                        Pc, Qc = X[xi(0)], Y[xi(0)]
                        for lvl in range(5):
                            xo, xn = xi(lvl), xi(lvl + 1)
                            last = lvl == 4
                            i = cn["g"] % 2; cn["g"] += 1
                            k.mm(pg[i][:, 0:128], Y[xo][:], X[xo][:], True, True, [("rw_Y", xo), ("rw_X", xo)], [("rw_pg", i)])
                            k.copy(k.evac_engine(), X[xn][:], pg[i][:, 0:128], [("rw_pg", i)], [("rw_X", xn)])
                            if not last:
                                i = cn["g"] % 2; cn["g"] += 1
                                k.mm(pg[i][:, 0:128], X[xo][:], Y[xo][:], True, True, [("rw_Y", xo), ("rw_X", xo)], [("rw_pg", i)])
                                k.copy(k.evac_engine(), Y[xn][:], pg[i][:, 0:128], [("rw_pg", i)], [("rw_Y", xn)])
                            pn = 2 * u + (lvl % 2)
                            i = cn["g"] % 2; cn["g"] += 1
                            k.mm(pg[i][:, 0:128], Qc[:], X[xn][:], True, False, [qcur, ("rw_X", xn)], [("rw_pg", i)])
                            k.mm(pg[i][:, 0:128], k.c_ident[:], X[xn][:], False, False, [("rw_X", xn)], [("rw_pg", i)])
                            k.mm(pg[i][:, 0:128], k.c_ident[:], Pc[:], False, True, [pcur], [("rw_pg", i)])
                            k.copy(k.evac_engine(), Pm[pn][:], pg[i][:, 0:128], [("rw_pg", i)], [("rw_Pm", pn)])
                            if not last:
                                i = cn["g"] % 2; cn["g"] += 1
                                k.mm(pg[i][:, 0:128], Pc[:], Y[xn][:], True, False, [pcur, ("rw_Y", xn)], [("rw_pg", i)])
                                k.mm(pg[i][:, 0:128], k.c_ident[:], Y[xn][:], False, False, [("rw_Y", xn)], [("rw_pg", i)])
                                k.mm(pg[i][:, 0:128], k.c_ident[:], Qc[:], False, True, [qcur], [("rw_pg", i)])
                                k.copy(k.evac_engine(), Qm[pn][:], pg[i][:, 0:128], [("rw_pg", i)], [("rw_Qm", pn)])
                                Qc, qcur = Qm[pn], ("rw_Qm", pn)
                            Pc, pcur = Pm[pn], ("rw_Pm", pn)
                        Tt, tkey = Pc, pcur
                        Vh = tm["v"][i2][:, hp, pr]; vkey = ("rw_tm", "v", i2)
                        i = cn["g"] % 2; cn["g"] += 1
                        k.mm(pg[i][:, 0:64], gm["Kt"][u][:], Vh, True, True, [("rw_Kt", u), vkey], [("rw_pg", i)])
                        k.copy(k.evac_engine(), zs[u][:], pg[i][:, 0:64], [("rw_pg", i)], [("rw_zs", u)])
                        i = cn["g"] % 2; cn["g"] += 1
                        k.mm(pg[i][:, 0:64], k.c_ident[:], zs[u][:], True, False, [("rw_zs", u)], [("rw_pg", i)])
                        k.mm(pg[i][:, 0:64], Tt[:], zs[u][:], False, True, [tkey, ("rw_zs", u)], [("rw_pg", i)])
                        k.copy(k.evac_engine(), uloc[u][:], pg[i][:, 0:64], [("rw_pg", i)], [("rw_uloc", u)])
                        i = cn["g"] % 2; cn["g"] += 1
                        k.mm(pg[i][pr, 0:128], tm["a"][i2][:, hp, pr], Tt[:], True, True, [("rw_tm", "a", i2), tkey], [("rw_pg", i)])
                        k.v("dve", "tensor_tensor", [("rw_pg", i), "rw_at"], [("rw_wt", u)], out=wt[u][pr, :], in0=pg[i][pr, 0:128], in1=A_,
                            op=ALU.add)
                        for c in range(2):
                            gch = tg * 2 + c
                            slot = gch % 4
                            cr = slice(c * 64, (c + 1) * 64)
                            gk = ("rw_Gbf", h, slot)
                            k.mm(pU[cr, 0:64], wt[u][pr, cr], Gbf[pr, hp, slot, :], True, True, [("rw_wt", u), gk], ["rw_pU"])
                            k.v("dve", "tensor_tensor", ["rw_pU", ("rw_uloc", u)], [("rw_ub", u)], out=ub[u][cr, :], in0=pU[cr, 0:64],
                                in1=uloc[u][cr, :], op=ALU.add)
                            k.mm(pY[cr, 0:64], rt[pr, hp, ti * 128 + c * 64: ti * 128 + (c + 1) * 64], Gbf[pr, hp, slot, :], True, False,
                                 ["rw_rt", gk], ["rw_pY"])
                            k.mm(pG[pr, 0:64], tm["bh"][i2][cr, hp, pr], ub[u][cr, :], True, False, [("rw_tm", "bh", i2), ("rw_ub", u)], ["rw_pG"])
                            k.mm(pG[pr, 0:64], tm["kh"][i2][cr, hp, pr], tm["v"][i2][cr, hp, pr], False, True, [("rw_tm", "kh", i2), vkey], ["rw_pG"])
                            k.v("dve", "scalar_tensor_tensor", ["rw_pG", ("rw_G32", h), "rw_pc"], [("rw_G32", h)], out=G32[pr, hp, :],
                                in0=G32[pr, hp, :], scalar=pc[pr, hp, ti * 2 + c: ti * 2 + c + 1], in1=pG[pr, 0:64], op0=ALU.mult, op1=ALU.add)
                            ns = (gch + 1) % 4
                            k.copy("act", Gbf[pr, hp, ns, :], G32[pr, hp, :], [("rw_G32", h)], [("rw_Gbf", h, ns)])
                        k.mm(pY[:, 0:64], gm["RBt"][u][:], ub[u][:], False, False, [("rw_RBt", u), ("rw_ub", u)], ["rw_pY"])
                        k.mm(pY[:, 0:64], gm["RKt"][u][:], Vh, False, True, [("rw_RKt", u), vkey], ["rw_pY"])
                        k.copy("act", ytm[:, h, :], pY[:, 0:64], ["rw_pY"], ["rw_ytm"])
                yk = ["rw_ytm"]
                k.v("dve", "tensor_reduce", yk, ["rw_st"], out=st[:, 0, :], in_=ytm[:], axis=AX.X, op=ALU.add)
                k.v("dve", "tensor_scalar", ["rw_st"], ["rw_st"], out=st[:, 0, :], in0=st[:, 0, :], scalar1=1.0 / 64, scalar2=None, op0=ALU.mult)
                b16 = lambda a: a.rearrange("p (h o) -> p h o", o=1).to_broadcast([128, 16, 64])
                k.v("dve", "tensor_tensor", yk + ["rw_st"], yk, out=ytm[:], in0=ytm[:], in1=b16(st[:, 0, :]), op=ALU.subtract)
                k.v("pool", "tensor_tensor", yk, ["rw_ysq"], out=ysq[:], in0=ytm[:], in1=ytm[:], op=ALU.mult)
                k.v("dve", "tensor_reduce", ["rw_ysq"], ["rw_st"], out=st[:, 1, :], in_=ysq[:], axis=AX.X, op=ALU.add)
                k.act(st[:, 2, :], st[:, 1, :], AF.Sqrt, ["rw_st"], ["rw_st"], scale=1.0 / 64, bias=k.c_eps[:, 2:3])
                k.v("dve", "reciprocal", ["rw_st"], ["rw_st"], out=st[:, 3, :], in_=st[:, 2, :])
                k.v("dve", "tensor_tensor", yk + ["rw_st"], yk, out=ytm[:], in0=ytm[:], in1=b16(st[:, 3, :]), op=ALU.mult)
                yf = ytm[:].rearrange("p h c -> p (h c)")
                k.v("pool", "tensor_tensor", yk + ["rw_lnw"], yk, out=yf, in0=yf, in1=lnw[:], op=ALU.mult)
                k.v("pool", "tensor_tensor", yk + ["rw_lnb"], yk, out=yf, in0=yf, in1=lnb[:], op=ALU.add)
                k.v("dve", "tensor_tensor", ["rw_rkb", ("rw_tm", "v", i2)], ["rw_ysq"], out=ysq[:], in0=b16(rkb[:]),
                    in1=tm["v"][i2][:].rearrange("p h (a c) -> p (h a) c", a=2), op=ALU.mult)
                k.v("dve", "tensor_tensor", yk + ["rw_ysq"], yk, out=ytm[:], in0=ytm[:], in1=ysq[:], op=ALU.add)
                k.v("dve", "tensor_tensor", yk + ["rw_gtm"], ["rw_yb"], out=yb[:], in0=yf, in1=gtm[:], op=ALU.mult)
                ip = cn["t"] % 2; cn["t"] += 1
                for hp in range(8):
                    k.tr(pTt[ip][:, hp * 128:(hp + 1) * 128], yb[:, hp * 128:(hp + 1) * 128], k.c_ident[:], ["rw_yb"], [("rw_pT", ip)])
                k.copy(k.evac_engine(), mb[:, :, sub * 128:(sub + 1) * 128], pTt[ip][:].rearrange("p (h c) -> p h c", c=128), [("rw_pT", ip)], [mbk])
                if sub == 3:
                    k.ld(k.A["mixT"][b][:, 0:8, :], mb[:], reads=[mbk], writes=[("D", "mixT", b, 0)], slot=("rwst", b % 2))


K.rwkv = _rwkv


def _mlstm(self, L, pT):
    k = self
    j = L // 2
    TB = 256
    I = k.I
    NT = TB // 128
    with k.phase() as P:
        mU = _blockmask(k, P, "ml_mU", 128, True)
        ones4 = P.sb("ml_ones4", [4, TB])
        k.v("pool", "memset", [], ["ml_ones4"], ap=ones4[:], constant=1.0)
        selh = P.sb("ml_selh", [4, 4, 128])
        k.v("pool", "memset", [], ["ml_selh"], ap=selh[:], constant=1.0)
        k.v("pool", "affine_select", ["ml_selh"], ["ml_selh"], out=selh[:], in_=selh[:], pattern=[[-1, 4], [0, 128]],
            compare_op=ALU.is_equal, fill=0.0, base=0, channel_multiplier=1)
        cw = P.sb("ml_cw", [128, 8, 4]); cb = P.sb("ml_cb", [128, 8])
        for i_ in range(4):
            k.sch.dma("sp", ("mlcw", i_), lambda e, i_=i_: e.dma_start(out=cw[:, :, i_], in_=I["mlstm_conv_w"][j, i_].rearrange("(c p) -> p c", p=128),
                                                                   allow_slow_non_contiguous=True), writes=["ml_cw"])
        k.sch.dma("sp", "mlcb", lambda e: e.dma_start(out=cb[:], in_=I["mlstm_conv_b"][j].rearrange("(c p) -> p c", p=128),
                                                       allow_slow_non_contiguous=True), writes=["ml_cb"])
        bi = P.sb("ml_bi", [4, 1]); bf = P.sb("ml_bf", [4, 1]); nbf = P.sb("ml_nbf", [4, 1]); one1 = P.sb("ml_one1", [4, 1])
        k.sch.dma("sp", "mlbi", lambda e: e.dma_start(out=bi[:], in_=I["mlstm_b_i"][j].rearrange("(h o) -> h o", o=1)), writes=["ml_bi"])
        k.sch.dma("sp", "mlbf", lambda e: e.dma_start(out=bf[:], in_=I["mlstm_b_f"][j].rearrange("(h o) -> h o", o=1)), writes=["ml_bf"])
        k.v("dve", "tensor_scalar", ["ml_bf"], ["ml_nbf"], out=nbf[:], in0=bf[:], scalar1=-1.0, scalar2=None, op0=ALU.mult)
        k.v("pool", "memset", [], ["ml_one1"], ap=one1[:], constant=1.0)
        qk = P.sb("ml_qk", [128, 8, TB + 3]); acc = P.sb("ml_acc", [128, 8, TB]); tmp = P.sb("ml_tmp", [128, 8, TB])
        qkb = P.sb("ml_qkb", [128, 8, TB], BF16)
        vv = P.sb("ml_v", [128, 8, TB]); vb = P.sb("ml_vb", [128, 8, TB], BF16)
        og = P.sb("ml_og", [128, 8, TB])
        gi = P.sb("ml_gi", [4, TB]); gf = P.sb("ml_gf", [4, TB])
        Fs = [P.sb(f"ml_F{i}", [4, TB]) for i in range(2)]; Ms = [P.sb(f"ml_M{i}", [4, TB]) for i in range(2)]
        cv = P.sb("ml_cv", [4, TB]); rw = P.sb("ml_rw", [4, 3, TB]); ec = P.sb("ml_ec", [4, NT]); mprev = P.sb("ml_mprev", [4, NT])
        zero1 = P.sb("ml_zero1", [4, 1])
        k.v("pool", "memset", [], ["ml_zero1"], ap=zero1[:], constant=0.0)
        tok = [P.sb(f"ml_tok{i}", [128, 12]) for i in range(2)]
        ecb = [P.sb(f"ml_ecb{i}", [128, 4]) for i in range(2)]
        ktm = [P.sb(f"ml_ktm{i}", [128, 128], BF16) for i in range(2)]
        vx = [P.sb(f"ml_vx{i}", [128, 257], BF16) for i in range(2)]
        aT = [P.sb(f"ml_aT{i}", [128, 128], BF16) for i in range(2)]
        sc = [P.sb(f"ml_sc{i}", [128, 8]) for i in range(2)]
        hb_ = [P.sb(f"ml_hb{i}", [128, 256], BF16) for i in range(2)]
        C32 = P.sb("ml_C32", [128, 4, 257]); Cbf = P.sb("ml_Cbf", [128, 4, 2, 257], BF16)
        k.v("pool", "memset", [], ["ml_C32"], ap=C32[:], constant=0.0)
        k.v("pool", "memset", [], [("ml_Cbf", h, 0) for h in range(4)], ap=Cbf[:, :, 0, :], constant=0.0)
        mixb = [P.sb(f"ml_mix{i}", [128, 8, BLK], BF16) for i in range(2)]
        pTk = P.ps("ml_pTk", [128, 1024], BF16); pTv = P.ps("ml_pTv", [128, 1024], BF16); pTh = P.ps("ml_pTh", [128, 1024], BF16)
        pA = P.ps("ml_pA", [128, 512]); pO = [P.ps(f"ml_pO{i}", [128, 512]) for i in range(2)]; pC = P.ps("ml_pC", [128, 512])
        pS = P.ps("ml_pS", [128, 512])
        fl = lambda t: t[:].rearrange("p h t -> p (h t)")
        nhb = k.S // TB
        cn = {"u": 0, "o": 0}
        for hb in range(nhb):
            t0 = hb * TB
            bks = sorted(set(t // BLK for t in (max(t0 - 3, 0), t0 + TB - 1)))
            rk = lambda c0, n: [("D", "pT", c0 + h, bb) for h in range(n) for bb in bks]
            if hb == 0:
                k.v("pool", "memset", [], ["ml_qk"], ap=qk[:, :, 0:3], constant=0.0)
                k.ld(qk[:, :, 3:], pT[24:32, :, 0:TB].rearrange("c p t -> p c t"), reads=rk(24, 8), writes=["ml_qk"], slot="mlqk")
            else:
                k.ld(qk[:], pT[24:32, :, t0 - 3:t0 + TB].rearrange("c p t -> p c t"), reads=rk(24, 8), writes=["ml_qk"], slot="mlqk")
            k.ld(vv[:], pT[32:40, :, t0:t0 + TB].rearrange("c p t -> p c t"), reads=rk(32, 8), writes=["ml_v"], slot="mlv")
            k.ld(og[:], pT[40:48, :, t0:t0 + TB].rearrange("c p t -> p c t"), reads=rk(40, 8), writes=["ml_og"], slot="mlog")
            k.ld(gi[:], pT[48, 0:4, t0:t0 + TB], reads=rk(48, 1), writes=["ml_gi"], slot="mlgi")
            k.ld(gf[:], pT[48, 4:8, t0:t0 + TB], reads=rk(48, 1), writes=["ml_gf"], slot="mlgf")
            wb = lambda i: cw[:, :, i:i + 1].to_broadcast([128, 8, TB])
            k.v("dve", "tensor_tensor", ["ml_qk", "ml_cw"], ["ml_acc"], out=acc[:], in0=qk[:, :, 3:3 + TB], in1=wb(3), op=ALU.mult)
            for i in range(3):
                k.v("pool", "tensor_tensor", ["ml_qk", "ml_cw"], ["ml_tmp"], out=tmp[:], in0=qk[:, :, i:i + TB], in1=wb(i), op=ALU.mult)
                k.v("dve", "tensor_tensor", ["ml_tmp", "ml_acc"], ["ml_acc"], out=fl(acc), in0=fl(acc), in1=fl(tmp), op=ALU.add)
            for c in range(8):
                k.act(qkb[:, c, :], acc[:, c, :], AF.Silu, ["ml_acc", "ml_cb"], [("ml_qkb", c)], bias=cb[:, c:c + 1])
            qkk = [("ml_qkb", c) for c in range(8)]
            k.v("pool", "tensor_copy", ["ml_v"], ["ml_vb"], out=fl(vb), in_=fl(vv))
            k.act(fl(og), fl(og), AF.Sigmoid, ["ml_og"], ["ml_og"])
            Fc, Fp = Fs[hb % 2], Fs[(hb + 1) % 2]
            Mc, Mp = Ms[hb % 2], Ms[(hb + 1) % 2]
            fk, fpk, mk, mpk = ("ml_F", hb % 2), ("ml_F", (hb + 1) % 2), ("ml_M", hb % 2), ("ml_M", (hb + 1) % 2)
            k.act(gf[:], gf[:], AF.Exp, ["ml_gf", "ml_nbf"], ["ml_gf"], scale=-1.0, bias=nbf[:, 0:1])
            k.act(gf[:], gf[:], AF.Ln, ["ml_gf", "ml_one1"], ["ml_gf"], bias=one1[:, 0:1])
            k.v("dve", "tensor_scalar", ["ml_gf"], ["ml_gf"], out=gf[:], in0=gf[:], scalar1=-1.0, scalar2=None, op0=ALU.mult)
            k.v("dve", "tensor_tensor_scan", ["ml_gf", "ml_ones4", fpk], [fk], out=Fc[:], data0=ones4[:], data1=gf[:],
                initial=(0.0 if hb == 0 else Fp[:, TB - 1:TB]), op0=ALU.mult, op1=ALU.add)
            k.v("dve", "scalar_tensor_tensor", ["ml_gi", "ml_bi", fk], ["ml_cv"], out=cv[:], in0=gi[:], scalar=bi[:, 0:1], in1=Fc[:],
                op0=ALU.add, op1=ALU.subtract)
            k.v("dve", "tensor_tensor_scan", ["ml_cv", "ml_ones4", mpk], [mk], out=Mc[:], data0=ones4[:], data1=cv[:],
                initial=(0.0 if hb == 0 else Mp[:, TB - 1:TB]), op0=ALU.mult, op1=ALU.max)
            for ti in range(NT):
                if ti == 0:
                    src = zero1[:, 0:1] if hb == 0 else Mp[:, TB - 1:TB]
                    sk = ["ml_zero1"] if hb == 0 else [mpk]
                else:
                    src = Mc[:, ti * 128 - 1:ti * 128]; sk = [mk]
                k.v("dve", "tensor_copy", sk, ["ml_mprev"], out=mprev[:, ti:ti + 1], in_=src)
            Mc3 = Mc[:].rearrange("p (c j) -> p c j", j=128)
            mpb = mprev[:].rearrange("p (c o) -> p c o", o=1).to_broadcast([4, NT, 128])
            mlast = Mc3[:, :, 127:128]
            r3 = lambda i: rw[:, i, :].rearrange("p (c j) -> p c j", j=128)
            cv3 = cv[:].rearrange("p (c j) -> p c j", j=128)
            k.v("dve", "tensor_tensor", [mk, "ml_mprev"], ["ml_rw"], out=r3(0), in0=mpb, in1=Mc3, op=ALU.subtract)
            k.v("dve", "tensor_tensor", ["ml_cv", "ml_mprev"], ["ml_rw"], out=r3(1), in0=cv3, in1=mpb, op=ALU.subtract)
            k.v("dve", "tensor_tensor", ["ml_cv", mk], ["ml_rw"], out=r3(2), in0=cv3, in1=mlast.to_broadcast([4, NT, 128]), op=ALU.subtract)
            k.act(rw[:].rearrange("p a t -> p (a t)"), rw[:].rearrange("p a t -> p (a t)"), AF.Exp, ["ml_rw"], ["ml_rw"])
            k.v("dve", "tensor_tensor", [mk, "ml_mprev"], ["ml_ec"], out=ec[:].rearrange("p (c o) -> p c o", o=1),
                in0=mprev[:].rearrange("p (c o) -> p c o", o=1), in1=mlast, op=ALU.subtract)
            k.act(ec[:], ec[:], AF.Exp, ["ml_ec"], ["ml_ec"])
            k.v("dve", "tensor_tensor", [fk, mk, "ml_rw"], ["ml_cv"], out=cv[:], in0=Fc[:], in1=Mc[:], op=ALU.add)
            k.act(cv[:], cv[:], AF.Exp, ["ml_cv"], ["ml_cv"], scale=-1.0)
            for ti in range(NT):
                tg = hb * NT + ti
                b = (tg * 128) // BLK
                sub = tg % 4
                mb = mixb[b % 2]; mbk = ("ml_mix", b % 2)
                ts = slice(ti * 128, (ti + 1) * 128)
                i2 = tg % 2
                tk_ = tok[i2]; tkk = ("ml_tok", i2)
                for a in range(3):
                    k.tr(pS[:, 4 * a:4 * a + 4], rw[:, a, ts], k.c_identf[0:4, 0:4], ["ml_rw", "c_identf"], ["ml_pS"])
                k.tr(pS[:, 12:16], cv[:, ts], k.c_identf[0:4, 0:4], ["ml_cv", "c_identf"], ["ml_pS"])
                for h in range(4):
                    k.mm(pS[:, 16 + h:17 + h], selh[:, h, :], ec[:, ti:ti + 1], True, True, ["ml_selh", "ml_ec"], ["ml_pS"])
                tkb = P.sb if False else None
                k.copy("act", tk_[:, 0:12], pS[:, 0:12], ["ml_pS"], [tkk])
                eb_ = ecb[i2]; ebk = ("ml_ecb", i2)
                k.copy("dve", eb_[:, 0:4], pS[:, 16:20], ["ml_pS"], [ebk])
                en_ = sc[i2]; enk = ("ml_sc", i2)
                k.copy("dve", en_[:, 0:4], pS[:, 12:16], ["ml_pS"], [enk])
                for h in range(4):
                    u = cn["u"] % 2; cn["u"] += 1
                    slot = tg % 2
                    k.tr(pTk[:, 0:128], qkb[:, 4 + h, ts], k.c_ident[:], qkk, ["ml_pTk"])
                    k.v("dve", "tensor_scalar", ["ml_pTk", tkk], [("ml_ktm", u)], out=ktm[u][:], in0=pTk[:, 0:128],
                        scalar1=tk_[:, 8 + h:9 + h], scalar2=128 ** -0.5, op0=ALU.mult, op1=ALU.mult)
                    for half in range(2):
                        k.tr(pTv[:, half * 128:(half + 1) * 128], vb[:, 2 * h + half, ts], k.c_ident[:], ["ml_vb"], ["ml_pTv"])
                    k.copy("act", vx[u][:, 0:256], pTv[:, 0:256], ["ml_pTv"], [("ml_vx", u)])
                    k.v("pool", "memset", [("ml_vx", u)], [("ml_vx", u)], ap=vx[u][:, 256:257], constant=1.0)
                    k.mm(pA[:, 0:128], qkb[:, 4 + h, ts], qkb[:, h, ts], True, True, qkk, ["ml_pA"])
                    k.v("dve", "tensor_scalar", ["ml_pA", tkk], [("ml_aT", u)], out=aT[u][:], in0=pA[:, 0:128],
                        scalar1=tk_[:, 4 + h:5 + h], scalar2=128 ** -0.5, op0=ALU.mult, op1=ALU.mult)
                    k.v("pool", "tensor_tensor", [("ml_aT", u), "ml_mU"], [("ml_aT", u)], out=aT[u][:], in0=aT[u][:], in1=mU[:], op=ALU.mult)
                    io = cn["o"] % 2; cn["o"] += 1
                    po = pO[io]; pok = ("ml_pO", io)
                    k.mm(po[:, 0:257], qkb[:, h, ts], Cbf[:, h, slot, :], True, False, qkk + [("ml_Cbf", h, slot)], [pok])
                    k.mm(po[:, 0:257], aT[u][:], vx[u][:], False, True, [("ml_aT", u), ("ml_vx", u)], [pok])
                    k.mm(pC[:, 0:257], ktm[u][:], vx[u][:], True, True, [("ml_ktm", u), ("ml_vx", u)], ["ml_pC"])
                    k.v("dve", "scalar_tensor_tensor", ["ml_pC", ("ml_C32", h), ebk], [("ml_C32", h)], out=C32[:, h, :], in0=C32[:, h, :],
                        scalar=eb_[:, h:h + 1], in1=pC[:, 0:257], op0=ALU.mult, op1=ALU.add)
                    k.copy("act", Cbf[:, h, 1 - slot, :], C32[:, h, :], [("ml_C32", h)], [("ml_Cbf", h, 1 - slot)])
                    s_ = sc[i2]
                    k.act(s_[:, 4:5], po[:, 256:257], AF.Abs, [pok, tkk, enk], [enk], scale=tk_[:, h:h + 1])
                    k.v("dve", "tensor_tensor", [enk], [enk], out=s_[:, 5:6], in0=s_[:, 4:5], in1=en_[:, h:h + 1], op=ALU.max)
                    k.v("dve", "reciprocal", [enk], [enk], out=s_[:, 6:7], in_=s_[:, 5:6])
                    k.v("dve", "tensor_tensor", [enk, tkk], [enk], out=s_[:, 7:8], in0=s_[:, 6:7], in1=tk_[:, h:h + 1], op=ALU.mult)
                    k.v("dve", "tensor_scalar", [pok, enk], [("ml_hb", u)], out=hb_[u][:], in0=po[:, 0:256], scalar1=s_[:, 7:8], scalar2=None,
                        op0=ALU.mult)
                    for half in range(2):
                        k.tr(pTh[:, half * 128:(half + 1) * 128], hb_[u][:, half * 128:(half + 1) * 128], k.c_ident[:], [("ml_hb", u)], ["ml_pTh"])
                    k.v("dve", "tensor_tensor", ["ml_pTh", "ml_og"], [mbk], out=mb[:, 2 * h:2 * h + 2, sub * 128:(sub + 1) * 128],
                        in0=pTh[:, 0:256].rearrange("p (a c) -> p a c", c=128), in1=og[:, 2 * h:2 * h + 2, ts], op=ALU.mult)
                if sub == 3:
                    k.ld(k.A["mixT"][b][:, 8:16, :], mb[:], reads=[mbk], writes=[("D", "mixT", b, 1)], slot=("mlst", b % 2))


K.mlstm = _mlstm


def _dil(self, L, pT):
    k = self
    S = k.S
    SB = min(2048, S)
    NSB = S // SB
    PAIRS = [(1, 0), (4, 1), (16, 2)]
    if "Vtm" not in k.A:
        k.A["Vtm"] = k.dram("dl_Vtm", [S, 1024], BF16)
        k.A["OP"] = k.dram("dl_OP", [3, S, 16 * 66], F32)
    Vtm, OP = k.A["Vtm"], k.A["OP"]
    with k.phase() as P:
        vf = [P.sb(f"dv_vf{i}", [128, 8, BLK]) for i in range(2)]
        vb = [P.sb(f"dv_vb{i}", [128, 8, BLK], BF16) for i in range(2)]
        vt = [P.sb(f"dv_vt{i}", [128, 1024], BF16) for i in range(2)]
        pt = [P.ps(f"dv_pt{i}", [128, 1024], BF16) for i in range(2)]
        n = 0
        for b in range(k.NB):
            i = b % 2
            k.ld(vf[i][:], pT[16:24, :, b * BLK:(b + 1) * BLK].rearrange("c p t -> p c t"), reads=[("D", "pT", 16 + h, b) for h in range(8)],
                 writes=[("dv_vf", i)], slot=("dvld", i))
            k.act(vb[i][:].rearrange("p h t -> p (h t)"), vf[i][:].rearrange("p h t -> p (h t)"), AF.Copy, [("dv_vf", i)], [("dv_vb", i)])
            for sub in range(4):
                ii = n % 2; n += 1
                for hp in range(8):
                    k.tr(pt[ii][:, hp * 128:(hp + 1) * 128], vb[i][:, hp, sub * 128:(sub + 1) * 128], k.c_ident[:], [("dv_vb", i)], [("dv_pt", ii)])
                k.copy("dve", vt[ii][:], pt[ii][:], [("dv_pt", ii)], [("dv_vt", ii)])
                t0 = b * BLK + sub * 128
                k.ld(Vtm[t0:t0 + 128, :], vt[ii][:], reads=[("dv_vt", ii)], writes=[("D", "Vtm", t0 // 128)], slot=("dvst", ii))
    with k.phase() as P:
        maskb = P.sb("dl_maskb", [128, 256])
        k.v("pool", "memset", [], ["dl_maskb"], ap=maskb[:], constant=0.0)
        k.v("pool", "affine_select", ["dl_maskb"], ["dl_maskb"], out=maskb[:, 0:128], in_=maskb[:, 0:128], pattern=[[1, 128]],
            compare_op=ALU.is_ge, fill=-30000.0, base=0, channel_multiplier=-1)
        k.v("pool", "affine_select", ["dl_maskb"], ["dl_maskb"], out=maskb[:, 128:256], in_=maskb[:, 128:256], pattern=[[-1, 128]],
            compare_op=ALU.is_ge, fill=-30000.0, base=0, channel_multiplier=1)
        qsb = P.sb("dl_q", [128, 8, SB], BF16)
        ksb = [P.sb(f"dl_k{i}", [128, 8, SB], BF16) for i in range(2)]
        stg = [P.sb(f"dl_stg{i}", [128, 8, BLK]) for i in range(2)]
        vcur = [P.sb(f"dl_vc{i}", [128, 1024], BF16) for i in range(2)]
        vprv = [P.sb(f"dl_vp{i}", [128, 1024], BF16) for i in range(2)]
        ou = [P.sb(f"dl_ou{i}", [128, 16, 66]) for i in range(2)]
        ssb = [P.sb(f"dl_ssb{i}", [128, 256]) for i in range(2)]
        nmx = [P.sb(f"dl_nmx{i}", [128, 1]) for i in range(2)]
        pb = [P.sb(f"dl_pb{i}", [128, 256], BF16) for i in range(2)]
        ptb = [P.sb(f"dl_ptb{i}", [128, 256], BF16) for i in range(2)]
        pSc = [P.ps(f"dl_pSc{i}", [128, 512]) for i in range(2)]
        pPT = [P.ps(f"dl_pPT{i}", [128, 1024], BF16) for i in range(2)]
        pOv = [P.ps(f"dl_pOv{i}", [128, 512]) for i in range(2)]
        ns = 0; nu = 0; nh = 0
        for sb in range(NSB):
            kb = ksb[sb % 2]; kbk = ("dl_k", sb % 2)
            kp = ksb[(sb + 1) % 2]; kpk = ("dl_k", (sb + 1) % 2)
            for piece in range(SB // BLK):
                b = sb * (SB // BLK) + piece
                for (dst, dkey, c0, scale) in ((qsb, "dl_q", 0, 0.125), (kb, kbk, 8, 1.0)):
                    sg_ = stg[ns % 2]; sk = ("dl_stg", ns % 2); ns += 1
                    k.ld(sg_[:], pT[c0:c0 + 8, :, b * BLK:(b + 1) * BLK].rearrange("c p t -> p c t"),
                         reads=[("D", "pT", c0 + h, b) for h in range(8)], writes=[sk], slot=("dlld", ns % 2))
                    k.act(dst[:, :, piece * BLK:(piece + 1) * BLK], sg_[:], AF.Copy, [sk], [dkey], scale=scale)
            for (d, pi) in PAIRS:
                nblk = SB // (128 * d)
                qv = qsb[:].rearrange("p h (m i d) -> p h m i d", i=128, d=d)
                kcv = kb[:].rearrange("p h (m i d) -> p h m i d", i=128, d=d)
                kpv = kp[:].rearrange("p h (m i d) -> p h m i d", i=128, d=d)
                Vd = Vtm.rearrange("(n d) c -> d n c", d=d)
                Od = OP[pi].rearrange("(n d) c -> d n c", d=d)
                for r in range(d):
                    for m in range(nblk):
                        n0 = (sb * SB + m * 128 * d) // d
                        hasprev = not (sb == 0 and m == 0)
                        iu = nu % 2; nu += 1
                        vc = vcur[iu]; vck = ("dl_vc", iu)
                        vp = vprv[iu]; vpk = ("dl_vp", iu)
                        tiles = lambda n_: sorted(set(((n_ + i_) * d + r) // 128 for i_ in range(128)))
                        k.ld(vc[:], Vd[r, n0:n0 + 128, :], reads=[("D", "Vtm", t_) for t_ in tiles(n0)], writes=[vck], slot=("dlvc", iu))
                        if hasprev:
                            k.ld(vp[:], Vd[r, n0 - 128:n0, :], reads=[("D", "Vtm", t_) for t_ in tiles(n0 - 128)], writes=[vpk], slot=("dlvp", iu))
                        o_u = ou[iu]; ouk = ("dl_ou", iu)
                        W = 256 if hasprev else 128
                        c_lo = 0 if hasprev else 128
                        for hp in range(8):
                            for par in range(2):
                                h = 2 * hp + par
                                pr = slice(par * 64, (par + 1) * 64)
                                ih = nh % 2; nh += 1
                                psc = pSc[ih]; psk = ("dl_pSc", ih)
                                qT = qv[pr, hp, m, :, r]
                                if hasprev:
                                    kprev = kcv[pr, hp, m - 1, :, r] if m > 0 else kpv[pr, hp, nblk - 1, :, r]
                                    k.mm(psc[:, 0:128], qT, kprev, True, True, ["dl_q", kbk if m > 0 else kpk], [psk])
                                k.mm(psc[:, 128:256], qT, kcv[pr, hp, m, :, r], True, True, ["dl_q", kbk], [psk])
                                s_ = ssb[ih]; ssk = ("dl_ssb", ih)
                                k.v("dve", "tensor_tensor", [psk, "dl_maskb"], [ssk], out=s_[:, c_lo:256], in0=psc[:, c_lo:256], in1=maskb[:, c_lo:256], op=ALU.add)
                                k.v("dve", "tensor_reduce", [ssk], [ouk], out=o_u[:, h, 64:65], in_=s_[:, c_lo:256], axis=AX.X, op=ALU.max)
                                k.v("dve", "tensor_scalar", [ouk], [("dl_nmx", ih)], out=nmx[ih][:], in0=o_u[:, h, 64:65], scalar1=-1.0, scalar2=None, op0=ALU.mult)
                                p_ = pb[ih]; pbk = ("dl_pb", ih)
                                k.act(p_[:, c_lo:256], s_[:, c_lo:256], AF.Exp, [ssk, ("dl_nmx", ih)], [pbk, ouk], bias=nmx[ih][:, 0:1], accum_out=o_u[:, h, 65:66])
                                ppt = pPT[ih]; ppk = ("dl_pPT", ih)
                                if hasprev:
                                    k.tr(ppt[:, 0:128], p_[:, 0:128], k.c_ident[:], [pbk], [ppk])
                                k.tr(ppt[:, 128:256], p_[:, 128:256], k.c_ident[:], [pbk], [ppk])
                                pt_ = ptb[ih]; ptk = ("dl_ptb", ih)
                                k.copy("dve" if ih else "act", pt_[:, c_lo:256], ppt[:, c_lo:256], [ppk], [ptk])
                                pov = pOv[ih]; pok = ("dl_pOv", ih)
                                if hasprev:
                                    k.mm(pov[:, 0:64], pt_[:, 0:128], vp[:, h * 64:(h + 1) * 64], True, False, [ptk, vpk], [pok])
                                k.mm(pov[:, 0:64], pt_[:, 128:256], vc[:, h * 64:(h + 1) * 64], not hasprev, True, [ptk, vck], [pok])
                                k.copy("act", o_u[:, h, 0:64], pov[:, 0:64], [pok], [ouk])
                        k.ld(Od[r, n0:n0 + 128, :], o_u[:].rearrange("p h c -> p (h c)"), reads=[ouk],
                             writes=[("D", "OP", pi, t_) for t_ in tiles(n0)], slot=("dlou", iu))
    with k.phase() as P:
        o3 = [[P.sb(f"dc_o{p}_{i}", [128, 16, 66]) for p in range(3)] for i in range(2)]
        mall = P.sb("dc_mall", [128, 16]); wts = P.sb("dc_w", [128, 3, 16]); den = P.sb("dc_den", [128, 16]); tmp16 = P.sb("dc_t16", [128, 16])
        num = P.sb("dc_num", [128, 16, 64]); tmp = P.sb("dc_tmp", [128, 16, 64]); yb = P.sb("dc_yb", [128, 1024], BF16)
        mixb = [P.sb(f"dc_mix{i}", [128, 8, BLK], BF16) for i in range(2)]
        pt = [P.ps(f"dc_pt{i}", [128, 1024], BF16) for i in range(2)]
        b16 = lambda a: a.rearrange("p (h o) -> p h o", o=1).to_broadcast([128, 16, 64])
        for tg in range(S // 128):
            i = tg % 2
            b = tg // 4; sub = tg % 4
            mb = mixb[b % 2]; mbk = ("dc_mix", b % 2)
            oks = []
            for p in range(3):
                k.ld(o3[i][p][:].rearrange("p h c -> p (h c)"), OP[p, tg * 128:(tg + 1) * 128, :], reads=[("D", "OP", p, tg)],
                     writes=[("dc_o", p, i)], slot=("dcld", p, i))
                oks.append(("dc_o", p, i))
            mv = lambda p: o3[i][p][:, :, 64]
            lv = lambda p: o3[i][p][:, :, 65]
            k.v("dve", "tensor_tensor", oks, ["dc_mall"], out=mall[:], in0=mv(0), in1=mv(1), op=ALU.max)
            k.v("dve", "tensor_tensor", oks + ["dc_mall"], ["dc_mall"], out=mall[:], in0=mall[:], in1=mv(2), op=ALU.max)
            for p in range(3):
                k.v("dve", "tensor_tensor", oks + ["dc_mall"], ["dc_w"], out=wts[:, p, :], in0=mv(p), in1=mall[:], op=ALU.subtract)
            k.act(wts[:].rearrange("p a h -> p (a h)"), wts[:].rearrange("p a h -> p (a h)"), AF.Exp, ["dc_w"], ["dc_w"])
            k.v("dve", "tensor_tensor", oks + ["dc_w"], ["dc_den"], out=den[:], in0=wts[:, 0, :], in1=lv(0), op=ALU.mult)
            k.v("dve", "tensor_tensor", oks + ["dc_w"], ["dc_num"], out=num[:], in0=o3[i][0][:, :, 0:64], in1=b16(wts[:, 0, :]), op=ALU.mult)
            for p in (1, 2):
                k.v("dve", "tensor_tensor", oks + ["dc_w"], ["dc_t16"], out=tmp16[:], in0=wts[:, p, :], in1=lv(p), op=ALU.mult)
                k.v("dve", "tensor_tensor", ["dc_t16", "dc_den"], ["dc_den"], out=den[:], in0=den[:], in1=tmp16[:], op=ALU.add)
                k.v("pool", "tensor_tensor", oks + ["dc_w"], ["dc_tmp"], out=tmp[:], in0=o3[i][p][:, :, 0:64], in1=b16(wts[:, p, :]), op=ALU.mult)
                k.v("dve", "tensor_tensor", ["dc_tmp", "dc_num"], ["dc_num"], out=num[:], in0=num[:], in1=tmp[:], op=ALU.add)
            k.v("dve", "reciprocal", ["dc_den"], ["dc_den"], out=den[:], in_=den[:])
            k.v("dve", "tensor_tensor", ["dc_num", "dc_den"], ["dc_yb"], out=yb[:].rearrange("p (h c) -> p h c", c=64), in0=num[:], in1=b16(den[:]),
                op=ALU.mult)
            for hp in range(8):
                k.tr(pt[i][:, hp * 128:(hp + 1) * 128], yb[:, hp * 128:(hp + 1) * 128], k.c_ident[:], ["dc_yb"], [("dc_pt", i)])
            k.copy(k.evac_engine(), mb[:, :, sub * 128:(sub + 1) * 128], pt[i][:].rearrange("p (h c) -> p h c", c=128), [("dc_pt", i)], [mbk])
            if sub == 3:
                k.ld(k.A["mixT"][b][:, 0:8, :], mb[:], reads=[mbk], writes=[("D", "mixT", b, 0)], slot=("dcst", b % 2))


K.dil = _dil


def _even_mixer(self, L, xin):
    pT = self.inproj(L, 7424, 26, self.I["rwkv_mu"][L // 2])
    self.rwkv(L, pT)
    self.hgrn(L, pT)


def _odd_mixer(self, L, xin):
    pT = self.inproj(L, 6152, 0, None)
    self.dil(L, pT)
    self.mlstm(L, pT)


K.even_mixer = _even_mixer
K.odd_mixer = _odd_mixer

_INPUT_NAMES = ["ev_w_in", "ev_w_out", "rwkv_mu", "rwkv_w0", "rwkv_w2", "rwkv_a0", "rwkv_a2", "rwkv_g2", "rwkv_kk", "rwkv_ka",
                "rwkv_rk", "rwkv_lnx_w", "rwkv_lnx_b", "hgrn_lb", "hgrn_norm_w", "od_w_in", "od_w_out", "mlstm_conv_w",
                "mlstm_conv_b", "mlstm_b_i", "mlstm_b_f", "ln_w", "ln_b", "moe_wg", "moe_bg", "moe_we", "moe_be", "moe_w1",
                "moe_w3", "moe_w2"]


def make_in_maps(inputs, n_seq):
    f = lambda a: np.ascontiguousarray(np.asarray(a, dtype=np.float32))
    shared = {}
    for n in _INPUT_NAMES:
        a = f(inputs[n])
        if n == "rwkv_rk":
            a = a.reshape(a.shape[0], -1)
        if n == "moe_be":
            a = a.reshape(a.shape[0], -1)
        shared[n] = a
    x = f(inputs["x"])
    maps = []
    for c in range(n_seq):
        m = dict(shared)
        m["x"] = x[c]
        maps.append(m)
    return maps


def kernel(**inputs):
    x = np.asarray(inputs["x"])
    B, S, _ = x.shape
    NG = np.asarray(inputs["moe_wg"]).shape[-1]
    k = build(S, DEPTH, NG, mode="full")
    in_maps = make_in_maps(inputs, B)
    res = run_bass_kernel_spmd(k.nc, in_maps, core_ids=list(range(B)))
    out = np.stack([np.asarray(res.results[c]["out"], dtype=np.float32) for c in range(B)], axis=0)
    return out


def _phase_s(self, L):
    k = self
    NE, NBLK, S = k.NE, k.NBLK, k.S
    NT = S // 128
    IOA = bass.IndirectOffsetOnAxis
    with k.phase() as P:
        lt = _blockmask(k, P, "s_lt", 128, False)
        ltb = P.sb("s_ltb", [128, 128], BF16); oneb = P.sb("s_oneb", [128, 128], BF16)
        k.copy("dve", ltb[:], lt[:], ["s_lt"], ["s_ltb"])
        k.v("pool", "memset", [], ["s_oneb"], ap=oneb[:], constant=1.0)
        tot = P.sb("s_tot", [128, NE])
        k.v("pool", "memset", [], ["s_tot"], ap=tot[:], constant=0.0)
        g = [P.sb(f"s_g{i}", [128, NE]) for i in range(2)]
        ab = [P.sb(f"s_ab{i}", [128, NE], BF16) for i in range(2)]
        rk = [P.sb(f"s_rk{i}", [128, NE]) for i in range(2)]
        pc = [P.ps(f"s_pc{i}", [128, 512]) for i in range(2)]
        pcs = [P.ps(f"s_pcs{i}", [128, 512]) for i in range(2)]
        zt = P.sb("s_zero", [128, D], BF16)
        k.v("pool", "memset", [], ["s_zero"], ap=zt[:], constant=0.0)
        for b in range(NBLK):
            k.ld(k.A["xs"][b * 128:(b + 1) * 128, :], zt[:], reads=["s_zero"], writes=[("D", "xs", b)], slot=("szero", b % 4))
        for n in range(NT):
            i = n % 2
            t0 = n * 128
            k.ld(g[i][:], k.A["gates"][t0:t0 + 128, :], reads=[("D", "gates", t0)], writes=[("s_g", i)], slot=("sg", i))
            k.v("dve", "tensor_scalar", [("s_g", i)], [("s_ab", i)], out=ab[i][:], in0=g[i][:], scalar1=0.0, scalar2=None, op0=ALU.is_gt)
            k.mm(pc[i][:, 0:NE], ltb[:], ab[i][:], True, True, ["s_ltb", ("s_ab", i)], [("s_pc", i)])
            k.mm(pcs[i][:, 0:NE], oneb[:], ab[i][:], True, True, ["s_oneb", ("s_ab", i)], [("s_pcs", i)])
            k.v("dve", "tensor_tensor", [("s_pc", i), "s_tot"], [("s_rk", i)], out=rk[i][:], in0=pc[i][:, 0:NE], in1=tot[:], op=ALU.add)
            k.v("dve", "tensor_tensor", [("s_pcs", i), "s_tot"], ["s_tot"], out=tot[:], in0=pcs[i][:, 0:NE], in1=tot[:], op=ALU.add)
            k.ld(k.A["rank"][t0:t0 + 128, :], rk[i][:], reads=[("s_rk", i)], writes=[("D", "rank", t0)], slot=("srk", i))
        pad = P.sb("s_pad", [128, NE]); pend = P.sb("s_pend", [128, NE]); pst = P.sb("s_pst", [128, NE]); ones = P.sb("s_ones", [128, max(NE, NBLK)])
        k.v("pool", "memset", [], ["s_ones"], ap=ones[:], constant=1.0)
        blk0 = P.sb("s_blk0", [128, NBLK]); c128a = P.sb("s_c128a", [128, NBLK]); cmp = P.sb("s_cmp", [128, NE, NBLK])
        k.v("pool", "memset", [], ["s_c128a"], ap=c128a[:], constant=128.0)
        k.v("dve", "tensor_tensor_scan", ["s_c128a", "s_ones"], ["s_blk0"], out=blk0[:], data0=ones[:, 0:NBLK], data1=c128a[:], initial=-128.0,
            op0=ALU.mult, op1=ALU.add)
        k.v("dve", "tensor_tensor", ["s_tot", "s_blk0"], ["s_cmp"], out=cmp[:], in0=tot[:].rearrange("p (e o) -> p e o", o=1).to_broadcast([128, NE, NBLK]),
            in1=blk0[:].rearrange("p (o b) -> p o b", o=1).to_broadcast([128, NE, NBLK]), op=ALU.is_gt)
        k.v("dve", "tensor_reduce", ["s_cmp"], ["s_pad"], out=pad[:], in_=cmp[:], axis=AX.X, op=ALU.add)
        k.v("dve", "tensor_scalar", ["s_pad"], ["s_pad"], out=pad[:], in0=pad[:], scalar1=128.0, scalar2=None, op0=ALU.mult)
        k.v("dve", "tensor_tensor_scan", ["s_pad", "s_ones"], ["s_pend"], out=pend[:], data0=ones[:, 0:NE], data1=pad[:], initial=0.0,
            op0=ALU.mult, op1=ALU.add)
        k.v("dve", "tensor_tensor", ["s_pend", "s_pad"], ["s_pst"], out=pst[:], in0=pend[:], in1=pad[:], op=ALU.subtract)
        blk = P.sb("s_blk", [128, NBLK]); be = P.sb("s_be", [128, NBLK]); c128 = P.sb("s_c128", [128, NBLK])
        k.v("pool", "memset", [], ["s_c128"], ap=c128[:], constant=128.0)
        k.v("dve", "tensor_tensor_scan", ["s_c128", "s_ones"], ["s_blk"], out=blk[:], data0=ones[:, 0:NBLK], data1=c128[:], initial=-128.0,
            op0=ALU.mult, op1=ALU.add)
        k.v("pool", "memset", [], ["s_be"], ap=be[:], constant=0.0)
        for e in range(NE):
            k.v("dve", "scalar_tensor_tensor", ["s_blk", "s_pend", "s_be"], ["s_be"], out=be[:], in0=blk[:], scalar=pend[:, e:e + 1], in1=be[:],
                op0=ALU.is_ge, op1=ALU.add)
        pidx = P.sb("s_pidx", [128, 1]); wi = P.sb("s_wi", [128, NBLK], I32)
        k.mm(pc[0][:, 0:1], ltb[:], oneb[:, 0:1], True, True, ["s_ltb", "s_oneb"] + [("s_rk", 0), ("s_rk", 1)], [("s_pc", 0)])
        k.copy("dve", pidx[:], pc[0][:, 0:1], [("s_pc", 0)], ["s_pidx"])
        k.v("dve", "tensor_scalar", ["s_be"], ["s_be"], out=be[:], in0=be[:], scalar1=float(NE - 1), scalar2=128.0, op0=ALU.min, op1=ALU.mult)
        k.v("dve", "tensor_scalar", ["s_be", "s_pidx"], ["s_be"], out=be[:], in0=be[:], scalar1=pidx[:, 0:1], scalar2=None, op0=ALU.add)
        k.copy("dve", wi[:], be[:], ["s_be"], ["s_wi"])
        k.ld(k.A["widx"], wi[:], reads=["s_wi"], writes=[("D", "widx")], slot="swi")
        xb = [P.sb(f"s_xb{i}", [128, D], BF16) for i in range(2)]
        top = [P.sb(f"s_top{i}", [128, 8]) for i in range(2)]
        oh = [P.sb(f"s_oh{i}", [128, NE]) for i in range(2)]
        pg = [P.sb(f"s_pg{i}", [128, 4]) for i in range(2)]
        pi = [P.sb(f"s_pi{i}", [128, 2], I32) for i in range(2)]
        for n in range(NT):
            i = n % 2
            t0 = n * 128
            k.ld(g[i][:], k.A["gates"][t0:t0 + 128, :], reads=[("D", "gates", t0)], writes=[("s_g", i)], slot=("sg", i))
            k.ld(rk[i][:], k.A["rank"][t0:t0 + 128, :], reads=[("D", "rank", t0)], writes=[("s_rk", i)], slot=("srk2", i))
            k.ld(xb[i][:], k.A["x1b"][t0:t0 + 128, :], reads=[("D", "x1b", t0)], writes=[("s_xb", i)], slot=("sxb", i))
            k.v("dve", "max", [("s_g", i)], [("s_top", i)], out=top[i][:], in_=g[i][:])
            k.v("dve", "tensor_tensor", [("s_rk", i), "s_pst"], [("s_rk", i)], out=rk[i][:], in0=rk[i][:], in1=pst[:], op=ALU.add)
            pgk = ("s_pg", i)
            for jx in range(2):
                k.v("dve", "tensor_scalar", [("s_g", i), ("s_top", i)], [("s_oh", i)], out=oh[i][:], in0=g[i][:], scalar1=top[i][:, jx:jx + 1],
                    scalar2=None, op0=ALU.is_equal)
                k.v("dve", "tensor_tensor", [("s_oh", i), ("s_rk", i)], [("s_oh", i)], out=oh[i][:], in0=oh[i][:], in1=rk[i][:], op=ALU.mult)
                k.v("dve", "tensor_reduce", [("s_oh", i)], [pgk], out=pg[i][:, jx:jx + 1], in_=oh[i][:], axis=AX.X, op=ALU.add)
            k.copy("dve", pg[i][:, 2:4], top[i][:, 0:2], [("s_top", i)], [pgk])
            k.copy("dve", pi[i][:], pg[i][:, 0:2], [pgk], [("s_pi", i)])
            k.ld(k.A["pgt"][t0:t0 + 128, :], pg[i][:], reads=[pgk], writes=[("D", "pgt", t0)], slot=("spg", i))
            for jx in range(2):
                k.sch.dma("pool", ("sscat", i, jx), lambda e, i=i, jx=jx: e.indirect_dma_start(
                    out=k.A["xs"][:, :], out_offset=IOA(ap=pi[i][:, jx:jx + 1], axis=0), in_=xb[i][:], in_offset=None),
                    reads=[("s_xb", i), ("s_pi", i)] + [("D", "xs", b) for b in range(NBLK)], writes=[("D", "xsc", n, jx)])


def _phase_m2(self, L):
    k = self
    NE, NBLK = k.NE, k.NBLK
    IOA = bass.IndirectOffsetOnAxis
    W1f = k.W["w1", L].rearrange("e p k f -> (e p) (k f)")
    W3f = k.W["w3", L].rearrange("e p k f -> (e p) (k f)")
    W2f = k.W["w2", L].rearrange("e p k f -> (e p) (k f)")
    allsc = [("D", "xsc", n, jx) for n in range(k.S // 128) for jx in range(2)]
    with k.phase() as P:
        wi = P.sb("m2_wi", [128, NBLK], I32)
        k.ld(wi[:], k.A["widx"], reads=[("D", "widx")], writes=["m2_wi"], slot="m2wi")
        w1 = [P.sb(f"m2_w1_{i}", [128, KC * 512], BF16) for i in range(2)]
        w3 = [P.sb(f"m2_w3_{i}", [128, KC * 512], BF16) for i in range(2)]
        w2 = [P.sb(f"m2_w2_{i}", [128, 4 * D], BF16) for i in range(2)]
        xs = [P.sb(f"m2_xs{i}", [128, D], BF16) for i in range(2)]
        xT = [P.sb(f"m2_xT{i}", [128, KC, 128], BF16) for i in range(2)]
        hT = [P.sb(f"m2_hT{i}", [128, 4, 128], BF16) for i in range(2)]
        sl = [P.sb(f"m2_sl{i}", [128, 128]) for i in range(2)]
        ysb = [P.sb(f"m2_y{i}", [128, D]) for i in range(2)]
        pt = P.ps("m2_pt", [128, KC, 128], BF16)
        p1 = [P.ps(f"m2_p1_{i}", [128, 512]) for i in range(1)]
        p3 = [P.ps(f"m2_p3_{i}", [128, 512]) for i in range(1)]
        po = [P.ps(f"m2_po{i}", [128, 512]) for i in range(2)]
        npo = 0
        first = True
        for b in range(NBLK):
            i = b % 2
            rds = ["m2_wi"] + (allsc if first else [])
            for (wt, Wf, nm) in ((w1[i], W1f, "w1"), (w3[i], W3f, "w3"), (w2[i], W2f, "w2")):
                k.sch.dma("pool", ("m2g", nm, i), lambda e, wt=wt, Wf=Wf, b=b: e.indirect_dma_start(
                    out=wt[:, :], out_offset=None, in_=Wf[:, :], in_offset=IOA(ap=wi[:, b:b + 1], axis=0)),
                    reads=["m2_wi", ("D", nm, L, 0)], writes=[("m2_" + nm, i)])
            k.ld(xs[i][:], k.A["xs"][b * 128:(b + 1) * 128, :], reads=rds + [("D", "xs", b)], writes=[("m2_xs", i)], slot=("m2xs", i))
            first = False
            for kc in range(KC):
                k.tr(pt[:, kc, :], xs[i][:, kc * 128:(kc + 1) * 128], k.c_ident[:], [("m2_xs", i)], ["m2_pt"])
            k.copy(k.evac_engine(), xT[i][:], pt[:], ["m2_pt"], [("m2_xT", i)])
            for fc in range(4):
                for kc in range(KC):
                    k.mm(p1[0][:, 0:128], w1[i][:, kc * 512 + fc * 128: kc * 512 + (fc + 1) * 128], xT[i][:, kc, :], kc == 0, kc == KC - 1,
                         [("m2_w1", i), ("m2_xT", i)], [("m2_p1", 0)])
                for kc in range(KC):
                    k.mm(p3[0][:, 0:128], w3[i][:, kc * 512 + fc * 128: kc * 512 + (fc + 1) * 128], xT[i][:, kc, :], kc == 0, kc == KC - 1,
                         [("m2_w3", i), ("m2_xT", i)], [("m2_p3", 0)])
                k.act(sl[fc % 2][:], p1[0][:, 0:128], AF.Silu, [("m2_p1", 0)], [("m2_sl", fc % 2)])
                k.v("dve", "tensor_tensor", [("m2_sl", fc % 2), ("m2_p3", 0)], [("m2_hT", i)], out=hT[i][:, fc, :], in0=sl[fc % 2][:], in1=p3[0][:, 0:128], op=ALU.mult)
            for dc in range(4):
                pp = po[npo % 2]; pk = ("m2_po", npo % 2); npo += 1
                for fc in range(4):
                    k.mm(pp[:], hT[i][:, fc, :], w2[i][:, fc * D + dc * 512: fc * D + (dc + 1) * 512], fc == 0, fc == 3, [("m2_hT", i), ("m2_w2", i)], [pk])
                k.copy(k.evac_engine(), ysb[i][:, dc * 512:(dc + 1) * 512], pp[:], [pk], [("m2_y", i)])
            for hf in range(2):
                k.ld(k.A["ys"][hf][b * 128:(b + 1) * 128, :], ysb[i][:, hf * 1024:(hf + 1) * 1024], reads=[("m2_y", i)], writes=[("D", "ys", b, hf)], slot=("m2ys", i, hf))


def _phase_m3(self, L, xout):
    k = self
    NBLK = k.NBLK
    IOA = bass.IndirectOffsetOnAxis
    allys = [("D", "ys", b, hf) for b in range(NBLK) for hf in range(2)]
    with k.phase() as P:
        T = _ln_alloc(P, 2)
        lnw = P.sb("m3_lnw", [128, D]); lnb = P.sb("m3_lnb", [128, D])
        k.ld(lnw[:], k.I["ln_w"][L, 1:2, :].partition_broadcast(128), writes=["lnw"])
        k.ld(lnb[:], k.I["ln_b"][L, 1:2, :].partition_broadcast(128), writes=["lnb"])
        pg = [P.sb(f"m3_pg{i}", [128, 4]) for i in range(2)]
        pi = [P.sb(f"m3_pi{i}", [128, 2], I32) for i in range(2)]
        y = [[P.sb(f"m3_y{jx}_{i}", [128, D]) for i in range(2)] for jx in range(2)]
        xr = [P.sb(f"m3_xr{i}", [128, D]) for i in range(2)]
        blk = [P.sb(f"m3_blk{i}", [128, KC, BLK], BF16) for i in range(2)]
        for n in range(k.S // 128):
            i = n % 2
            t0 = n * 128
            b = n // 4; sub = n % 4
            bb = blk[b % 2]; bk = ("m3_blk", b % 2)
            k.ld(pg[i][:], k.A["pgt"][t0:t0 + 128, :], reads=[("D", "pgt", t0)], writes=[("m3_pg", i)], slot=("m3pg", i))
            k.copy("dve", pi[i][:], pg[i][:, 0:2], [("m3_pg", i)], [("m3_pi", i)])
            for jx in range(2):
                for hf in range(2):
                    k.sch.dma("pool", ("m3g", jx, hf, i), lambda e, i=i, jx=jx, hf=hf: e.indirect_dma_start(
                        out=y[jx][i][:, hf * 1024:(hf + 1) * 1024], out_offset=None, in_=k.A["ys"][hf][:, :],
                        in_offset=IOA(ap=pi[i][:, jx:jx + 1], axis=0)),
                        reads=[("m3_pi", i)] + (allys if n == 0 else []), writes=[("m3_y", jx, i)])
            xk = ("m3_xr", i)
            k.ld(xr[i][:], k.A["x1"][t0:t0 + 128, :], reads=[("D", "tm", id(k.A["x1"]), t0)], writes=[xk], slot=("m3xr", i))
            k.v("dve", "scalar_tensor_tensor", [xk, ("m3_y", 0, i), ("m3_pg", i)], [("m3_y", 0, i)], out=y[0][i][:], in0=y[0][i][:], scalar=pg[i][:, 2:3],
                in1=y[1][i][:], op0=ALU.mult, op1=ALU.bypass) if False else None
            k.v("pool", "tensor_scalar", [("m3_y", 0, i), ("m3_pg", i)], [("m3_y", 0, i)], out=y[0][i][:], in0=y[0][i][:], scalar1=pg[i][:, 2:3], scalar2=None, op0=ALU.mult)
            k.v("dve", "scalar_tensor_tensor", [("m3_y", 1, i), ("m3_y", 0, i), ("m3_pg", i)], [("m3_y", 0, i)], out=y[0][i][:], in0=y[1][i][:],
                scalar=pg[i][:, 3:4], in1=y[0][i][:], op0=ALU.mult, op1=ALU.add)
            k.v("dve", "scalar_tensor_tensor", [xk, ("m3_y", 0, i)], [xk], out=xr[i][:], in0=xr[i][:], scalar=DN_ALPHA, in1=y[0][i][:],
                op0=ALU.mult, op1=ALU.add)
            _ln_tail(k, P, T, xr[i], xk, lnw, lnb, xout, t0, bb, bk, sub, n)
            if sub == 3:
                k.ld(k.A["xT"][b], bb[:], reads=[bk], writes=[("D", "xT", b)], slot=("m3xT", b % 2))


K.phase_s = _phase_s
K.phase_m2 = _phase_m2
K.phase_m3 = _phase_m3
```
